# Optimizing a Trainium2 kernel written in Bass

```python
import math
import jax, jax.numpy as jnp
from jax import lax
import numpy as np

D_MODEL = 1024
BATCH = 32
SEQ = 2048
DEPTH = 2

RET_HEADS = 4
RET_DK = 256
RET_DV = 512
RET_CHUNK = 128
ROPE_BASE = 10000.0
GN_EPS = 1e-5
NSA_HEADS = 16
NSA_KV_GROUPS = 2
NSA_HD = 64
CMP_LEN = 32
CMP_STRIDE = 16
CMP_HIDDEN = 256
SEL_BLOCK = 64
SEL_TOPK = 8
WINDOW = 512
NSA_Q_BLOCK = 64
FORCED_SCORE = 1e4
REL_BUCKETS = 32
REL_MAX_DIST = 128
FFN_HIDDEN = -(-8 * D_MODEL // (3 * 256)) * 256
EPS = 1e-6
NEG_INF = -1e30
SPLIT_SIZES = (RET_HEADS * RET_DK, RET_HEADS * RET_DK, RET_HEADS * RET_DV, RET_HEADS * RET_DV,
               NSA_HEADS * NSA_HD, 6 * NSA_KV_GROUPS * NSA_HD, 3 * NSA_HEADS, D_MODEL, D_MODEL)
C_IN = sum(SPLIT_SIZES)

kernel_name = "hybrid_retnet_nsa_gated_block"


def rms_norm(x, g):
    xf = x.astype(jnp.float32)
    y = xf * lax.rsqrt(jnp.mean(xf * xf, -1, keepdims=True) + EPS)
    return (y * g.astype(jnp.float32)).astype(x.dtype)


def masked_softmax(logits, mask):
    z = jnp.where(mask, logits.astype(jnp.float32), NEG_INF)
    z = z - jnp.max(z, -1, keepdims=True)
    e = jnp.exp(z) * mask
    return e / jnp.maximum(jnp.sum(e, -1, keepdims=True), 1e-30)


def t5_bucket(dist):
    dist = jnp.maximum(dist, 0)
    max_exact = REL_BUCKETS // 2
    large = max_exact + (jnp.log(jnp.maximum(dist, 1).astype(jnp.float32) / max_exact)
                         / math.log(REL_MAX_DIST / max_exact) * (REL_BUCKETS - max_exact)).astype(jnp.int32)
    large = jnp.minimum(large, REL_BUCKETS - 1)
    return jnp.where(dist < max_exact, dist, large)


def rotary(x, pos):
    half = x.shape[-1] // 2
    freqs = ROPE_BASE ** (-jnp.arange(half, dtype=jnp.float32) / half)
    ang = pos.astype(jnp.float32)[:, None] * freqs
    cos = jnp.cos(ang)[None, :, None, :]
    sin = jnp.sin(ang)[None, :, None, :]
    x1, x2 = x[..., :half], x[..., half:]
    return jnp.concatenate([x1 * cos - x2 * sin, x1 * sin + x2 * cos], -1)


def retention(q, k, v):
    B, T, H, dk = q.shape
    dv = v.shape[-1]
    nc = T // RET_CHUNK
    lg = jnp.log(1.0 - 2.0 ** (-5.0 - jnp.arange(H, dtype=jnp.float32)))
    n = jnp.arange(RET_CHUNK, dtype=jnp.float32)
    diff = n[:, None] - n[None, :]
    dmask = jnp.where(diff >= 0, jnp.exp(jnp.maximum(diff, 0.0)[None] * lg[:, None, None]), 0.0)
    xi = jnp.exp((n + 1.0)[None] * lg[:, None])
    zeta = jnp.exp((RET_CHUNK - 1.0 - n)[None] * lg[:, None])
    g_chunk = jnp.exp(RET_CHUNK * lg)

    def to_chunks(t):
        return t.reshape(B, nc, RET_CHUNK, H, t.shape[-1]).transpose(1, 0, 3, 2, 4)

    def step(R, qkv):
        qc, kc, vc = qkv
        s = jnp.einsum('bhnd,bhmd->bhnm', qc, kc) * dmask
        o = (jnp.einsum('bhnm,bhme->bhne', s, vc)
             + jnp.einsum('bhnd,bhde->bhne', qc, R) * xi[:, :, None])
        R = g_chunk[:, None, None] * R + jnp.einsum('bhmd,bhme->bhde', kc * zeta[:, :, None], vc)
        return R, o

    R0 = jnp.zeros((B, H, dk, dv), jnp.float32)
    _, o = lax.scan(step, R0, (to_chunks(q), to_chunks(k), to_chunks(v)))
    return o.transpose(1, 0, 3, 2, 4).reshape(B, T, H, dv)


def compress(kv, pos_emb, w1, w2):
    T = kv.shape[2]
    n_cmp = (T - CMP_LEN) // CMP_STRIDE + 1
    idx = jnp.arange(n_cmp)[:, None] * CMP_STRIDE + jnp.arange(CMP_LEN)[None]
    blocks = kv[:, :, idx, :] + pos_emb
    flat = blocks.reshape(blocks.shape[0], blocks.shape[1], n_cmp, CMP_LEN * NSA_HD)
    return jax.nn.gelu(flat @ w1) @ w2


def nsa(q, k_c, v_c, k_s, v_s, k_w, v_w, gates, pos_k, pos_v, w1_k, w2_k, w1_v, w2_v, rel_bias):
    B, T, H, hd = q.shape
    G = NSA_KV_GROUPS
    HG = H // G
    QB = NSA_Q_BLOCK
    scale = hd ** -0.5
    grp = lambda t: t.transpose(0, 2, 1, 3)
    kc = compress(grp(k_c), pos_k, w1_k, w2_k)
    vc = compress(grp(v_c), pos_v, w1_v, w2_v)
    n_cmp = kc.shape[2]
    cmp_end = jnp.arange(n_cmp) * CMP_STRIDE + CMP_LEN - 1
    n_sel = T // SEL_BLOCK
    sel_k = min(SEL_TOPK, n_sel)
    sel_start = jnp.arange(n_sel) * SEL_BLOCK
    overlap = ((cmp_end[:, None] - CMP_LEN + 1 < sel_start[None] + SEL_BLOCK)
               & (cmp_end[:, None] >= sel_start[None])).astype(jnp.float32)
    ks_blocks = grp(k_s).reshape(B, G, n_sel, SEL_BLOCK, hd)
    vs_blocks = grp(v_s).reshape(B, G, n_sel, SEL_BLOCK, hd)
    kw_pad = jnp.pad(grp(k_w), ((0, 0), (0, 0), (WINDOW, 0), (0, 0)))
    vw_pad = jnp.pad(grp(v_w), ((0, 0), (0, 0), (WINDOW, 0), (0, 0)))
    table = rel_bias.reshape(REL_BUCKETS, G, HG).astype(jnp.float32)
    nq = T // QB
    qb = q.reshape(B, nq, QB, G, HG, hd).transpose(1, 0, 3, 4, 2, 5)
    gb = gates.reshape(B, nq, QB, G, HG, 3).transpose(1, 0, 3, 4, 2, 5)
    starts = jnp.arange(nq, dtype=jnp.int32) * QB
    bidx = jnp.arange(B)[:, None, None, None]
    gidx = jnp.arange(G)[None, :, None, None]
    jj = jnp.arange(n_sel)

    def head_bias(dist):
        return jnp.moveaxis(table[t5_bucket(dist)], (-2, -1), (0, 1))

    def sel_bias(dist):
        b = jax.vmap(lambda tb, bk: tb[bk], in_axes=(1, 1), out_axes=1)(table, t5_bucket(dist))
        return jnp.moveaxis(b, -1, 2)

    def block(args):
        qx, gx, s = args
        pos = s + jnp.arange(QB, dtype=jnp.int32)
        dc = pos[:, None] - cmp_end[None]
        lc = jnp.einsum('bghqd,bgnd->bghqn', qx, kc) * scale + head_bias(dc)
        pc = masked_softmax(lc, dc >= 0)
        oc = jnp.einsum('bghqn,bgnd->bghqd', pc.astype(vc.dtype), vc)
        imp = jnp.einsum('bghqn,nj->bgqj', pc, overlap)
        cur = pos // SEL_BLOCK
        forced = (jj[None] == 0) | (jj[None] == cur[:, None]) | (jj[None] == cur[:, None] - 1)
        imp = jnp.where(forced, FORCED_SCORE, imp)
        imp = jnp.where(sel_start[None] <= pos[:, None], imp, NEG_INF)
        _, sel = lax.top_k(imp, sel_k)
        ksel = ks_blocks[bidx, gidx, sel]
        vsel = vs_blocks[bidx, gidx, sel]
        kpos = sel[..., None] * SEL_BLOCK + jnp.arange(SEL_BLOCK)
        ds = pos[:, None, None] - kpos
        ls = jnp.einsum('bghqd,bgqkld->bghqkl', qx, ksel) * scale + sel_bias(ds)
        bsz, g_, hg_, qn, kk, bs = ls.shape
        ps = masked_softmax(ls.reshape(bsz, g_, hg_, qn, kk * bs),
                            (ds >= 0)[:, :, None].reshape(bsz, g_, 1, qn, kk * bs))
        os_ = jnp.einsum('bghqm,bgqmd->bghqd', ps.astype(vsel.dtype),
                         vsel.reshape(bsz, g_, qn, kk * bs, hd))
        kw = lax.dynamic_slice_in_dim(kw_pad, s, WINDOW + QB, axis=2)
        vw = lax.dynamic_slice_in_dim(vw_pad, s, WINDOW + QB, axis=2)
        wpos = s - WINDOW + jnp.arange(WINDOW + QB, dtype=jnp.int32)
        dw = pos[:, None] - wpos[None]
        lw = jnp.einsum('bghqd,bgld->bghql', qx, kw) * scale + head_bias(dw)
        pw = masked_softmax(lw, (dw >= 0) & (dw < WINDOW) & (wpos[None] >= 0))
        ow = jnp.einsum('bghql,bgld->bghqd', pw.astype(vw.dtype), vw)
        return gx[..., 0:1] * oc + gx[..., 1:2] * os_ + gx[..., 2:3] * ow

    out = lax.map(block, (qb, gb, starts))
    return out.transpose(1, 0, 4, 2, 3, 5).reshape(B, T, H * hd)


def setup_inputs(seed: int = 0) -> dict:
    key = jax.random.key(seed)
    ks = jax.random.split(key, 20)
    nrm = lambda k, shape, fan: jax.random.normal(k, shape, jnp.float32) * (fan ** -0.5)
    gain = lambda k, shape: 1.0 + 0.01 * jax.random.normal(k, shape, jnp.float32)
    L = DEPTH
    return {
        "x": jax.random.normal(ks[0], (BATCH, SEQ, D_MODEL), jnp.float32),
        "norm_mix_g": gain(ks[1], (L, D_MODEL)),
        "w_in": nrm(ks[2], (L, D_MODEL, C_IN), D_MODEL),
        "cmp_pos_k": 0.02 * jax.random.normal(ks[3], (L, CMP_LEN, NSA_HD), jnp.float32),
        "cmp_pos_v": 0.02 * jax.random.normal(ks[4], (L, CMP_LEN, NSA_HD), jnp.float32),
        "cmp_w1_k": nrm(ks[5], (L, CMP_LEN * NSA_HD, CMP_HIDDEN), CMP_LEN * NSA_HD),
        "cmp_w2_k": nrm(ks[6], (L, CMP_HIDDEN, NSA_HD), CMP_HIDDEN),
        "cmp_w1_v": nrm(ks[7], (L, CMP_LEN * NSA_HD, CMP_HIDDEN), CMP_LEN * NSA_HD),
        "cmp_w2_v": nrm(ks[8], (L, CMP_HIDDEN, NSA_HD), CMP_HIDDEN),
        "w_o_ret": nrm(ks[9], (L, RET_HEADS * RET_DV, D_MODEL), RET_HEADS * RET_DV),
        "w_o_nsa": nrm(ks[10], (L, NSA_HEADS * NSA_HD, D_MODEL), NSA_HEADS * NSA_HD),
        "w_out": nrm(ks[11], (L, D_MODEL, D_MODEL), D_MODEL),
        "norm_ffn_g": gain(ks[12], (L, D_MODEL)),
        "w_ffn_in": nrm(ks[13], (L, D_MODEL, 2 * FFN_HIDDEN), D_MODEL),
        "w_ffn_out": nrm(ks[14], (L, FFN_HIDDEN, D_MODEL), FFN_HIDDEN),
        "rel_bias": 0.2 * jax.random.normal(ks[15], (REL_BUCKETS, NSA_HEADS), jnp.float32),
        "norm_final_g": gain(ks[16], (D_MODEL,)),
    }


def reference(x, norm_mix_g, w_in, cmp_pos_k, cmp_pos_v, cmp_w1_k, cmp_w2_k, cmp_w1_v, cmp_w2_v,
              w_o_ret, w_o_nsa, w_out, norm_ffn_g, w_ffn_in, w_ffn_out, rel_bias, norm_final_g):
    B, T, _ = x.shape
    pos = jnp.arange(T, dtype=jnp.int32)
    cuts = [int(c) for c in np.cumsum(SPLIT_SIZES)[:-1]]
    for i in range(DEPTH):
        h = rms_norm(x, norm_mix_g[i])
        proj = h @ w_in[i]
        q_r, k_r, v_r, g_r, q_n, kv_n, gate_n, m_a, m_b = jnp.split(proj, cuts, axis=-1)
        qr = rotary(q_r.reshape(B, T, RET_HEADS, RET_DK).astype(jnp.float32), pos)
        kr = rotary(k_r.reshape(B, T, RET_HEADS, RET_DK).astype(jnp.float32), pos) * (RET_DK ** -0.5)
        vr = v_r.reshape(B, T, RET_HEADS, RET_DV).astype(jnp.float32)
        o_r = retention(qr, kr, vr)
        mu = jnp.mean(o_r, -1, keepdims=True)
        var = jnp.mean(jnp.square(o_r - mu), -1, keepdims=True)
        o_r = ((o_r - mu) * lax.rsqrt(var + GN_EPS)).reshape(B, T, RET_HEADS * RET_DV).astype(x.dtype)
        y_ret = (jax.nn.silu(g_r) * o_r) @ w_o_ret[i]
        kv = kv_n.reshape(B, T, 6, NSA_KV_GROUPS, NSA_HD)
        o_n = nsa(q_n.reshape(B, T, NSA_HEADS, NSA_HD),
                  kv[:, :, 0], kv[:, :, 1], kv[:, :, 2], kv[:, :, 3], kv[:, :, 4], kv[:, :, 5],
                  jax.nn.sigmoid(gate_n).reshape(B, T, NSA_HEADS, 3),
                  cmp_pos_k[i], cmp_pos_v[i], cmp_w1_k[i], cmp_w2_k[i], cmp_w1_v[i], cmp_w2_v[i],
                  rel_bias)
        y_nsa = o_n.astype(x.dtype) @ w_o_nsa[i]
        mixed = (jax.nn.sigmoid(m_a) * y_ret + jax.nn.sigmoid(m_b) * y_nsa) @ w_out[i]
        x = x + mixed.astype(x.dtype)
        h = rms_norm(x, norm_ffn_g[i])
        a, b = jnp.split(h @ w_ffn_in[i], 2, axis=-1)
        x = x + ((jax.nn.silu(a) * b) @ w_ffn_out[i]).astype(x.dtype)
    return rms_norm(x, norm_final_g)
```

```python
import math
import numpy as np
import ml_dtypes
import concourse.bass as bass
import concourse.mybir as mybir
from concourse.bass_utils import run_bass_kernel_spmd

F32 = mybir.dt.float32
BF16 = mybir.dt.bfloat16
AF = mybir.ActivationFunctionType
ALU = mybir.AluOpType
AX = mybir.AxisListType

T = 2048
D = 1024
NT = 16
DEPTH = 2
CIN = 10032
NSEQ = 4
NCORES = 8
FFN = 2816
NEGM = 30000.0
O_QR, O_KR, O_VR, O_GR, O_QN, O_KV, O_GATE, O_MA, O_MB = 0, 1024, 2048, 4096, 6144, 7168, 7936, 7984, 9008


def _t5_bucket_np(dist):
    dist = np.maximum(dist, 0)
    d32 = np.maximum(dist, 1).astype(np.float32)
    large = 16 + (np.log(d32 / np.float32(16)) / np.float32(math.log(128 / 16)) * np.float32(16)).astype(np.int32)
    large = np.minimum(large, 31)
    return np.where(dist < 16, dist, large)


def _consts():
    c = {}
    half = 128
    freqs = (10000.0 ** (-np.arange(half, dtype=np.float32) / half)).astype(np.float32)
    ang = (np.arange(T, dtype=np.float32)[None, :] * freqs[:, None]).astype(np.float32)
    c["cosT"] = np.cos(ang).astype(np.float32)
    c["sinT"] = np.sin(ang).astype(np.float32)
    c["ident"] = np.eye(128, dtype=np.float32).astype(ml_dtypes.bfloat16)
    lg = np.log(1.0 - 2.0 ** (-5.0 - np.arange(4, dtype=np.float64)))
    m = np.arange(128, dtype=np.float64)
    dm = np.zeros((128, 4, 128), np.float32)
    for h in range(4):
        dm[:, h, :] = (np.exp(-(m + 1.0) * lg[h])[:, None] * (m[None, :] >= m[:, None])).astype(np.float32)
    c["dmaskT"] = dm
    rs = np.zeros((128, 12), np.float32)
    for h in range(4):
        rs[:, h] = np.exp((m + 1.0) * lg[h])
        rs[:, 4 + h] = np.exp((127.0 - m) * lg[h])
    c["retsc"] = rs
    c["gchunk"] = [float(np.exp(128.0 * lg[h])) for h in range(4)]
    bk = _t5_bucket_np(np.arange(0, 4096))
    c["thr"] = [float(np.argmax(bk >= b)) for b in range(32)]
    r = np.arange(128, dtype=np.float32)[:, None]
    cc = np.arange(128, dtype=np.float32)[None, :]
    c["dist0"] = (cc - r).astype(np.float32)
    c["dist1"] = (cc - r + 128).astype(np.float32)
    cg = np.arange(247, dtype=np.float32)[None, :]
    c["distg"] = (r - 16.0 * (cg - 120.0) - 31.0).astype(np.float32)
    c["wedge"] = (cc < r).astype(np.float32).astype(ml_dtypes.bfloat16)
    ex = np.zeros((64, T), np.float32)
    for j in range(32):
        ex[j, 64 * j:64 * j + 64] = 1.0
    c["expand"] = ex.astype(ml_dtypes.bfloat16)
    cmp_end = np.arange(127) * 16 + 31
    sel_start = np.arange(32) * 64
    ov = ((cmp_end[:, None] - 31 < sel_start[None] + 64) & (cmp_end[:, None] >= sel_start[None]))
    c["overlap"] = ov.astype(np.float32).astype(ml_dtypes.bfloat16)
    keep = np.zeros((128, 16, 32), np.float32)
    add = np.zeros((128, 16, 32), np.float32)
    jj = np.arange(32)
    for t in range(16):
        pos = 128 * t + np.arange(128)
        cur = pos // 64
        forced = (jj[None] == 0) | (jj[None] == cur[:, None]) | (jj[None] == cur[:, None] - 1)
        valid = sel_start[None] <= pos[:, None]
        keep[:, t, :] = (valid & ~forced)
        add[:, t, :] = np.where(valid, np.where(forced, 1e4, 0.0), -1e30)
    c["keep"] = keep
    c["addm"] = add
    return c


NO_SAME_ENGINE_SYNC = False


class KB:
    def __init__(self):
        self.nc = bass.Bass("TRN2", target_bir_lowering=False)
        nc = self.nc
        self.ctx = []
        self.eng = {"pe": nc.tensor, "act": nc.scalar, "dve": nc.vector, "pool": nc.gpsimd, "sp": nc.sync}
        self.streams = {}
        for e in self.eng:
            self.new_stream(e, e, 1)
        self.NDQ = 16
        self.dq = {}
        self.dqi = {}
        for q in ("sp", "pool", "act"):
            self.dq[q] = []
            self.dqi[q] = 0
            for j in range(self.NDQ if q != "pool" else 6):
                nm = "d%s%d" % (q, j)
                self.new_stream(nm, q, 16)
                self.dq[q].append(nm)
        self.clock = {e: {} for e in self.eng}
        self.evclock = {}
        self.state = {}
        self.nwaits = 0
        self.ninst = 0
        self._uid = 0

    def enter(self, cm):
        v = cm.__enter__()
        self.ctx.append(cm)
        return v

    def mark(self):
        return len(self.ctx)

    def release(self, mark):
        if len(self.ctx) > mark and hasattr(self, "clock"):
            self.barrier()
        while len(self.ctx) > mark:
            self.ctx.pop().__exit__(None, None, None)

    def barrier(self):
        for en, e in self.eng.items():
            clk = self.clock[en]
            for sname, sd in self.streams.items():
                if sd["count"] > clk.get(sname, 0):
                    e.wait_ge(sd["sem"], sd["count"] * sd["inc"])
                    clk[sname] = sd["count"]
                    self.nwaits += 1

    def new_stream(self, name, eng, inc):
        sem = self.enter(self.nc.semaphore("sem_" + name))
        self.streams[name] = dict(sem=sem, count=0, inc=inc, eng=eng)

    def sb(self, name, shape, dtype):
        self._uid += 1
        return self.enter(self.nc.sbuf_tensor("%s_%d" % (name, self._uid), list(shape), dtype))

    def ps(self, name, shape, dtype):
        self._uid += 1
        return self.enter(self.nc.psum_tensor("%s_%d" % (name, self._uid), list(shape), dtype))

    def dram(self, name, shape, dtype, kind="Internal"):
        return self.nc.dram_tensor(name, list(shape), dtype, kind=kind).ap()

    def op(self, eng, fn, reads=(), writes=(), stream=None, nosame=False, extra=()):
        st = self.state
        need = {}
        for w in extra:
            if w[1] > 0 and need.get(w[0], 0) < w[1]:
                need[w[0]] = w[1]
        for k in reads:
            s = st.get(k)
            if s and s[0]:
                w = s[0]
                if need.get(w[0], 0) < w[1]:
                    need[w[0]] = w[1]
        for k in writes:
            s = st.get(k)
            if s:
                if s[0]:
                    w = s[0]
                    if need.get(w[0], 0) < w[1]:
                        need[w[0]] = w[1]
                for w in s[1]:
                    if need.get(w[0], 0) < w[1]:
                        need[w[0]] = w[1]
        clk = self.clock[eng]
        e = self.eng[eng]
        for sname, idx in need.items():
            if (nosame or NO_SAME_ENGINE_SYNC) and sname == eng:
                continue
            if clk.get(sname, 0) >= idx:
                continue
            sd = self.streams[sname]
            e.wait_ge(sd["sem"], idx * sd["inc"])
            self.nwaits += 1
            ev = self.evclock.get((sname, idx))
            clk[sname] = idx
            if ev:
                for k2, v2 in ev.items():
                    if clk.get(k2, 0) < v2:
                        clk[k2] = v2
        inst = fn()
        sname = stream or eng
        sd = self.streams[sname]
        sd["count"] += 1
        idx = sd["count"]
        inst.then_inc(sd["sem"], sd["inc"])
        self.ninst += 1
        snap = dict(clk)
        self.evclock[(sname, idx)] = snap
        evt = (sname, idx)
        for k in reads:
            s = st.get(k)
            if s is None:
                st[k] = [None, [evt]]
            else:
                s[1].append(evt)
        for k in writes:
            st[k] = [evt, []]
        return evt

    def prune(self):
        if len(self.evclock) > 400000:
            keep = {}
            for k, s in self.state.items():
                if s[0]:
                    keep[s[0]] = self.evclock.get(s[0])
                for w in s[1]:
                    keep[w] = self.evclock.get(w)
            self.evclock = {k: v for k, v in keep.items() if v is not None}

    def dma(self, out, in_, reads, writes, q="sp"):
        eng = q
        j = self.dqi[q]
        self.dqi[q] = (j + 1) % len(self.dq[q])
        stream = self.dq[q][j]
        e = self.eng[eng]
        prev = (stream, self.streams[stream]["count"])
        return self.op(eng, lambda: e.dma_start(out=out, in_=in_), reads, writes, stream=stream, extra=[prev])

    def mm(self, out, lhsT, rhs, start, stop, reads, writes, sgc=False):
        pe = self.nc.tensor
        if sgc:
            return self.op("pe", lambda: pe.matmul(out, lhsT, rhs, start=start, stop=stop, skip_group_check=True),
                           reads, writes, nosame=True)
        return self.op("pe", lambda: pe.matmul(out, lhsT, rhs, start=start, stop=stop), reads, writes, nosame=True)

    def tr(self, out, in_, ident, reads, writes):
        pe = self.nc.tensor
        return self.op("pe", lambda: pe.transpose(out, in_, ident), reads, writes, nosame=True)

    def act(self, out, in_, func, reads, writes, scale=1.0, bias=0.0, accum_out=None):
        a = self.nc.scalar
        if accum_out is not None:
            return self.op("act", lambda: a.activation(out=out, in_=in_, func=func, bias=bias, scale=scale,
                                                       accum_out=accum_out), reads, writes)
        return self.op("act", lambda: a.activation(out=out, in_=in_, func=func, bias=bias, scale=scale), reads, writes)

    def v(self, eng, fn, reads, writes):
        return self.op(eng, fn, reads, writes)


class _Stop(Exception):
    pass


class Rot:
    def __init__(self, kb, name, shape, dtype, n, psum=False):
        self.tiles = [(kb.ps if psum else kb.sb)(name, shape, dtype) for _ in range(n)]
        self.keys = ["%s#%d#%d" % (name, id(self) % 100000, i) for i in range(n)]
        self.i = 0

    def next(self):
        t, k = self.tiles[self.i], self.keys[self.i]
        self.i = (self.i + 1) % len(self.tiles)
        return t, k


def bcast_ap(ap, dims):
    pa = ap.ap
    return bass.AP(tensor=ap.tensor, offset=ap.offset, ap=[[pa[0][0], pa[0][1]]] + [list(d) for d in dims])


def build(nseq=NSEQ, depth=DEPTH, debug=False, stop_after=None):
    C = _consts()
    kb = KB()
    nc = kb.nc
    V = nc.vector
    G = nc.gpsimd
    dk = "ExternalOutput" if debug else "Internal"

    def dbg(name, ap, shape, dtype, keys):
        if not debug:
            return
        d = nc.dram_tensor("dbg_" + name, list(shape), dtype, kind="ExternalOutput").ap()
        kb.dma(d, ap, keys, [("dbg", name)])

    def din(name, shape, dtype=F32):
        return nc.dram_tensor(name, list(shape), dtype, kind="ExternalInput").ap()

    x_d = din("x", [nseq * T, D])
    y_d = nc.dram_tensor("y", [nseq * T, D], F32, kind="ExternalOutput").ap()
    w_f32 = {
        "w_in": din("w_in", [DEPTH * D, CIN]),
        "cmp_w1_k": din("cmp_w1_k", [DEPTH * 2048, 256]),
        "cmp_w2_k": din("cmp_w2_k", [DEPTH * 256, 64]),
        "cmp_w1_v": din("cmp_w1_v", [DEPTH * 2048, 256]),
        "cmp_w2_v": din("cmp_w2_v", [DEPTH * 256, 64]),
        "w_o_ret": din("w_o_ret", [DEPTH * 2048, D]),
        "w_o_nsa": din("w_o_nsa", [DEPTH * D, D]),
        "w_out": din("w_out", [DEPTH * D, D]),
        "w_ffn_in": din("w_ffn_in", [DEPTH * D, 2 * FFN]),
        "w_ffn_out": din("w_ffn_out", [DEPTH * FFN, D]),
    }
    norm_mix_g = din("norm_mix_g", [DEPTH, D])
    norm_ffn_g = din("norm_ffn_g", [DEPTH, D])
    norm_final_g = din("norm_final_g", [1, D])
    cmp_pos_k = din("cmp_pos_k", [DEPTH * 32, 64])
    cmp_pos_v = din("cmp_pos_v", [DEPTH * 32, 64])
    rel_bias = din("rel_bias", [1, 512])
    c_cos = din("c_cos", [128, T]); c_sin = din("c_sin", [128, T])
    c_ident = din("c_ident", [128, 128], BF16)
    c_dmask = din("c_dmask", [128, 512]); c_retsc = din("c_retsc", [128, 12])
    c_dist0 = din("c_dist0", [128, 128]); c_dist1 = din("c_dist1", [128, 128]); c_distg = din("c_distg", [128, 247])
    c_wedge = din("c_wedge", [128, 128], BF16)
    c_expand = din("c_expand", [64, T], BF16)
    c_overlap = din("c_overlap", [127, 32], BF16)
    c_keep = din("c_keep", [128, 512]); c_addm = din("c_addm", [128, 512])

    wb = {k: kb.dram("wb_" + k, v.shape, BF16) for k, v in w_f32.items()}
    P_qrT = kb.dram("P_qrT", [1024, T], BF16, dk)
    P_krT = kb.dram("P_krT", [1024, T], BF16, dk)
    P_kz = kb.dram("P_kz", [T, 1024], BF16, dk)
    P_v = kb.dram("P_v", [T, 2048], BF16, dk)
    P_sg = kb.dram("P_sg", [T, 2048], BF16, dk)
    P_qnT = kb.dram("P_qnT", [1024, T], BF16, dk)
    P_kcT = kb.dram("P_kcT", [128, T], BF16, dk)
    P_vcT = kb.dram("P_vcT", [128, T], BF16, dk)
    P_ksT = kb.dram("P_ksT", [256, T], BF16, dk)
    P_kwT = kb.dram("P_kwT", [256, T], BF16, dk)
    P_vs = kb.dram("P_vs", [T, 128], BF16, dk)
    P_vw = kb.dram("P_vw", [T, 128], BF16, dk)
    P_gate = kb.dram("P_gate", [T, 48], F32, dk)
    P_ma = kb.dram("P_ma", [T, 1024], BF16, dk)
    P_mb = kb.dram("P_mb", [T, 1024], BF16, dk)
    Z_T = kb.dram("Z_T", [2048, T], BF16, dk)
    ON_T = kb.dram("ON_T", [1024, T], BF16, dk)
    U_T = kb.dram("U_T", [FFN, T], BF16, dk)

    for k, src in w_f32.items():
        rows = src.shape[0]
        step = 256
        for r0 in range(0, rows, step):
            r1 = min(rows, r0 + step)
            kb.dma(wb[k][r0:r1, :], src[r0:r1, :], reads=[], writes=[("wb", k)], q="pool")

    x_sb = kb.sb("x", [128, NT, D], F32)
    ident = kb.sb("ident", [128, 128], BF16)
    kb.dma(ident[:], c_ident, [], ["ident"])
    PB = [kb.ps("pb", [128, 512], F32) for _ in range(6)]
    PBK = ["pb%d" % i for i in range(6)]
    PT2 = [kb.ps("pt", [128, 1024], BF16) for _ in range(2)]
    PTK = ["pt0", "pt1"]
    prr = [0]
    ptr = [0]

    def pbank():
        i = prr[0]; prr[0] = (i + 1) % 6
        return PB[i], PBK[i]

    def ptbank():
        i = ptr[0]; ptr[0] = (i + 1) % 2
        return PT2[i], PTK[i]

    Gt = kb.sb("Gt", [128, 16, 247], BF16)
    E0 = kb.sb("E0", [128, 16, 128], BF16)
    E1 = kb.sb("E1", [128, 16, 128], BF16)
    wedge = kb.sb("wedge", [128, 128], BF16)
    kb.dma(wedge[:], c_wedge, [], ["wedge"])

    def build_tables():
        mk = kb.mark()
        relb = kb.sb("relb", [128, 32, 16], F32)
        kb.dma(relb[:].rearrange("p b h -> p (b h)"),
               bass.AP(tensor=rel_bias.tensor, offset=0, ap=[[0, 128], [1, 512]]), [], ["relb"])
        dl = kb.sb("dl", [128, 32, 16], F32)
        kb.v("dve", lambda: V.tensor_sub(out=dl[:, 1:32, :], in0=relb[:, 1:32, :], in1=relb[:, 0:31, :]), ["relb"], ["dl"])
        kb.v("dve", lambda: V.tensor_sub(out=dl[:, 0:1, :], in0=relb[:, 0:1, :], in1=relb[:, 31:32, :]), ["relb"], ["dl"])
        W = 503
        dist = kb.sb("dist", [128, W], F32)
        kb.dma(dist[:, 0:128], c_dist0, [], ["dist"])
        kb.dma(dist[:, 128:256], c_dist1, [], ["dist"])
        kb.dma(dist[:, 256:503], c_distg, [], ["dist"])
        acc = kb.sb("acc", [128, 16, W], F32)
        tmps = Rot(kb, "tmp01", [128, W], F32, 3)
        ACCK = [("acc", h) for h in range(16)]
        kb.v("dve", lambda: V.tensor_copy(out=acc[:], in_=bcast_ap(dl[:, 0, :], [[1, 16], [0, W]])), ["dl"], ACCK)
        for b in range(1, 33):
            tmp, tk = tmps.next()
            if b < 32:
                thr = C["thr"][b]
                kb.v("dve", lambda: V.tensor_single_scalar(out=tmp[:], in_=dist[:], scalar=thr, op=ALU.is_ge), ["dist"], [tk])
                for h in range(16):
                    kb.v("dve", lambda: V.scalar_tensor_tensor(out=acc[:, h, :], in0=tmp[:], scalar=dl[:, b, h:h + 1], in1=acc[:, h, :],
                                                               op0=ALU.mult, op1=ALU.add), [tk, "dl", ("acc", h)], [("acc", h)])
            else:
                kb.v("dve", lambda: V.tensor_scalar(out=tmp[:], in0=dist[:], scalar1=0.0, scalar2=-NEGM, op0=ALU.is_lt, op1=ALU.mult),
                     ["dist"], [tk])
                kb.v("dve", lambda: V.tensor_add(out=acc[:], in0=acc[:], in1=bcast_ap(tmp[:], [[0, 16], [1, W]])), ACCK + [tk], ACCK)
        kb.act(E0[:], acc[:, :, 0:128], AF.Exp, ACCK, ["E0"])
        kb.act(E1[:], acc[:, :, 128:256], AF.Exp, ACCK, ["E1"])
        kb.v("dve", lambda: V.tensor_copy(out=Gt[:], in_=acc[:, :, 256:503]), ACCK, ["Gt"])
        kb.release(mk)

    build_tables()

    HT_ALL = [("hT", t) for t in range(NT)]
    dbg_once = [True]

    def rmsnorm_to_hT(hT, g_row_ap):
        mk = kb.mark()
        gbc = kb.sb("gbc", [128, D], F32)
        kb.dma(gbc[:], g_row_ap, [], ["gbc"])
        junk = Rot(kb, "junk", [128, D], BF16, 2)
        ssr = Rot(kb, "ss", [128, 4], F32, 3)
        hbr = Rot(kb, "hb", [128, D], BF16, 2)
        for t in range(NT):
            jt, jk = junk.next(); ss, sk = ssr.next(); hb, hk = hbr.next()
            kb.act(jt[:], x_sb[:, t, :], AF.Square, [("x", t)], [jk, sk + "a"], accum_out=ss[:, 0:1])
            kb.act(ss[:, 1:2], ss[:, 0:1], AF.Sqrt, [sk + "a"], [sk + "b"], scale=1.0 / D, bias=1e-6)
            kb.v("dve", lambda: V.reciprocal(out=ss[:, 2:3], in_=ss[:, 1:2]), [sk + "b"], [sk + "c"])
            kb.v("dve", lambda: V.scalar_tensor_tensor(out=hb[:], in0=x_sb[:, t, :], scalar=ss[:, 2:3], in1=gbc[:],
                                                       op0=ALU.mult, op1=ALU.mult), [("x", t), sk + "c", "gbc"], [hk])
            pt, pk = ptbank()
            for k in range(8):
                kb.tr(pt[:, 128 * k:128 * k + 128], hb[:, 128 * k:128 * k + 128], ident[:], [hk, "ident"], [pk])
            kb.act(hT[:, :, 128 * t:128 * t + 128], pt[:].rearrange("p (k n) -> p k n", k=8), AF.Copy, [pk], [("hT", t)])
        kb.release(mk)


    def load_w(pool, wname, row0, c0, ncols, dup64=None):
        wt, wk = pool.next()
        src = wb[wname]
        if dup64 is None:
            kb.dma(wt[:, :, 0:ncols], src[row0:row0 + 1024, c0:c0 + ncols].rearrange("(k p) c -> p k c", p=128),
                   [("wb", wname)], [wk])
        else:
            for hh in range(2):
                kb.dma(wt[:, :, 64 * hh:64 * hh + 64], src[row0:row0 + 1024, c0:c0 + 64].rearrange("(k p) c -> p k c", p=128),
                       [("wb", wname)], [wk])
        return wt, wk

    def gemm_fm(hT, wt, wk, jb, ncols=128, c0=0):
        pb, pk = pbank()
        for k in range(8):
            kb.mm(pb[0:ncols, :], wt[:, k, c0:c0 + ncols], hT[:, k, 512 * jb:512 * jb + 512], k == 0, k == 7,
                  [wk] + [("hT", 4 * jb + i) for i in range(4)], [pk])
        return pb, pk

    def gemm_tm(hT, wt, wk, t, ncols):
        pb, pk = pbank()
        for k in range(8):
            kb.mm(pb[:, 0:ncols], hT[:, k, 128 * t:128 * t + 128], wt[:, k, 0:ncols], k == 0, k == 7,
                  [wk, ("hT", t)], [pk])
        return pb, pk

    def stage_proj(li, hT):
        mk = kb.mark()
        row0 = li * D
        wpool = Rot(kb, "wt", [128, 8, 512], BF16, 3)
        cosT = kb.sb("cosT", [128, T], F32); sinT = kb.sb("sinT", [128, T], F32)
        kb.dma(cosT[:], c_cos, [], ["cosT"]); kb.dma(sinT[:], c_sin, [], ["sinT"])
        retsc = kb.sb("retsc", [128, 12], F32)
        kb.dma(retsc[:], c_retsc, [], ["retsc"])
        xab = Rot(kb, "xab", [128, 2, 512], F32, 2)
        tmpr = Rot(kb, "rtmp", [128, 4, 512], F32, 2)
        rotr = Rot(kb, "rot", [128, 2, 512], BF16, 2)
        kzr = Rot(kb, "kz", [128, 4, 256], BF16, 2)
        stg = Rot(kb, "stg", [128, 512], BF16, 4)
        stgf = Rot(kb, "stgf", [128, 48], F32, 2)
        flip = [0]

        for (isk, obase, dst) in ((0, O_QR, P_qrT), (1, O_KR, P_krT)):
            sc = 1.0 / 16.0 if isk else 1.0
            for h in range(4):
                wt, wk = load_w(wpool, "w_in", row0, obase + 256 * h, 256)
                for jb in range(4):
                    pa, pak = gemm_fm(hT, wt, wk, jb, 128, 0)
                    pbb, pbk = gemm_fm(hT, wt, wk, jb, 128, 128)
                    xa, xk = xab.next()
                    kb.act(xa[:, 0, :], pa[:], AF.Copy, [pak], [xk + "a"], scale=sc)
                    kb.act(xa[:, 1, :], pbb[:], AF.Copy, [pbk], [xk + "b"], scale=sc)
                    tm, tk = tmpr.next()
                    cs = cosT[:, 512 * jb:512 * jb + 512]; sn = sinT[:, 512 * jb:512 * jb + 512]
                    kb.v("dve", lambda: V.tensor_mul(out=tm[:, 0, :], in0=xa[:, 0, :], in1=cs), [xk + "a", "cosT"], [tk + "0"])
                    kb.v("pool", lambda: G.tensor_mul(out=tm[:, 1, :], in0=xa[:, 1, :], in1=sn), [xk + "b", "sinT"], [tk + "1"])
                    kb.v("dve", lambda: V.tensor_mul(out=tm[:, 2, :], in0=xa[:, 0, :], in1=sn), [xk + "a", "sinT"], [tk + "2"])
                    kb.v("pool", lambda: G.tensor_mul(out=tm[:, 3, :], in0=xa[:, 1, :], in1=cs), [xk + "b", "cosT"], [tk + "3"])
                    ro, rk = rotr.next()
                    kb.v("dve", lambda: V.tensor_sub(out=ro[:, 0, :], in0=tm[:, 0, :], in1=tm[:, 1, :]), [tk + "0", tk + "1"], [rk + "a"])
                    kb.v("pool", lambda: G.tensor_add(out=ro[:, 1, :], in0=tm[:, 2, :], in1=tm[:, 3, :]), [tk + "2", tk + "3"], [rk + "b"])
                    kb.dma(dst[256 * h:256 * h + 256, 512 * jb:512 * jb + 512].rearrange("(c p) n -> p c n", p=128), ro[:],
                           [rk + "a", rk + "b"], [("P_r", isk, h, jb)], q="pool")
                    if isk:
                        kz, kzk = kzr.next()
                        pt, pk = ptbank()
                        for i in range(4):
                            for c in range(2):
                                kb.tr(pt[:, 256 * i + 128 * c:256 * i + 128 * c + 128], ro[:, c, 128 * i:128 * i + 128], ident[:],
                                      [rk + "a", rk + "b", "ident"], [pk])
                        kb.act(kz[:].rearrange("p i d -> p (i d)"), pt[:], AF.Copy, [pk, "retsc"], [kzk], scale=retsc[:, 4 + h:5 + h])
                        kb.dma(P_kz[512 * jb:512 * jb + 512, 256 * h:256 * h + 256].rearrange("(i p) d -> p i d", p=128), kz[:],
                               [kzk], [("P_kz", h, jb)], q="pool")

        def tm_group(obase, ncols_total, dst, func, dkey, dstf32=False):
            for c0 in range(0, ncols_total, 512):
                ncol = min(512, ncols_total - c0)
                wt, wk = load_w(wpool, "w_in", row0, obase + c0, ncol)
                for t in range(NT):
                    pb, pk = gemm_tm(hT, wt, wk, t, ncol)
                    if dstf32:
                        sg, sk = stgf.next()
                    else:
                        sg, sk = stg.next()
                    if func == AF.Copy and (flip[0] % 2 == 0):
                        kb.v("dve", lambda: V.tensor_copy(out=sg[:, 0:ncol], in_=pb[:, 0:ncol]), [pk], [sk])
                    else:
                        kb.act(sg[:, 0:ncol], pb[:, 0:ncol], func, [pk], [sk])
                    flip[0] += 1
                    kb.dma(dst[128 * t:128 * t + 128, c0:c0 + ncol], sg[:, 0:ncol], [sk], [(dkey, c0 // 512, t)], q="pool")

        tm_group(O_VR, 2048, P_v, AF.Copy, "P_v")
        tm_group(O_GR, 2048, P_sg, AF.Silu, "P_sg")
        tm_group(O_KV + 3 * 128, 128, P_vs, AF.Copy, "P_vs")
        tm_group(O_KV + 5 * 128, 128, P_vw, AF.Copy, "P_vw")
        tm_group(O_GATE, 48, P_gate, AF.Sigmoid, "P_gate", dstf32=True)
        tm_group(O_MA, 1024, P_ma, AF.Sigmoid, "P_ma")
        tm_group(O_MB, 1024, P_mb, AF.Sigmoid, "P_mb")

        def fm_chunk(c0, dst_rows, scale, dkey, dup=False):
            wt, wk = load_w(wpool, "w_in", row0, c0, 128, dup64=(True if dup else None))
            for jb in range(4):
                pb, pk = gemm_fm(hT, wt, wk, jb, 128, 0)
                sg, sk = stg.next()
                kb.act(sg[:], pb[:], AF.Copy, [pk], [sk], scale=scale)
                kb.dma(dst_rows[:, 512 * jb:512 * jb + 512], sg[:], [sk], [(dkey, jb)], q="pool")

        for c in range(8):
            fm_chunk(O_QN + 128 * c, P_qnT[128 * c:128 * c + 128, :], 0.125, ("P_qnT", c))
        fm_chunk(O_KV + 0, P_kcT, 1.0, "P_kcT")
        fm_chunk(O_KV + 128, P_vcT, 1.0, "P_vcT")
        for g in range(2):
            fm_chunk(O_KV + 256 + 64 * g, P_ksT[128 * g:128 * g + 128, :], 1.0, ("P_ksT", g), dup=True)
            fm_chunk(O_KV + 512 + 64 * g, P_kwT[128 * g:128 * g + 128, :], 1.0, ("P_kwT", g), dup=True)
        kb.release(mk)

    PROJ_R_KEYS = [("P_r", isk, h, jb) for isk in range(2) for h in range(4) for jb in range(4)]

    def stage_retention(li):
        mk = kb.mark()
        dmask = kb.sb("dmask", [128, 4, 128], F32)
        kb.dma(dmask[:].rearrange("p h n -> p (h n)"), c_dmask, [], ["dmask"])
        retsc = kb.sb("retsc", [128, 12], F32)
        kb.dma(retsc[:], c_retsc, [], ["retsc"])
        R32s = [kb.sb("R32", [128, 2, 512], F32) for _ in range(4)]
        Rbs = [kb.sb("Rb", [128, 2, 512], BF16) for _ in range(4)]
        qTr = Rot(kb, "qT", [128, 2, 128], BF16, 8)
        kTr = Rot(kb, "kT", [128, 2, 128], BF16, 8)
        kzr = Rot(kb, "kzl", [128, 256], BF16, 8)
        vr = Rot(kb, "vl", [128, 512], BF16, 8)
        sgr = Rot(kb, "sgl", [128, 512], BF16, 8)
        sTr = Rot(kb, "sTb", [128, 128], BF16, 4)
        osr = Rot(kb, "osb", [128, 512], F32, 4)
        str_ = Rot(kb, "stat", [128, 16], F32, 6)
        zr = Rot(kb, "z", [128, 512], BF16, 4)
        z2r = Rot(kb, "z2", [128, 512], F32, 4)
        zTr = Rot(kb, "zT", [128, 4, 128], BF16, 4)
        for h in range(4):
            kb.v("pool", lambda: G.memset(R32s[h][:], 0.0), [], ["R32a%d" % h, "R32b%d" % h])
            kb.v("pool", lambda: G.memset(Rbs[h][:], 0.0), [], ["Rba%d" % h, "Rbb%d" % h])
        units = [(c, h) for c in range(NT) for h in range(4)]

        def r_load(c, h):
            jb = c // 4
            qT, qk = qTr.next(); kT, kk = kTr.next(); kz, kzk = kzr.next(); vv, vk = vr.next(); sg, sgk = sgr.next()
            cols = slice(128 * c, 128 * c + 128)
            kb.dma(qT[:], P_qrT[256 * h:256 * h + 256, cols].rearrange("(c p) n -> p c n", p=128), [("P_r", 0, h, jb)], [qk])
            kb.dma(kT[:], P_krT[256 * h:256 * h + 256, cols].rearrange("(c p) n -> p c n", p=128), [("P_r", 1, h, jb)], [kk])
            kb.dma(kz[:], P_kz[cols, 256 * h:256 * h + 256], [("P_kz", h, jb)], [kzk])
            kb.dma(vv[:], P_v[cols, 512 * h:512 * h + 512], [("P_v", h, c)], [vk])
            kb.dma(sg[:], P_sg[cols, 512 * h:512 * h + 512], [("P_sg", h, c)], [sgk])
            return dict(qT=qT, qk=qk, kT=kT, kk=kk, kz=kz, kzk=kzk, vv=vv, vk=vk, sg=sg, sgk=sgk)

        def r_p1(c, h, L):
            Rb = Rbs[h]
            ps_, psk = pbank()
            for cc in range(2):
                kb.mm(ps_[:, 0:128], L["kT"][:, cc, :], L["qT"][:, cc, :], cc == 0, cc == 1, [L["kk"], L["qk"]], [psk])
            sT, sTk = sTr.next()
            kb.v("dve", lambda: V.tensor_mul(out=sT[:], in0=ps_[:, 0:128], in1=dmask[:, h, :]), [psk, "dmask"], [sTk])
            po, pok = pbank()
            kb.mm(po[:], sT[:], L["vv"][:], True, False, [sTk, L["vk"]], [pok])
            for cc in range(2):
                kb.mm(po[:], L["qT"][:, cc, :], Rb[:, cc, :], False, cc == 1, [L["qk"], "Rb" + "ab"[cc] + str(h)], [pok])
            osb, osk = osr.next()
            kb.act(osb[:], po[:], AF.Copy, [pok, "retsc"], [osk], scale=retsc[:, h:h + 1])
            st, stk = str_.next()
            kb.v("dve", lambda: V.bn_stats(out=st[:, 0:6], in_=osb[:]), [osk], [stk + "a"])
            kb.v("dve", lambda: V.bn_aggr(out=st[:, 6:8], in_=st[:, 0:6]), [stk + "a"], [stk + "b"])
            kb.act(st[:, 8:9], st[:, 7:8], AF.Sqrt, [stk + "b"], [stk + "c"], bias=1e-5)
            L.update(osb=osb, osk=osk, st=st, stk=stk)

        def r_p2(c, h, L):
            gch = C["gchunk"][h]
            R32 = R32s[h]; Rb = Rbs[h]
            st, stk, osb, osk = L["st"], L["stk"], L["osb"], L["osk"]
            cols = slice(128 * c, 128 * c + 128)
            if c < NT - 1:
                for cc in range(2):
                    pr, prk = pbank()
                    kb.mm(pr[:], L["kz"][:, 128 * cc:128 * cc + 128], L["vv"][:], True, True, [L["kzk"], L["vk"]], [prk])
                    kb.v("dve", lambda: V.scalar_tensor_tensor(out=R32[:, cc, :], in0=R32[:, cc, :], scalar=gch, in1=pr[:],
                                                               op0=ALU.mult, op1=ALU.add), ["R32" + "ab"[cc] + str(h), prk], ["R32" + "ab"[cc] + str(h)])
                    kb.act(Rb[:, cc, :], R32[:, cc, :], AF.Copy, ["R32" + "ab"[cc] + str(h)], ["Rb" + "ab"[cc] + str(h)])
            kb.v("dve", lambda: V.reciprocal(out=st[:, 9:10], in_=st[:, 8:9]), [stk + "c"], [stk + "d"])
            z2, z2k = z2r.next()
            kb.v("dve", lambda: V.tensor_scalar(out=z2[:], in0=osb[:], scalar1=st[:, 6:7], scalar2=st[:, 9:10],
                                                op0=ALU.subtract, op1=ALU.mult), [osk, stk + "b", stk + "d"], [z2k])
            z, zk = zr.next()
            kb.v("dve", lambda: V.tensor_mul(out=z[:], in0=z2[:], in1=L["sg"][:]), [z2k, L["sgk"]], [zk])
            L.update(z=z, zk=zk)

        def r_p3(c, h, L):
            z, zk = L["z"], L["zk"]
            cols = slice(128 * c, 128 * c + 128)
            pt, pk = ptbank()
            for e in range(4):
                kb.tr(pt[:, 128 * e:128 * e + 128], z[:, 128 * e:128 * e + 128], ident[:], [zk, "ident"], [pk])
            zT, zTk = zTr.next()
            kb.act(zT[:].rearrange("p e n -> p (e n)"), pt[:, 0:512], AF.Copy, [pk], [zTk])
            kb.dma(Z_T[512 * h:512 * h + 512, cols].rearrange("(e p) n -> p e n", p=128), zT[:], [zTk], [("Z_T", c, h)], q="pool")

        NU = len(units)
        PRE = 4
        Ls = {}
        for n in range(min(PRE, NU)):
            Ls[n] = r_load(*units[n])
        for n in range(NU + 2):
            if n + PRE < NU:
                Ls[n + PRE] = r_load(*units[n + PRE])
            if n < NU:
                r_p1(*units[n], Ls[n])
            if 1 <= n <= NU:
                r_p2(*units[n - 1], Ls[n - 1])
            if n >= 2:
                r_p3(*units[n - 2], Ls.pop(n - 2))
        kb.release(mk)

    def stage_nsa(li):
        mk = kb.mark()
        keep = kb.sb("keep", [128, 16, 32], F32); addm = kb.sb("addm", [128, 16, 32], F32)
        kb.dma(keep[:].rearrange("p t j -> p (t j)"), c_keep, [], ["keep"])
        kb.dma(addm[:].rearrange("p t j -> p (t j)"), c_addm, [], ["addm"])
        gates = kb.sb("gates", [128, 16, 48], F32)
        kb.dma(gates[:], P_gate.rearrange("(t p) c -> p t c", p=128), [("P_gate", 0, t) for t in range(NT)], ["gates"])
        if stop_after == "nsa0":
            raise _Stop()
        kcx = kb.sb("kcx", [128, 2, 2, 128], BF16)
        kb.v("pool", lambda: G.memset(kcx[:], 0.0), [], ["kcx0", "kcx1"])
        vca = kb.sb("vcaug", [128, 2, 97], BF16)
        kb.v("pool", lambda: G.memset(vca[:], 1.0), [], ["vca0", "vca1"])
        for g in range(2):
            kb.dma(vca[0:127, g, 64:96], c_overlap, [], ["vca%d" % g])
        mk2 = kb.mark()
        w1 = Rot(kb, "w1", [128, 32, 256], BF16, 2)
        for kv in range(2):
            nm1 = "cmp_w1_v" if kv else "cmp_w1_k"
            nm2 = "cmp_w2_v" if kv else "cmp_w2_k"
            pos_d = cmp_pos_v if kv else cmp_pos_k
            w1t, w1k = w1.next()
            for hh in range(2):
                kb.dma(w1t[64 * hh:64 * hh + 64, :, :], wb[nm1][2048 * li:2048 * li + 2048, :].rearrange("(l d) h -> d l h", d=64),
                       [("wb", nm1)], [w1k])
            w2t = kb.sb("w2t", [128, 2, 128], BF16)
            for hh in range(2):
                kb.dma(w2t[:, :, 64 * hh:64 * hh + 64], wb[nm2][256 * li:256 * li + 256, :].rearrange("(c p) d -> p c d", p=128),
                       [("wb", nm2)], ["w2t"])
            posl = kb.sb("posl", [32, 64], F32)
            kb.dma(posl[:], pos_d[32 * li:32 * li + 32, :], [], ["posl"])
            posb = kb.sb("posb", [32, 64], BF16)
            kb.v("dve", lambda: V.tensor_copy(out=posb[:], in_=posl[:]), ["posl"], ["posb"])
            pt, pk = ptbank()
            kb.tr(pt[0:64, 0:32], posb[:], ident[0:32, 0:32], ["posb", "ident"], [pk])
            posT = kb.sb("posT", [128, 32], F32)
            kb.act(posT[0:64, :], pt[0:64, 0:32], AF.Copy, [pk], ["posT"])
            kb.act(posT[64:128, :], pt[0:64, 0:32], AF.Copy, [pk], ["posT"])
            kvT = kb.sb("kvT", [128, T], BF16)
            src = P_vcT if kv else P_kcT
            kb.dma(kvT[:], src, [("P_vcT" if kv else "P_kcT", jb) for jb in range(4)], ["kvT"])
            kvA = kb.sb("kvA", [128, T], BF16); kvB = kb.sb("kvB", [128, T], BF16)
            kb.v("dve", lambda: V.tensor_add(out=kvA[:].rearrange("p (a b) -> p a b", b=16), in0=kvT[:].rearrange("p (a b) -> p a b", b=16),
                                             in1=bcast_ap(posT[:, 0:16], [[0, 128], [1, 16]])), ["kvT", "posT"], ["kvA"])
            kb.v("dve", lambda: V.tensor_add(out=kvB[:].rearrange("p (a b) -> p a b", b=16), in0=kvT[:].rearrange("p (a b) -> p a b", b=16),
                                             in1=bcast_ap(posT[:, 16:32], [[0, 128], [1, 16]])), ["kvT", "posT"], ["kvB"])
            for g in range(2):
                pr = slice(64 * g, 64 * g + 64)
                gT = kb.sb("gT", [128, 2, 128], BF16)
                for ch in range(2):
                    pb, pk_ = pbank()
                    for l in range(32):
                        srcT = kvA if l < 16 else kvB
                        rhs = bcast_ap(srcT[pr, l:l + 1], [[16, 127]])
                        kb.mm(pb[:, 0:127], w1t[pr, l, 128 * ch:128 * ch + 128], rhs, l == 0, l == 31,
                              [w1k, "kvA", "kvB"], [pk_])
                    hs = kb.sb("hs", [128, 4, 127], F32)
                    kb.act(hs[:, 0, :], pb[:, 0:127], AF.Copy, [pk_], ["hs0"])
                    kb.v("dve", lambda: V.tensor_mul(out=hs[:, 1, :], in0=hs[:, 0, :], in1=hs[:, 0, :]), ["hs0"], ["hs1"])
                    kb.v("dve", lambda: V.tensor_scalar(out=hs[:, 2, :], in0=hs[:, 1, :], scalar1=0.044715, scalar2=1.0,
                                                        op0=ALU.mult, op1=ALU.add), ["hs1"], ["hs2"])
                    kb.v("dve", lambda: V.tensor_mul(out=hs[:, 3, :], in0=hs[:, 2, :], in1=hs[:, 0, :]), ["hs2", "hs0"], ["hs3"])
                    kb.act(hs[:, 1, :], hs[:, 3, :], AF.Sigmoid, ["hs3", "hs2"], ["hs1"], scale=2.0 * math.sqrt(2.0 / math.pi))
                    kb.v("dve", lambda: V.tensor_mul(out=gT[:, ch, 0:127], in0=hs[:, 1, :], in1=hs[:, 0, :]), ["hs1", "hs0"], ["gT%d" % ch])
                pb, pk_ = pbank()
                if kv == 0:
                    for ch in range(2):
                        kb.mm(pb[:, 0:127], w2t[:, ch, :], gT[:, ch, 0:127], ch == 0, ch == 1, ["w2t", "gT%d" % ch], [pk_])
                    for par in range(2):
                        kb.act(kcx[64 * par:64 * par + 64, par, g, 0:127], pb[64 * par:64 * par + 64, 0:127], AF.Copy, [pk_], ["kcx%d" % g])
                else:
                    for ch in range(2):
                        kb.mm(pb[0:127, 0:64], gT[:, ch, 0:127], w2t[:, ch, 0:64], ch == 0, ch == 1, ["w2t", "gT%d" % ch], [pk_])
                    kb.act(vca[0:127, g, 0:64], pb[0:127, 0:64], AF.Copy, [pk_], ["vca%d" % g])
        kb.release(mk2)
        if stop_after == "nsa1":
            raise _Stop()

        PTr = Rot(kb, "PT", [128, 512], BF16, 4)
        for g in range(2):
            mk3 = kb.mark()
            ksx = kb.sb("ksx", [128, 2, T], BF16); kwx = kb.sb("kwx", [128, 2, T], BF16)
            kb.v("pool", lambda: G.memset(kwx[:], 0.0), [], ["kwx"])
            for par in range(2):
                own = slice(64 * par, 64 * par + 64)
                oth = slice(64 * (1 - par), 64 * (1 - par) + 64)
                kb.dma(ksx[own, par, :], P_ksT[128 * g + 64 * par:128 * g + 64 * par + 64, :], [(("P_ksT", g), jb) for jb in range(4)], ["ksx"])
                kb.dma(ksx[oth, par, :], c_expand, [], ["ksx"])
                kb.dma(kwx[own, par, :], P_kwT[128 * g + 64 * par:128 * g + 64 * par + 64, :], [(("P_kwT", g), jb) for jb in range(4)], ["kwx"])
            vsa = kb.sb("vsa", [128, 16, 65], BF16); vwa = kb.sb("vwa", [128, 16, 65], BF16)
            kb.v("pool", lambda: G.memset(vsa[:], 1.0), [], ["vsa"])
            kb.v("pool", lambda: G.memset(vwa[:], 1.0), [], ["vwa"])
            kb.dma(vsa[:, :, 0:64], P_vs[:, 64 * g:64 * g + 64].rearrange("(t p) d -> p t d", p=128),
                   [("P_vs", 0, t) for t in range(NT)], ["vsa"])
            kb.dma(vwa[:, :, 0:64], P_vw[:, 64 * g:64 * g + 64].rearrange("(t p) d -> p t d", p=128),
                   [("P_vw", 0, t) for t in range(NT)], ["vwa"])
            qs = kb.sb("qs", [128, 8, T], BF16)
            kb.v("pool", lambda: G.memset(qs[:, 0:4, :], 0.0), [], [("qsz", 0)])
            kb.v("pool", lambda: G.memset(qs[:, 4:8, :], 0.0), [], [("qsz", 1)])
            for hh in range(8):
                par = hh % 2
                own = slice(64 * par, 64 * par + 64)
                kb.dma(qs[own, hh, :], P_qnT[128 * (4 * g + hh // 2) + 64 * par:128 * (4 * g + hh // 2) + 64 * par + 64, :],
                       [(("P_qnT", 4 * g + hh // 2), jb) for jb in range(4)] + [("qsz", hh // 4)], [("qsq", hh)])
            og = kb.sb("og", [128, 16, 512], F32)
            ocr = Rot(kb, "ocimp", [128, 8, 96], F32, 2)
            zcr = Rot(kb, "zc", [128, 127], F32, 3)
            pcr = Rot(kb, "pc", [128, 128], BF16, 4)
            pTr = Rot(kb, "pTc", [128, 128], BF16, 4)
            smr = Rot(kb, "sm", [128, 12], F32, 4)
            impr = Rot(kb, "imp", [128, 4, 32], F32, 2)
            ngr = Rot(kb, "ngm", [128, 64], BF16, 2)
            for i in range(2):
                kb.v("pool", lambda: G.memset(ngr.tiles[i][:], 0.0), [], [ngr.keys[i]])
            citems = [(t, hh) for t in range(NT) for hh in range(8)]
            octile = {}

            def cA(t, hh):
                pr = slice(64 * (hh % 2), 64 * (hh % 2) + 64)
                h = 8 * g + hh
                pb, pk = pbank()
                kb.mm(pb[:, 0:127], qs[:, hh, 128 * t:128 * t + 128], kcx[:, hh % 2, g, 0:127], True, True,
                      [("qsq", hh), ("qsz", hh // 4), ("qsm", hh % 2, t), "kcx%d" % g], [pk])
                zc, zk = zcr.next()
                kb.v("dve", lambda: V.tensor_add(out=zc[:], in0=pb[:, 0:127], in1=Gt[:, h, 120 - 8 * t:120 - 8 * t + 127]), [pk, "Gt"], [zk])
                pc, pck = pcr.next()
                kb.act(pc[:, 0:127], zc[:], AF.Exp, [zk], [pck])
                return pc, pck

            def cB(t, hh, pc, pck):
                pt, ptk = ptbank()
                kb.tr(pt[0:127, 0:128], pc[:, 0:127], ident[:], [pck, "ident"], [ptk])
                pT, pTk = pTr.next()
                kb.act(pT[0:127, :], pt[0:127, 0:128], AF.Copy, [ptk], [pTk])
                return pT, pTk

            def cC(t, hh, pT, pTk):
                h = 8 * g + hh
                if hh == 0:
                    octile[t] = ocr.next()
                oc, ock = octile[t]
                po, pok = pbank()
                kb.mm(po[:, 0:97], pT[0:127, :], vca[0:127, g, :], True, True, [pTk, "vca%d" % g], [pok])
                sm, smk = smr.next()
                kb.v("dve", lambda: V.tensor_scalar_max(out=sm[:, 0:1], in0=po[:, 96:97], scalar1=1e-30), [pok], [smk + "a"])
                kb.v("dve", lambda: V.reciprocal(out=sm[:, 1:2], in_=sm[:, 0:1]), [smk + "a"], [smk + "b"])
                kb.v("dve", lambda: V.tensor_scalar_mul(out=oc[:, hh, :], in0=po[:, 0:96], scalar1=sm[:, 1:2]), [pok, smk + "b"], [(ock, hh)])
                kb.act(og[:, t, 64 * hh:64 * hh + 64], oc[:, hh, 0:64], AF.Copy, [(ock, hh), "gates"], [("og", t, hh)],
                       scale=gates[:, t, 3 * h:3 * h + 1])
                if hh == 7:
                    im, imk = impr.next()
                    kb.v("dve", lambda: V.tensor_reduce(out=im[:, 0, :], in_=oc[:, :, 64:96].rearrange("p h j -> p j h"), axis=AX.X, op=ALU.add),
                         [(ock, x_) for x_ in range(8)], [imk + "0"])
                    kb.v("dve", lambda: V.tensor_mul(out=im[:, 1, :], in0=im[:, 0, :], in1=keep[:, t, :]), [imk + "0", "keep"], [imk + "1"])
                    kb.v("dve", lambda: V.tensor_add(out=im[:, 2, :], in0=im[:, 1, :], in1=addm[:, t, :]), [imk + "1", "addm"], [imk + "2"])
                    sm2, smk2 = smr.next()
                    kb.v("dve", lambda: V.max(out=sm2[:, 0:8], in_=im[:, 2, :]), [imk + "2"], [smk2 + "a"])
                    kb.v("dve", lambda: V.tensor_scalar(out=im[:, 3, :], in0=im[:, 2, :], scalar1=sm2[:, 7:8], scalar2=None, op0=ALU.is_ge),
                         [imk + "2", smk2 + "a"], [imk + "3"])
                    ng, ngk = ngr.next()
                    kb.v("dve", lambda: V.tensor_scalar(out=ng[:, 0:32], in0=im[:, 3, :], scalar1=-1.0, scalar2=NEGM, op0=ALU.add, op1=ALU.mult),
                         [imk + "3"], [ngk])
                    pt, ptk = ptbank()
                    kb.tr(pt[0:64, 0:128], ng[:], ident[:], [ngk, "ident"], [ptk])
                    for par in range(2):
                        oth = slice(64 * (1 - par), 64 * (1 - par) + 64)
                        kb.act(bcast_ap(qs[oth, par, 128 * t:128 * t + 128], [[2 * T, 4], [1, 128]]),
                               bcast_ap(pt[0:64, 0:128], [[0, 4], [1, 128]]), AF.Copy,
                               [ptk, ("qsz", 0), ("qsz", 1)], [("qsm", par, t)])

            nci = len(citems)
            hA = {}
            hB = {}
            for n in range(nci + 2):
                if n < nci:
                    hA[n] = cA(*citems[n])
                if 1 <= n <= nci:
                    hB[n - 1] = cB(*citems[n - 1], *hA.pop(n - 1))
                if 2 <= n:
                    cC(*citems[n - 2], *hB.pop(n - 2))
            if stop_after == "nsa2":
                raise _Stop()
            LOOK = 2
            STB, STK = PB[0:4], PBK[0:4]
            ACB, ACK = PB[4:6], PBK[4:6]
            tmpr = Rot(kb, "fin", [128, 4, 64], F32, 2)
            units = [(hh, qb, br) for hh in range(8) for qb in range(4) for br in range(2)]
            items = []
            for ui, (hh, qb, br) in enumerate(units):
                kt_lo = 0 if br == 0 else max(0, 4 * qb - 4)
                for kt in range(kt_lo, 4 * qb + 4):
                    items.append((ui, hh, qb, br, kt, kt == kt_lo, kt == 4 * qb + 3))

            def sQK(n, item):
                ui, hh, qb, br, kt, isfirst, islast = item
                h = 8 * g + hh
                pr = slice(64 * (hh % 2), 64 * (hh % 2) + 64)
                c = hh // 2
                kx_, kxk = (ksx, "ksx") if br == 0 else (kwx, "kwx")
                st_, stk_ = STB[n % 4], STK[n % 4]
                par = hh % 2
                vi = [i for i in range(4) if 0 <= 4 * qb + i - kt and not (br == 1 and 4 * qb + i - kt > 4)]
                c0_, c1_ = 128 * vi[0], 128 * vi[-1] + 128
                kb.mm(st_[:, c0_:c1_], kx_[:, par, 128 * kt:128 * kt + 128], qs[:, hh, 512 * qb + c0_:512 * qb + c1_], True, True,
                      [kxk, ("qsq", hh), ("qsz", hh // 4)] + [("qsm", par, 4 * qb + i) for i in vi], [stk_])
                PTt, PTk = PTr.next()
                kb.act(PTt[:, c0_:c1_], st_[:, c0_:c1_], AF.Exp, [stk_], [(PTk, i) for i in range(4)])
                for i in range(4):
                    off = 4 * qb + i - kt
                    if off < 0 or (br == 1 and off > 4):
                        continue
                    sl = slice(128 * i, 128 * i + 128)
                    if off == 0:
                        kb.v("dve", lambda: V.tensor_mul(out=PTt[:, sl], in0=PTt[:, sl], in1=E0[:, h, :]), [(PTk, i), "E0"], [(PTk, i)])
                    elif off == 1:
                        kb.v("pool", lambda: G.tensor_mul(out=PTt[:, sl], in0=PTt[:, sl], in1=E1[:, h, :]), [(PTk, i), "E1"], [(PTk, i)])
                    elif off == 4 and br == 1:
                        kb.v("pool", lambda: G.tensor_mul(out=PTt[:, sl], in0=PTt[:, sl], in1=wedge[:]), [(PTk, i), "wedge"], [(PTk, i)])
                return PTt, PTk

            def sPV(item, PTt, PTk):
                ui, hh, qb, br, kt, isfirst, islast = item
                h = 8 * g + hh
                va, vak = (vsa, "vsa") if br == 0 else (vwa, "vwa")
                accb, acck = ACB[ui % 2], ACK[ui % 2]
                A = accb[:].rearrange("p (i c) -> p i c", c=128)
                firstmm = isfirst
                for i in range(4):
                    off = 4 * qb + i - kt
                    if off < 0 or (br == 1 and off > 4):
                        continue
                    sl = slice(128 * i, 128 * i + 128)
                    kb.mm(A[:, i, 0:65], PTt[:, sl], va[:, kt, :], firstmm, kt == 4 * qb + i, [(PTk, i), vak], [acck], sgc=True)
                    firstmm = False
                if islast:
                    sm, smk = smr.next()
                    kb.v("dve", lambda: V.tensor_scalar_max(out=sm[:, 0:4], in0=A[:, :, 64], scalar1=1e-30), [acck], [smk + "a"])
                    kb.v("dve", lambda: V.reciprocal(out=sm[:, 4:8], in_=sm[:, 0:4]), [smk + "a"], [smk + "b"])
                    kb.v("dve", lambda: V.tensor_mul(out=sm[:, 8:12], in0=sm[:, 4:8], in1=gates[:, 4 * qb:4 * qb + 4, 3 * h + 1 + br]),
                         [smk + "b", "gates"], [smk + "c"])
                    tm, tmk = tmpr.next()
                    kb.v("dve", lambda: V.tensor_tensor(out=tm[:], in0=A[:, :, 0:64], in1=bcast_ap(sm[:, 8:12], [[1, 4], [0, 64]]), op=ALU.mult),
                         [acck, smk + "c"], [tmk])
                    ogk = [("og", 4 * qb + i, hh) for i in range(4)]
                    kb.v("pool", lambda: G.tensor_add(out=og[:, 4 * qb:4 * qb + 4, 64 * hh:64 * hh + 64],
                                                      in0=og[:, 4 * qb:4 * qb + 4, 64 * hh:64 * hh + 64], in1=tm[:]), ogk + [tmk], ogk)

            nit = len(items)
            hq = {}
            for n in range(nit + LOOK):
                if n < nit:
                    hq[n] = sQK(n, items[n])
                if n >= LOOK:
                    sPV(items[n - LOOK], *hq.pop(n - LOOK))
            obr = Rot(kb, "ob", [128, 512], BF16, 2)
            oTr = Rot(kb, "oT", [128, 4, 128], BF16, 2)
            for t in range(NT):
                ob, obk = obr.next()
                kb.act(ob[:], og[:, t, :], AF.Copy, [("og", t, hh) for hh in range(8)], [obk])
                pt, ptk = ptbank()
                for e in range(4):
                    kb.tr(pt[:, 128 * e:128 * e + 128], ob[:, 128 * e:128 * e + 128], ident[:], [obk, "ident"], [ptk])
                oT, oTk = oTr.next()
                kb.act(oT[:].rearrange("p e n -> p (e n)"), pt[:, 0:512], AF.Copy, [ptk], [oTk])
                kb.dma(ON_T[512 * g:512 * g + 512, 128 * t:128 * t + 128].rearrange("(e p) n -> p e n", p=128), oT[:], [oTk],
                       [("ON_T", g, t)], q="pool")
            kb.release(mk3)
        kb.release(mk)

    prr2 = [0]

    def stage_merge(li):
        mk = kb.mark()
        mT = kb.sb("mT", [128, 8, T], BF16)
        mk2 = kb.mark()
        wor = kb.sb("wor", [128, 16, D], BF16)
        won = kb.sb("won", [128, 8, D], BF16)
        for k4 in range(4):
            kb.dma(wor[:, 4 * k4:4 * k4 + 4, :], wb["w_o_ret"][2048 * li + 512 * k4:2048 * li + 512 * k4 + 512, :].rearrange("(k p) c -> p k c", p=128),
                   [("wb", "w_o_ret")], ["wor"])
        for k4 in range(2):
            kb.dma(won[:, 4 * k4:4 * k4 + 4, :], wb["w_o_nsa"][D * li + 512 * k4:D * li + 512 * k4 + 512, :].rearrange("(k p) c -> p k c", p=128),
                   [("wb", "w_o_nsa")], ["won"])
        zTr = Rot(kb, "zTl", [128, 16, 128], BF16, 2)
        oTr = Rot(kb, "oTl", [128, 8, 128], BF16, 2)
        sar = Rot(kb, "sa", [128, D], BF16, 2)
        sbr = Rot(kb, "sb", [128, D], BF16, 2)
        t1r = Rot(kb, "t1", [128, 512], F32, 2)
        t2r = Rot(kb, "t2", [128, 512], F32, 2)
        mbr = Rot(kb, "mb", [128, D], BF16, 2)
        for t in range(NT):
            cols = slice(128 * t, 128 * t + 128)
            zT, zk = zTr.next(); oT, ok = oTr.next(); sa, sak = sar.next(); sb_, sbk = sbr.next()
            kb.dma(zT[:], Z_T[:, cols].rearrange("(k p) n -> p k n", p=128), [("Z_T", t, h_) for h_ in range(4)], [zk])
            kb.dma(oT[:], ON_T[:, cols].rearrange("(k p) n -> p k n", p=128), [("ON_T", 0, t), ("ON_T", 1, t)], [ok])
            kb.dma(sa[:], P_ma[cols, :], [("P_ma", 0, t), ("P_ma", 1, t)], [sak])
            kb.dma(sb_[:], P_mb[cols, :], [("P_mb", 0, t), ("P_mb", 1, t)], [sbk])
            mb, mbk = mbr.next()
            for half in range(2):
                hs = slice(512 * half, 512 * half + 512)
                pr_, prk = pbank()
                for k in range(16):
                    kb.mm(pr_[:], zT[:, k, :], wor[:, k, hs], k == 0, k == 15, [zk, "wor"], [prk])
                pn, pnk = pbank()
                for k in range(8):
                    kb.mm(pn[:], oT[:, k, :], won[:, k, hs], k == 0, k == 7, [ok, "won"], [pnk])
                t1, t1k = t1r.next(); t2, t2k = t2r.next()
                kb.v("dve", lambda: V.tensor_mul(out=t1[:], in0=pr_[:], in1=sa[:, hs]), [prk, sak], [t1k])
                kb.v("dve", lambda: V.tensor_mul(out=t2[:], in0=pn[:], in1=sb_[:, hs]), [pnk, sbk], [t2k])
                kb.v("pool", lambda: G.tensor_add(out=mb[:, hs], in0=t1[:], in1=t2[:]), [t1k, t2k], [(mbk, half)])
            pt, ptk = ptbank()
            for k in range(8):
                kb.tr(pt[:, 128 * k:128 * k + 128], mb[:, 128 * k:128 * k + 128], ident[:], [(mbk, 0), (mbk, 1), "ident"], [ptk])
            kb.act(mT[:, :, cols], pt[:].rearrange("p (k n) -> p k n", k=8), AF.Copy, [ptk], [("mT", t)])
        kb.release(mk2)
        wo = kb.sb("wo", [128, 8, D], BF16)
        for k4 in range(2):
            kb.dma(wo[:, 4 * k4:4 * k4 + 4, :], wb["w_out"][D * li + 512 * k4:D * li + 512 * k4 + 512, :].rearrange("(k p) c -> p k c", p=128),
                   [("wb", "w_out")], ["wo"])
        for t in range(NT):
            for half in range(2):
                hs = slice(512 * half, 512 * half + 512)
                pb, pk = pbank()
                for k in range(8):
                    kb.mm(pb[:], mT[:, k, 128 * t:128 * t + 128], wo[:, k, hs], k == 0, k == 7, [("mT", t), "wo"], [pk])
                kb.v("dve", lambda: V.tensor_add(out=x_sb[:, t, hs], in0=x_sb[:, t, hs], in1=pb[:]), [("x", t), pk], [("x", t)])
        kb.release(mk)

    def stage_ffn(li, hT):
        mk = kb.mark()
        wpool = Rot(kb, "wf", [128, 8, 256], BF16, 3)
        sar = Rot(kb, "fsa", [128, 512], F32, 2)
        ur = Rot(kb, "fu", [128, 512], BF16, 3)
        for j in range(22):
            wt, wk = wpool.next()
            kb.dma(wt[:, :, 0:128], wb["w_ffn_in"][D * li:D * li + D, 128 * j:128 * j + 128].rearrange("(k p) c -> p k c", p=128),
                   [("wb", "w_ffn_in")], [wk])
            kb.dma(wt[:, :, 128:256], wb["w_ffn_in"][D * li:D * li + D, FFN + 128 * j:FFN + 128 * j + 128].rearrange("(k p) c -> p k c", p=128),
                   [("wb", "w_ffn_in")], [wk])
            for jb in range(4):
                pa, pak = gemm_fm(hT, wt, wk, jb, 128, 0)
                pb, pbk = gemm_fm(hT, wt, wk, jb, 128, 128)
                sa, sak = sar.next()
                kb.act(sa[:], pa[:], AF.Silu, [pak], [sak])
                u, uk = ur.next()
                kb.v("dve", lambda: V.tensor_mul(out=u[:], in0=pb[:], in1=sa[:]), [pbk, sak], [uk])
                kb.dma(U_T[128 * j:128 * j + 128, 512 * jb:512 * jb + 512], u[:], [uk], [("U_T", j, jb)], q="pool")
        kb.release(mk)
        mk = kb.mark()
        wfo = kb.sb("wfo", [128, 22, D], BF16)
        for k0 in range(0, 22, 4):
            k1 = min(22, k0 + 4)
            kb.dma(wfo[:, k0:k1, :], wb["w_ffn_out"][FFN * li + 128 * k0:FFN * li + 128 * k1, :].rearrange("(k p) c -> p k c", p=128),
                   [("wb", "w_ffn_out")], ["wfo"])
        uTr = Rot(kb, "uTl", [128, 22, 128], BF16, 2)
        for t in range(NT):
            uT, uk = uTr.next()
            kb.dma(uT[:], U_T[:, 128 * t:128 * t + 128].rearrange("(k p) n -> p k n", p=128), [("U_T", j, t // 4) for j in range(22)], [uk])
            for half in range(2):
                hs = slice(512 * half, 512 * half + 512)
                pb, pk = pbank()
                for k in range(22):
                    kb.mm(pb[:], uT[:, k, :], wfo[:, k, hs], k == 0, k == 21, [uk, "wfo"], [pk])
                kb.v("dve", lambda: V.tensor_add(out=x_sb[:, t, hs], in0=x_sb[:, t, hs], in1=pb[:]), [("x", t), pk], [("x", t)])
        kb.release(mk)

    def final_norm(s):
        mk = kb.mark()
        gbc = kb.sb("gbcf", [128, D], F32)
        kb.dma(gbc[:], bass.AP(tensor=norm_final_g.tensor, offset=0, ap=[[0, 128], [1, D]]), [], ["gbcf"])
        junk = Rot(kb, "junkf", [128, D], BF16, 2)
        ssr = Rot(kb, "ssf", [128, 4], F32, 3)
        yr = Rot(kb, "yo", [128, D], F32, 3)
        for t in range(NT):
            jt, jk = junk.next(); ss, sk = ssr.next(); yo, yk = yr.next()
            kb.act(jt[:], x_sb[:, t, :], AF.Square, [("x", t)], [jk, sk + "a"], accum_out=ss[:, 0:1])
            kb.act(ss[:, 1:2], ss[:, 0:1], AF.Sqrt, [sk + "a"], [sk + "b"], scale=1.0 / D, bias=1e-6)
            kb.v("dve", lambda: V.reciprocal(out=ss[:, 2:3], in_=ss[:, 1:2]), [sk + "b"], [sk + "c"])
            kb.v("dve", lambda: V.scalar_tensor_tensor(out=yo[:], in0=x_sb[:, t, :], scalar=ss[:, 2:3], in1=gbc[:],
                                                       op0=ALU.mult, op1=ALU.mult), [("x", t), sk + "c", "gbcf"], [yk])
            ev = kb.dma(y_d[s * T + 128 * t:s * T + 128 * t + 128, :], yo[:], [yk], [("y", s, t)], q="sp")
            out_events.append(ev)
        kb.release(mk)

    out_events = []

    for s in range(nseq):
        for t in range(NT):
            kb.dma(x_sb[:, t, :], x_d[s * T + 128 * t:s * T + 128 * t + 128, :], [], [("x", t)])
        for li in range(depth):
            mk = kb.mark()
            hT = kb.sb("hT", [128, 8, T], BF16)
            rmsnorm_to_hT(hT, bass.AP(tensor=norm_mix_g.tensor, offset=li * D, ap=[[0, 128], [1, D]]))
            if stop_after == "norm":
                break
            stage_proj(li, hT)
            kb.release(mk)
            if stop_after == "proj":
                break
            stage_retention(li)
            if stop_after == "ret":
                break
            try:
                stage_nsa(li)
            except _Stop:
                break
            if stop_after == "nsa":
                break
            stage_merge(li)
            if stop_after == "merge":
                break
            mk = kb.mark()
            hT = kb.sb("hT2", [128, 8, T], BF16)
            rmsnorm_to_hT(hT, bass.AP(tensor=norm_ffn_g.tensor, offset=li * D, ap=[[0, 128], [1, D]]))
            stage_ffn(li, hT)
            kb.release(mk)
            kb.prune()
        final_norm(s)

    sp = nc.sync
    for sname, sd in kb.streams.items():
        if sd["count"] > 0:
            sp.wait_ge(sd["sem"], sd["count"] * sd["inc"])
    kb.release(0)
    return nc, kb


_CACHE = {}


def _host_inputs(inputs):
    C = _consts()
    f = lambda a: np.ascontiguousarray(np.asarray(a, dtype=np.float32))
    m = {
        "w_in": f(inputs["w_in"]).reshape(DEPTH * D, CIN),
        "cmp_w1_k": f(inputs["cmp_w1_k"]).reshape(DEPTH * 2048, 256),
        "cmp_w2_k": f(inputs["cmp_w2_k"]).reshape(DEPTH * 256, 64),
        "cmp_w1_v": f(inputs["cmp_w1_v"]).reshape(DEPTH * 2048, 256),
        "cmp_w2_v": f(inputs["cmp_w2_v"]).reshape(DEPTH * 256, 64),
        "w_o_ret": f(inputs["w_o_ret"]).reshape(DEPTH * 2048, D),
        "w_o_nsa": f(inputs["w_o_nsa"]).reshape(DEPTH * D, D),
        "w_out": f(inputs["w_out"]).reshape(DEPTH * D, D),
        "w_ffn_in": f(inputs["w_ffn_in"]).reshape(DEPTH * D, 2 * FFN),
        "w_ffn_out": f(inputs["w_ffn_out"]).reshape(DEPTH * FFN, D),
        "norm_mix_g": f(inputs["norm_mix_g"]),
        "norm_ffn_g": f(inputs["norm_ffn_g"]),
        "norm_final_g": f(inputs["norm_final_g"]).reshape(1, D),
        "cmp_pos_k": f(inputs["cmp_pos_k"]).reshape(DEPTH * 32, 64),
        "cmp_pos_v": f(inputs["cmp_pos_v"]).reshape(DEPTH * 32, 64),
        "rel_bias": f(inputs["rel_bias"]).reshape(1, 512),
        "c_cos": C["cosT"], "c_sin": C["sinT"], "c_ident": C["ident"],
        "c_dmask": C["dmaskT"].reshape(128, 512), "c_retsc": C["retsc"],
        "c_dist0": C["dist0"], "c_dist1": C["dist1"], "c_distg": C["distg"],
        "c_wedge": C["wedge"], "c_expand": C["expand"], "c_overlap": C["overlap"],
        "c_keep": C["keep"].reshape(128, 512), "c_addm": C["addm"].reshape(128, 512),
    }
    return m


def kernel(**inputs):
    x = np.ascontiguousarray(np.asarray(inputs["x"], dtype=np.float32))
    B = x.shape[0]
    if "nc" not in _CACHE:
        _CACHE["nc"] = build()[0]
    nc = _CACHE["nc"]
    shared = _host_inputs(inputs)
    per = B // NCORES
    in_maps = []
    for c in range(NCORES):
        m = dict(shared)
        m["x"] = x[c * per:(c + 1) * per].reshape(per * T, D)
        in_maps.append(m)
    res = run_bass_kernel_spmd(nc, in_maps, core_ids=list(range(NCORES)))
    out = np.concatenate([r["y"].reshape(per, T, D) for r in res.results], axis=0)
    return out.astype(np.float32)
```

```python
import math
import numpy as np
import ml_dtypes
import concourse.bass as bass
import concourse.mybir as mybir
from concourse.bass_utils import run_bass_kernel_spmd

F32 = mybir.dt.float32
BF16 = mybir.dt.bfloat16
AF = mybir.ActivationFunctionType
ALU = mybir.AluOpType
AX = mybir.AxisListType

T = 2048
D = 1024
NT = 16
DEPTH = 2
CIN = 10032
NSEQ = 4
NCORES = 8
FFN = 2816
NEGM = 30000.0
O_QR, O_KR, O_VR, O_GR, O_QN, O_KV, O_GATE, O_MA, O_MB = 0, 1024, 2048, 4096, 6144, 7168, 7936, 7984, 9008


def _t5_bucket_np(dist):
    dist = np.maximum(dist, 0)
    d32 = np.maximum(dist, 1).astype(np.float32)
    large = 16 + (np.log(d32 / np.float32(16)) / np.float32(math.log(128 / 16)) * np.float32(16)).astype(np.int32)
    large = np.minimum(large, 31)
    return np.where(dist < 16, dist, large)


def _consts():
    c = {}
    half = 128
    freqs = (10000.0 ** (-np.arange(half, dtype=np.float32) / half)).astype(np.float32)
    ang = (np.arange(T, dtype=np.float32)[None, :] * freqs[:, None]).astype(np.float32)
    c["cosT"] = np.cos(ang).astype(np.float32)
    c["sinT"] = np.sin(ang).astype(np.float32)
    c["ident"] = np.eye(128, dtype=np.float32).astype(ml_dtypes.bfloat16)
    lg = np.log(1.0 - 2.0 ** (-5.0 - np.arange(4, dtype=np.float64)))
    m = np.arange(128, dtype=np.float64)
    dm = np.zeros((128, 4, 128), np.float32)
    for h in range(4):
        dm[:, h, :] = (np.exp(-(m + 1.0) * lg[h])[:, None] * (m[None, :] >= m[:, None])).astype(np.float32)
    c["dmaskT"] = dm
    rs = np.zeros((128, 12), np.float32)
    for h in range(4):
        rs[:, h] = np.exp((m + 1.0) * lg[h])
        rs[:, 4 + h] = np.exp((127.0 - m) * lg[h])
    c["retsc"] = rs
    c["gchunk"] = [float(np.exp(128.0 * lg[h])) for h in range(4)]
    bk = _t5_bucket_np(np.arange(0, 4096))
    c["thr"] = [float(np.argmax(bk >= b)) for b in range(32)]
    r = np.arange(128, dtype=np.float32)[:, None]
    cc = np.arange(128, dtype=np.float32)[None, :]
    c["dist0"] = (cc - r).astype(np.float32)
    c["dist1"] = (cc - r + 128).astype(np.float32)
    cg = np.arange(247, dtype=np.float32)[None, :]
    c["distg"] = (r - 16.0 * (cg - 120.0) - 31.0).astype(np.float32)
    c["wedge"] = (cc < r).astype(np.float32).astype(ml_dtypes.bfloat16)
    ex = np.zeros((64, T), np.float32)
    for j in range(32):
        ex[j, 64 * j:64 * j + 64] = 1.0
    c["expand"] = ex.astype(ml_dtypes.bfloat16)
    cmp_end = np.arange(127) * 16 + 31
    sel_start = np.arange(32) * 64
    ov = ((cmp_end[:, None] - 31 < sel_start[None] + 64) & (cmp_end[:, None] >= sel_start[None]))
    c["overlap"] = ov.astype(np.float32).astype(ml_dtypes.bfloat16)
    keep = np.zeros((128, 16, 32), np.float32)
    add = np.zeros((128, 16, 32), np.float32)
    jj = np.arange(32)
    for t in range(16):
        pos = 128 * t + np.arange(128)
        cur = pos // 64
        forced = (jj[None] == 0) | (jj[None] == cur[:, None]) | (jj[None] == cur[:, None] - 1)
        valid = sel_start[None] <= pos[:, None]
        keep[:, t, :] = (valid & ~forced)
        add[:, t, :] = np.where(valid, np.where(forced, 1e4, 0.0), -1e30)
    c["keep"] = keep
    c["addm"] = add
    return c


NO_SAME_ENGINE_SYNC = False


class KB:
    def __init__(self):
        self.nc = bass.Bass("TRN2", target_bir_lowering=False)
        nc = self.nc
        self.ctx = []
        self.eng = {"pe": nc.tensor, "act": nc.scalar, "dve": nc.vector, "pool": nc.gpsimd, "sp": nc.sync}
        self.streams = {}
        for e in self.eng:
            self.new_stream(e, e, 1)
        self.NDQ = 16
        self.dq = {}
        self.dqi = {}
        for q in ("sp", "pool", "act"):
            self.dq[q] = []
            self.dqi[q] = 0
            for j in range(self.NDQ if q != "pool" else 6):
                nm = "d%s%d" % (q, j)
                self.new_stream(nm, q, 16)
                self.dq[q].append(nm)
        self.clock = {e: {} for e in self.eng}
        self.evclock = {}
        self.state = {}
        self.nwaits = 0
        self.ninst = 0
        self._uid = 0

    def enter(self, cm):
        v = cm.__enter__()
        self.ctx.append(cm)
        return v

    def mark(self):
        return len(self.ctx)

    def release(self, mark):
        if len(self.ctx) > mark and hasattr(self, "clock"):
            self.barrier()
        while len(self.ctx) > mark:
            self.ctx.pop().__exit__(None, None, None)

    def barrier(self):
        for en, e in self.eng.items():
            clk = self.clock[en]
            for sname, sd in self.streams.items():
                if sd["count"] > clk.get(sname, 0):
                    e.wait_ge(sd["sem"], sd["count"] * sd["inc"])
                    clk[sname] = sd["count"]
                    self.nwaits += 1

    def new_stream(self, name, eng, inc):
        sem = self.enter(self.nc.semaphore("sem_" + name))
        self.streams[name] = dict(sem=sem, count=0, inc=inc, eng=eng)

    def sb(self, name, shape, dtype):
        self._uid += 1
        return self.enter(self.nc.sbuf_tensor("%s_%d" % (name, self._uid), list(shape), dtype))

    def ps(self, name, shape, dtype):
        self._uid += 1
        return self.enter(self.nc.psum_tensor("%s_%d" % (name, self._uid), list(shape), dtype))

    def dram(self, name, shape, dtype, kind="Internal"):
        return self.nc.dram_tensor(name, list(shape), dtype, kind=kind).ap()

    def op(self, eng, fn, reads=(), writes=(), stream=None, nosame=False, extra=()):
        st = self.state
        need = {}
        for w in extra:
            if w[1] > 0 and need.get(w[0], 0) < w[1]:
                need[w[0]] = w[1]
        for k in reads:
            s = st.get(k)
            if s and s[0]:
                w = s[0]
                if need.get(w[0], 0) < w[1]:
                    need[w[0]] = w[1]
        for k in writes:
            s = st.get(k)
            if s:
                if s[0]:
                    w = s[0]
                    if need.get(w[0], 0) < w[1]:
                        need[w[0]] = w[1]
                for w in s[1]:
                    if need.get(w[0], 0) < w[1]:
                        need[w[0]] = w[1]
        clk = self.clock[eng]
        e = self.eng[eng]
        for sname, idx in need.items():
            if (nosame or NO_SAME_ENGINE_SYNC) and sname == eng:
                continue
            if clk.get(sname, 0) >= idx:
                continue
            sd = self.streams[sname]
            e.wait_ge(sd["sem"], idx * sd["inc"])
            self.nwaits += 1
            ev = self.evclock.get((sname, idx))
            clk[sname] = idx
            if ev:
                for k2, v2 in ev.items():
                    if clk.get(k2, 0) < v2:
                        clk[k2] = v2
        inst = fn()
        sname = stream or eng
        sd = self.streams[sname]
        sd["count"] += 1
        idx = sd["count"]
        inst.then_inc(sd["sem"], sd["inc"])
        self.ninst += 1
        snap = dict(clk)
        self.evclock[(sname, idx)] = snap
        evt = (sname, idx)
        for k in reads:
            s = st.get(k)
            if s is None:
                st[k] = [None, [evt]]
            else:
                s[1].append(evt)
        for k in writes:
            st[k] = [evt, []]
        return evt

    def prune(self):
        if len(self.evclock) > 400000:
            keep = {}
            for k, s in self.state.items():
                if s[0]:
                    keep[s[0]] = self.evclock.get(s[0])
                for w in s[1]:
                    keep[w] = self.evclock.get(w)
            self.evclock = {k: v for k, v in keep.items() if v is not None}

    def dma(self, out, in_, reads, writes, q="sp"):
        eng = q
        j = self.dqi[q]
        self.dqi[q] = (j + 1) % len(self.dq[q])
        stream = self.dq[q][j]
        e = self.eng[eng]
        prev = (stream, self.streams[stream]["count"])
        return self.op(eng, lambda: e.dma_start(out=out, in_=in_), reads, writes, stream=stream, extra=[prev])

    def mm(self, out, lhsT, rhs, start, stop, reads, writes, sgc=False):
        pe = self.nc.tensor
        if sgc:
            return self.op("pe", lambda: pe.matmul(out, lhsT, rhs, start=start, stop=stop, skip_group_check=True),
                           reads, writes, nosame=True)
        return self.op("pe", lambda: pe.matmul(out, lhsT, rhs, start=start, stop=stop), reads, writes, nosame=True)

    def tr(self, out, in_, ident, reads, writes):
        pe = self.nc.tensor
        return self.op("pe", lambda: pe.transpose(out, in_, ident), reads, writes, nosame=True)

    def act(self, out, in_, func, reads, writes, scale=1.0, bias=0.0, accum_out=None):
        a = self.nc.scalar
        if accum_out is not None:
            return self.op("act", lambda: a.activation(out=out, in_=in_, func=func, bias=bias, scale=scale,
                                                       accum_out=accum_out), reads, writes)
        return self.op("act", lambda: a.activation(out=out, in_=in_, func=func, bias=bias, scale=scale), reads, writes)

    def v(self, eng, fn, reads, writes):
        return self.op(eng, fn, reads, writes)


class _Stop(Exception):
    pass


class Rot:
    def __init__(self, kb, name, shape, dtype, n, psum=False):
        self.tiles = [(kb.ps if psum else kb.sb)(name, shape, dtype) for _ in range(n)]
        self.keys = ["%s#%d#%d" % (name, id(self) % 100000, i) for i in range(n)]
        self.i = 0

    def next(self):
        t, k = self.tiles[self.i], self.keys[self.i]
        self.i = (self.i + 1) % len(self.tiles)
        return t, k


def bcast_ap(ap, dims):
    pa = ap.ap
    return bass.AP(tensor=ap.tensor, offset=ap.offset, ap=[[pa[0][0], pa[0][1]]] + [list(d) for d in dims])


def build(nseq=NSEQ, depth=DEPTH, debug=False, stop_after=None):
    C = _consts()
    kb = KB()
    nc = kb.nc
    V = nc.vector
    G = nc.gpsimd
    dk = "ExternalOutput" if debug else "Internal"

    def dbg(name, ap, shape, dtype, keys):
        if not debug:
            return
        d = nc.dram_tensor("dbg_" + name, list(shape), dtype, kind="ExternalOutput").ap()
        kb.dma(d, ap, keys, [("dbg", name)])

    def din(name, shape, dtype=F32):
        return nc.dram_tensor(name, list(shape), dtype, kind="ExternalInput").ap()

    x_d = din("x", [nseq * T, D])
    y_d = nc.dram_tensor("y", [nseq * T, D], F32, kind="ExternalOutput").ap()
    w_f32 = {
        "w_in": din("w_in", [DEPTH * D, CIN]),
        "cmp_w1_k": din("cmp_w1_k", [DEPTH * 2048, 256]),
        "cmp_w2_k": din("cmp_w2_k", [DEPTH * 256, 64]),
        "cmp_w1_v": din("cmp_w1_v", [DEPTH * 2048, 256]),
        "cmp_w2_v": din("cmp_w2_v", [DEPTH * 256, 64]),
        "w_o_ret": din("w_o_ret", [DEPTH * 2048, D]),
        "w_o_nsa": din("w_o_nsa", [DEPTH * D, D]),
        "w_out": din("w_out", [DEPTH * D, D]),
        "w_ffn_in": din("w_ffn_in", [DEPTH * D, 2 * FFN]),
        "w_ffn_out": din("w_ffn_out", [DEPTH * FFN, D]),
    }
    norm_mix_g = din("norm_mix_g", [DEPTH, D])
    norm_ffn_g = din("norm_ffn_g", [DEPTH, D])
    norm_final_g = din("norm_final_g", [1, D])
    cmp_pos_k = din("cmp_pos_k", [DEPTH * 32, 64])
    cmp_pos_v = din("cmp_pos_v", [DEPTH * 32, 64])
    rel_bias = din("rel_bias", [1, 512])
    c_cos = din("c_cos", [128, T]); c_sin = din("c_sin", [128, T])
    c_ident = din("c_ident", [128, 128], BF16)
    c_dmask = din("c_dmask", [128, 512]); c_retsc = din("c_retsc", [128, 12])
    c_dist0 = din("c_dist0", [128, 128]); c_dist1 = din("c_dist1", [128, 128]); c_distg = din("c_distg", [128, 247])
    c_wedge = din("c_wedge", [128, 128], BF16)
    c_expand = din("c_expand", [64, T], BF16)
    c_overlap = din("c_overlap", [127, 32], BF16)
    c_keep = din("c_keep", [128, 512]); c_addm = din("c_addm", [128, 512])

    wb = {k: kb.dram("wb_" + k, v.shape, BF16) for k, v in w_f32.items()}
    P_qrT = kb.dram("P_qrT", [1024, T], BF16, dk)
    P_krT = kb.dram("P_krT", [1024, T], BF16, dk)
    P_kz = kb.dram("P_kz", [T, 1024], BF16, dk)
    P_v = kb.dram("P_v", [T, 2048], BF16, dk)
    P_sg = kb.dram("P_sg", [T, 2048], BF16, dk)
    P_qnT = kb.dram("P_qnT", [1024, T], BF16, dk)
    P_kcT = kb.dram("P_kcT", [128, T], BF16, dk)
    P_vcT = kb.dram("P_vcT", [128, T], BF16, dk)
    P_ksT = kb.dram("P_ksT", [256, T], BF16, dk)
    P_kwT = kb.dram("P_kwT", [256, T], BF16, dk)
    P_vs = kb.dram("P_vs", [T, 128], BF16, dk)
    P_vw = kb.dram("P_vw", [T, 128], BF16, dk)
    P_gate = kb.dram("P_gate", [T, 48], F32, dk)
    P_ma = kb.dram("P_ma", [T, 1024], BF16, dk)
    P_mb = kb.dram("P_mb", [T, 1024], BF16, dk)
    Z_T = kb.dram("Z_T", [2048, T], BF16, dk)
    ON_T = kb.dram("ON_T", [1024, T], BF16, dk)
    U_T = kb.dram("U_T", [FFN, T], BF16, dk)

    for k, src in w_f32.items():
        rows = src.shape[0]
        step = 256
        for r0 in range(0, rows, step):
            r1 = min(rows, r0 + step)
            kb.dma(wb[k][r0:r1, :], src[r0:r1, :], reads=[], writes=[("wb", k)], q="pool")

    x_sb = kb.sb("x", [128, NT, D], F32)
    ident = kb.sb("ident", [128, 128], BF16)
    kb.dma(ident[:], c_ident, [], ["ident"])
    PB = [kb.ps("pb", [128, 512], F32) for _ in range(6)]
    PBK = ["pb%d" % i for i in range(6)]
    PT2 = [kb.ps("pt", [128, 1024], BF16) for _ in range(2)]
    PTK = ["pt0", "pt1"]
    prr = [0]
    ptr = [0]

    def pbank():
        i = prr[0]; prr[0] = (i + 1) % 6
        return PB[i], PBK[i]

    def ptbank():
        i = ptr[0]; ptr[0] = (i + 1) % 2
        return PT2[i], PTK[i]

    Gt = kb.sb("Gt", [128, 16, 247], BF16)
    E0 = kb.sb("E0", [128, 16, 128], BF16)
    E1 = kb.sb("E1", [128, 16, 128], BF16)
    wedge = kb.sb("wedge", [128, 128], BF16)
    kb.dma(wedge[:], c_wedge, [], ["wedge"])

    def build_tables():
        mk = kb.mark()
        relb = kb.sb("relb", [128, 32, 16], F32)
        kb.dma(relb[:].rearrange("p b h -> p (b h)"),
               bass.AP(tensor=rel_bias.tensor, offset=0, ap=[[0, 128], [1, 512]]), [], ["relb"])
        dl = kb.sb("dl", [128, 32, 16], F32)
        kb.v("dve", lambda: V.tensor_sub(out=dl[:, 1:32, :], in0=relb[:, 1:32, :], in1=relb[:, 0:31, :]), ["relb"], ["dl"])
        kb.v("dve", lambda: V.tensor_sub(out=dl[:, 0:1, :], in0=relb[:, 0:1, :], in1=relb[:, 31:32, :]), ["relb"], ["dl"])
        W = 503
        dist = kb.sb("dist", [128, W], F32)
        kb.dma(dist[:, 0:128], c_dist0, [], ["dist"])
        kb.dma(dist[:, 128:256], c_dist1, [], ["dist"])
        kb.dma(dist[:, 256:503], c_distg, [], ["dist"])
        acc = kb.sb("acc", [128, 16, W], F32)
        tmps = Rot(kb, "tmp01", [128, W], F32, 3)
        ACCK = [("acc", h) for h in range(16)]
        kb.v("dve", lambda: V.tensor_copy(out=acc[:], in_=bcast_ap(dl[:, 0, :], [[1, 16], [0, W]])), ["dl"], ACCK)
        for b in range(1, 33):
            tmp, tk = tmps.next()
            if b < 32:
                thr = C["thr"][b]
                kb.v("dve", lambda: V.tensor_single_scalar(out=tmp[:], in_=dist[:], scalar=thr, op=ALU.is_ge), ["dist"], [tk])
                for h in range(16):
                    kb.v("dve", lambda: V.scalar_tensor_tensor(out=acc[:, h, :], in0=tmp[:], scalar=dl[:, b, h:h + 1], in1=acc[:, h, :],
                                                               op0=ALU.mult, op1=ALU.add), [tk, "dl", ("acc", h)], [("acc", h)])
            else:
                kb.v("dve", lambda: V.tensor_scalar(out=tmp[:], in0=dist[:], scalar1=0.0, scalar2=-NEGM, op0=ALU.is_lt, op1=ALU.mult),
                     ["dist"], [tk])
                kb.v("dve", lambda: V.tensor_add(out=acc[:], in0=acc[:], in1=bcast_ap(tmp[:], [[0, 16], [1, W]])), ACCK + [tk], ACCK)
        kb.act(E0[:], acc[:, :, 0:128], AF.Exp, ACCK, ["E0"])
        kb.act(E1[:], acc[:, :, 128:256], AF.Exp, ACCK, ["E1"])
        kb.v("dve", lambda: V.tensor_copy(out=Gt[:], in_=acc[:, :, 256:503]), ACCK, ["Gt"])
        kb.release(mk)

    build_tables()

    HT_ALL = [("hT", t) for t in range(NT)]
    dbg_once = [True]

    def rmsnorm_to_hT(hT, g_row_ap):
        mk = kb.mark()
        gbc = kb.sb("gbc", [128, D], F32)
        kb.dma(gbc[:], g_row_ap, [], ["gbc"])
        junk = Rot(kb, "junk", [128, D], BF16, 2)
        ssr = Rot(kb, "ss", [128, 4], F32, 3)
        hbr = Rot(kb, "hb", [128, D], BF16, 2)
        for t in range(NT):
            jt, jk = junk.next(); ss, sk = ssr.next(); hb, hk = hbr.next()
            kb.act(jt[:], x_sb[:, t, :], AF.Square, [("x", t)], [jk, sk + "a"], accum_out=ss[:, 0:1])
            kb.act(ss[:, 1:2], ss[:, 0:1], AF.Sqrt, [sk + "a"], [sk + "b"], scale=1.0 / D, bias=1e-6)
            kb.v("dve", lambda: V.reciprocal(out=ss[:, 2:3], in_=ss[:, 1:2]), [sk + "b"], [sk + "c"])
            kb.v("dve", lambda: V.scalar_tensor_tensor(out=hb[:], in0=x_sb[:, t, :], scalar=ss[:, 2:3], in1=gbc[:],
                                                       op0=ALU.mult, op1=ALU.mult), [("x", t), sk + "c", "gbc"], [hk])
            pt, pk = ptbank()
            for k in range(8):
                kb.tr(pt[:, 128 * k:128 * k + 128], hb[:, 128 * k:128 * k + 128], ident[:], [hk, "ident"], [pk])
            kb.act(hT[:, :, 128 * t:128 * t + 128], pt[:].rearrange("p (k n) -> p k n", k=8), AF.Copy, [pk], [("hT", t)])
        kb.release(mk)


    def load_w(pool, wname, row0, c0, ncols, dup64=None):
        wt, wk = pool.next()
        src = wb[wname]
        if dup64 is None:
            kb.dma(wt[:, :, 0:ncols], src[row0:row0 + 1024, c0:c0 + ncols].rearrange("(k p) c -> p k c", p=128),
                   [("wb", wname)], [wk])
        else:
            for hh in range(2):
                kb.dma(wt[:, :, 64 * hh:64 * hh + 64], src[row0:row0 + 1024, c0:c0 + 64].rearrange("(k p) c -> p k c", p=128),
                       [("wb", wname)], [wk])
        return wt, wk

    def gemm_fm(hT, wt, wk, jb, ncols=128, c0=0):
        pb, pk = pbank()
        for k in range(8):
            kb.mm(pb[0:ncols, :], wt[:, k, c0:c0 + ncols], hT[:, k, 512 * jb:512 * jb + 512], k == 0, k == 7,
                  [wk] + [("hT", 4 * jb + i) for i in range(4)], [pk])
        return pb, pk

    def gemm_tm(hT, wt, wk, t, ncols):
        pb, pk = pbank()
        for k in range(8):
            kb.mm(pb[:, 0:ncols], hT[:, k, 128 * t:128 * t + 128], wt[:, k, 0:ncols], k == 0, k == 7,
                  [wk, ("hT", t)], [pk])
        return pb, pk

    def stage_proj(li, hT):
        mk = kb.mark()
        row0 = li * D
        wpool = Rot(kb, "wt", [128, 8, 512], BF16, 3)
        cosT = kb.sb("cosT", [128, T], F32); sinT = kb.sb("sinT", [128, T], F32)
        kb.dma(cosT[:], c_cos, [], ["cosT"]); kb.dma(sinT[:], c_sin, [], ["sinT"])
        retsc = kb.sb("retsc", [128, 12], F32)
        kb.dma(retsc[:], c_retsc, [], ["retsc"])
        xab = Rot(kb, "xab", [128, 2, 512], F32, 2)
        tmpr = Rot(kb, "rtmp", [128, 4, 512], F32, 2)
        rotr = Rot(kb, "rot", [128, 2, 512], BF16, 2)
        kzr = Rot(kb, "kz", [128, 4, 256], BF16, 2)
        stg = Rot(kb, "stg", [128, 512], BF16, 4)
        stgf = Rot(kb, "stgf", [128, 48], F32, 2)
        flip = [0]

        for (isk, obase, dst) in ((0, O_QR, P_qrT), (1, O_KR, P_krT)):
            sc = 1.0 / 16.0 if isk else 1.0
            for h in range(4):
                wt, wk = load_w(wpool, "w_in", row0, obase + 256 * h, 256)
                for jb in range(4):
                    pa, pak = gemm_fm(hT, wt, wk, jb, 128, 0)
                    pbb, pbk = gemm_fm(hT, wt, wk, jb, 128, 128)
                    xa, xk = xab.next()
                    kb.act(xa[:, 0, :], pa[:], AF.Copy, [pak], [xk + "a"], scale=sc)
                    kb.act(xa[:, 1, :], pbb[:], AF.Copy, [pbk], [xk + "b"], scale=sc)
                    tm, tk = tmpr.next()
                    cs = cosT[:, 512 * jb:512 * jb + 512]; sn = sinT[:, 512 * jb:512 * jb + 512]
                    kb.v("dve", lambda: V.tensor_mul(out=tm[:, 0, :], in0=xa[:, 0, :], in1=cs), [xk + "a", "cosT"], [tk + "0"])
                    kb.v("pool", lambda: G.tensor_mul(out=tm[:, 1, :], in0=xa[:, 1, :], in1=sn), [xk + "b", "sinT"], [tk + "1"])
                    kb.v("dve", lambda: V.tensor_mul(out=tm[:, 2, :], in0=xa[:, 0, :], in1=sn), [xk + "a", "sinT"], [tk + "2"])
                    kb.v("pool", lambda: G.tensor_mul(out=tm[:, 3, :], in0=xa[:, 1, :], in1=cs), [xk + "b", "cosT"], [tk + "3"])
                    ro, rk = rotr.next()
                    kb.v("dve", lambda: V.tensor_sub(out=ro[:, 0, :], in0=tm[:, 0, :], in1=tm[:, 1, :]), [tk + "0", tk + "1"], [rk + "a"])
                    kb.v("pool", lambda: G.tensor_add(out=ro[:, 1, :], in0=tm[:, 2, :], in1=tm[:, 3, :]), [tk + "2", tk + "3"], [rk + "b"])
                    kb.dma(dst[256 * h:256 * h + 256, 512 * jb:512 * jb + 512].rearrange("(c p) n -> p c n", p=128), ro[:],
                           [rk + "a", rk + "b"], [("P_r", isk, h, jb)], q="pool")
                    if isk:
                        kz, kzk = kzr.next()
                        pt, pk = ptbank()
                        for i in range(4):
                            for c in range(2):
                                kb.tr(pt[:, 256 * i + 128 * c:256 * i + 128 * c + 128], ro[:, c, 128 * i:128 * i + 128], ident[:],
                                      [rk + "a", rk + "b", "ident"], [pk])
                        kb.act(kz[:].rearrange("p i d -> p (i d)"), pt[:], AF.Copy, [pk, "retsc"], [kzk], scale=retsc[:, 4 + h:5 + h])
                        kb.dma(P_kz[512 * jb:512 * jb + 512, 256 * h:256 * h + 256].rearrange("(i p) d -> p i d", p=128), kz[:],
                               [kzk], [("P_kz", h, jb)], q="pool")

        def tm_group(obase, ncols_total, dst, func, dkey, dstf32=False):
            for c0 in range(0, ncols_total, 512):
                ncol = min(512, ncols_total - c0)
                wt, wk = load_w(wpool, "w_in", row0, obase + c0, ncol)
                for t in range(NT):
                    pb, pk = gemm_tm(hT, wt, wk, t, ncol)
                    if dstf32:
                        sg, sk = stgf.next()
                    else:
                        sg, sk = stg.next()
                    if func == AF.Copy and (flip[0] % 2 == 0):
                        kb.v("dve", lambda: V.tensor_copy(out=sg[:, 0:ncol], in_=pb[:, 0:ncol]), [pk], [sk])
                    else:
                        kb.act(sg[:, 0:ncol], pb[:, 0:ncol], func, [pk], [sk])
                    flip[0] += 1
                    kb.dma(dst[128 * t:128 * t + 128, c0:c0 + ncol], sg[:, 0:ncol], [sk], [(dkey, c0 // 512, t)], q="pool")

        tm_group(O_VR, 2048, P_v, AF.Copy, "P_v")
        tm_group(O_GR, 2048, P_sg, AF.Silu, "P_sg")
        tm_group(O_KV + 3 * 128, 128, P_vs, AF.Copy, "P_vs")
        tm_group(O_KV + 5 * 128, 128, P_vw, AF.Copy, "P_vw")
        tm_group(O_GATE, 48, P_gate, AF.Sigmoid, "P_gate", dstf32=True)
        tm_group(O_MA, 1024, P_ma, AF.Sigmoid, "P_ma")
        tm_group(O_MB, 1024, P_mb, AF.Sigmoid, "P_mb")

        def fm_chunk(c0, dst_rows, scale, dkey, dup=False):
            wt, wk = load_w(wpool, "w_in", row0, c0, 128, dup64=(True if dup else None))
            for jb in range(4):
                pb, pk = gemm_fm(hT, wt, wk, jb, 128, 0)
                sg, sk = stg.next()
                kb.act(sg[:], pb[:], AF.Copy, [pk], [sk], scale=scale)
                kb.dma(dst_rows[:, 512 * jb:512 * jb + 512], sg[:], [sk], [(dkey, jb)], q="pool")

        for c in range(8):
            fm_chunk(O_QN + 128 * c, P_qnT[128 * c:128 * c + 128, :], 0.125, ("P_qnT", c))
        fm_chunk(O_KV + 0, P_kcT, 1.0, "P_kcT")
        fm_chunk(O_KV + 128, P_vcT, 1.0, "P_vcT")
        for g in range(2):
            fm_chunk(O_KV + 256 + 64 * g, P_ksT[128 * g:128 * g + 128, :], 1.0, ("P_ksT", g), dup=True)
            fm_chunk(O_KV + 512 + 64 * g, P_kwT[128 * g:128 * g + 128, :], 1.0, ("P_kwT", g), dup=True)
        kb.release(mk)

    PROJ_R_KEYS = [("P_r", isk, h, jb) for isk in range(2) for h in range(4) for jb in range(4)]

    def stage_retention(li):
        mk = kb.mark()
        dmask = kb.sb("dmask", [128, 4, 128], F32)
        kb.dma(dmask[:].rearrange("p h n -> p (h n)"), c_dmask, [], ["dmask"])
        retsc = kb.sb("retsc", [128, 12], F32)
        kb.dma(retsc[:], c_retsc, [], ["retsc"])
        R32s = [kb.sb("R32", [128, 2, 512], F32) for _ in range(4)]
        Rbs = [kb.sb("Rb", [128, 2, 512], BF16) for _ in range(4)]
        qTr = Rot(kb, "qT", [128, 2, 128], BF16, 8)
        kTr = Rot(kb, "kT", [128, 2, 128], BF16, 8)
        kzr = Rot(kb, "kzl", [128, 256], BF16, 8)
        vr = Rot(kb, "vl", [128, 512], BF16, 8)
        sgr = Rot(kb, "sgl", [128, 512], BF16, 8)
        sTr = Rot(kb, "sTb", [128, 128], BF16, 4)
        osr = Rot(kb, "osb", [128, 512], F32, 4)
        str_ = Rot(kb, "stat", [128, 16], F32, 6)
        zr = Rot(kb, "z", [128, 512], BF16, 4)
        z2r = Rot(kb, "z2", [128, 512], F32, 4)
        zTr = Rot(kb, "zT", [128, 4, 128], BF16, 4)
        for h in range(4):
            kb.v("pool", lambda: G.memset(R32s[h][:], 0.0), [], ["R32a%d" % h, "R32b%d" % h])
            kb.v("pool", lambda: G.memset(Rbs[h][:], 0.0), [], ["Rba%d" % h, "Rbb%d" % h])
        units = [(c, h) for c in range(NT) for h in range(4)]

        def r_load(c, h):
            jb = c // 4
            qT, qk = qTr.next(); kT, kk = kTr.next(); kz, kzk = kzr.next(); vv, vk = vr.next(); sg, sgk = sgr.next()
            cols = slice(128 * c, 128 * c + 128)
            kb.dma(qT[:], P_qrT[256 * h:256 * h + 256, cols].rearrange("(c p) n -> p c n", p=128), [("P_r", 0, h, jb)], [qk])
            kb.dma(kT[:], P_krT[256 * h:256 * h + 256, cols].rearrange("(c p) n -> p c n", p=128), [("P_r", 1, h, jb)], [kk])
            kb.dma(kz[:], P_kz[cols, 256 * h:256 * h + 256], [("P_kz", h, jb)], [kzk])
            kb.dma(vv[:], P_v[cols, 512 * h:512 * h + 512], [("P_v", h, c)], [vk])
            kb.dma(sg[:], P_sg[cols, 512 * h:512 * h + 512], [("P_sg", h, c)], [sgk])
            return dict(qT=qT, qk=qk, kT=kT, kk=kk, kz=kz, kzk=kzk, vv=vv, vk=vk, sg=sg, sgk=sgk)

        def r_p1(c, h, L):
            Rb = Rbs[h]
            ps_, psk = pbank()
            for cc in range(2):
                kb.mm(ps_[:, 0:128], L["kT"][:, cc, :], L["qT"][:, cc, :], cc == 0, cc == 1, [L["kk"], L["qk"]], [psk])
            sT, sTk = sTr.next()
            kb.v("dve", lambda: V.tensor_mul(out=sT[:], in0=ps_[:, 0:128], in1=dmask[:, h, :]), [psk, "dmask"], [sTk])
            po, pok = pbank()
            kb.mm(po[:], sT[:], L["vv"][:], True, False, [sTk, L["vk"]], [pok])
            for cc in range(2):
                kb.mm(po[:], L["qT"][:, cc, :], Rb[:, cc, :], False, cc == 1, [L["qk"], "Rb" + "ab"[cc] + str(h)], [pok])
            osb, osk = osr.next()
            kb.act(osb[:], po[:], AF.Copy, [pok, "retsc"], [osk], scale=retsc[:, h:h + 1])
            st, stk = str_.next()
            kb.v("dve", lambda: V.bn_stats(out=st[:, 0:6], in_=osb[:]), [osk], [stk + "a"])
            kb.v("dve", lambda: V.bn_aggr(out=st[:, 6:8], in_=st[:, 0:6]), [stk + "a"], [stk + "b"])
            kb.act(st[:, 8:9], st[:, 7:8], AF.Sqrt, [stk + "b"], [stk + "c"], bias=1e-5)
            L.update(osb=osb, osk=osk, st=st, stk=stk)

        def r_p2(c, h, L):
            gch = C["gchunk"][h]
            R32 = R32s[h]; Rb = Rbs[h]
            st, stk, osb, osk = L["st"], L["stk"], L["osb"], L["osk"]
            cols = slice(128 * c, 128 * c + 128)
            if c < NT - 1:
                for cc in range(2):
                    pr, prk = pbank()
                    kb.mm(pr[:], L["kz"][:, 128 * cc:128 * cc + 128], L["vv"][:], True, True, [L["kzk"], L["vk"]], [prk])
                    kb.v("dve", lambda: V.scalar_tensor_tensor(out=R32[:, cc, :], in0=R32[:, cc, :], scalar=gch, in1=pr[:],
                                                               op0=ALU.mult, op1=ALU.add), ["R32" + "ab"[cc] + str(h), prk], ["R32" + "ab"[cc] + str(h)])
                    kb.act(Rb[:, cc, :], R32[:, cc, :], AF.Copy, ["R32" + "ab"[cc] + str(h)], ["Rb" + "ab"[cc] + str(h)])
            kb.v("dve", lambda: V.reciprocal(out=st[:, 9:10], in_=st[:, 8:9]), [stk + "c"], [stk + "d"])
            z2, z2k = z2r.next()
            kb.v("dve", lambda: V.tensor_scalar(out=z2[:], in0=osb[:], scalar1=st[:, 6:7], scalar2=st[:, 9:10],
                                                op0=ALU.subtract, op1=ALU.mult), [osk, stk + "b", stk + "d"], [z2k])
            z, zk = zr.next()
            kb.v("dve", lambda: V.tensor_mul(out=z[:], in0=z2[:], in1=L["sg"][:]), [z2k, L["sgk"]], [zk])
            L.update(z=z, zk=zk)

        def r_p3(c, h, L):
            z, zk = L["z"], L["zk"]
            cols = slice(128 * c, 128 * c + 128)
            pt, pk = ptbank()
            for e in range(4):
                kb.tr(pt[:, 128 * e:128 * e + 128], z[:, 128 * e:128 * e + 128], ident[:], [zk, "ident"], [pk])
            zT, zTk = zTr.next()
            kb.act(zT[:].rearrange("p e n -> p (e n)"), pt[:, 0:512], AF.Copy, [pk], [zTk])
            kb.dma(Z_T[512 * h:512 * h + 512, cols].rearrange("(e p) n -> p e n", p=128), zT[:], [zTk], [("Z_T", c, h)], q="pool")

        NU = len(units)
        PRE = 4
        Ls = {}
        for n in range(min(PRE, NU)):
            Ls[n] = r_load(*units[n])
        for n in range(NU + 2):
            if n + PRE < NU:
                Ls[n + PRE] = r_load(*units[n + PRE])
            if n < NU:
                r_p1(*units[n], Ls[n])
            if 1 <= n <= NU:
                r_p2(*units[n - 1], Ls[n - 1])
            if n >= 2:
                r_p3(*units[n - 2], Ls.pop(n - 2))
        kb.release(mk)

    def stage_nsa(li):
        mk = kb.mark()
        keep = kb.sb("keep", [128, 16, 32], F32); addm = kb.sb("addm", [128, 16, 32], F32)
        kb.dma(keep[:].rearrange("p t j -> p (t j)"), c_keep, [], ["keep"])
        kb.dma(addm[:].rearrange("p t j -> p (t j)"), c_addm, [], ["addm"])
        gates = kb.sb("gates", [128, 16, 48], F32)
        kb.dma(gates[:], P_gate.rearrange("(t p) c -> p t c", p=128), [("P_gate", 0, t) for t in range(NT)], ["gates"])
        if stop_after == "nsa0":
            raise _Stop()
        kcx = kb.sb("kcx", [128, 2, 2, 128], BF16)
        kb.v("pool", lambda: G.memset(kcx[:], 0.0), [], ["kcx0", "kcx1"])
        vca = kb.sb("vcaug", [128, 2, 97], BF16)
        kb.v("pool", lambda: G.memset(vca[:], 1.0), [], ["vca0", "vca1"])
        for g in range(2):
            kb.dma(vca[0:127, g, 64:96], c_overlap, [], ["vca%d" % g])
        mk2 = kb.mark()
        w1 = Rot(kb, "w1", [128, 32, 256], BF16, 2)
        for kv in range(2):
            nm1 = "cmp_w1_v" if kv else "cmp_w1_k"
            nm2 = "cmp_w2_v" if kv else "cmp_w2_k"
            pos_d = cmp_pos_v if kv else cmp_pos_k
            w1t, w1k = w1.next()
            for hh in range(2):
                kb.dma(w1t[64 * hh:64 * hh + 64, :, :], wb[nm1][2048 * li:2048 * li + 2048, :].rearrange("(l d) h -> d l h", d=64),
                       [("wb", nm1)], [w1k])
            w2t = kb.sb("w2t", [128, 2, 128], BF16)
            for hh in range(2):
                kb.dma(w2t[:, :, 64 * hh:64 * hh + 64], wb[nm2][256 * li:256 * li + 256, :].rearrange("(c p) d -> p c d", p=128),
                       [("wb", nm2)], ["w2t"])
            posl = kb.sb("posl", [32, 64], F32)
            kb.dma(posl[:], pos_d[32 * li:32 * li + 32, :], [], ["posl"])
            posb = kb.sb("posb", [32, 64], BF16)
            kb.v("dve", lambda: V.tensor_copy(out=posb[:], in_=posl[:]), ["posl"], ["posb"])
            pt, pk = ptbank()
            kb.tr(pt[0:64, 0:32], posb[:], ident[0:32, 0:32], ["posb", "ident"], [pk])
            posT = kb.sb("posT", [128, 32], F32)
            kb.act(posT[0:64, :], pt[0:64, 0:32], AF.Copy, [pk], ["posT"])
            kb.act(posT[64:128, :], pt[0:64, 0:32], AF.Copy, [pk], ["posT"])
            kvT = kb.sb("kvT", [128, T], BF16)
            src = P_vcT if kv else P_kcT
            kb.dma(kvT[:], src, [("P_vcT" if kv else "P_kcT", jb) for jb in range(4)], ["kvT"])
            kvA = kb.sb("kvA", [128, T], BF16); kvB = kb.sb("kvB", [128, T], BF16)
            kb.v("dve", lambda: V.tensor_add(out=kvA[:].rearrange("p (a b) -> p a b", b=16), in0=kvT[:].rearrange("p (a b) -> p a b", b=16),
                                             in1=bcast_ap(posT[:, 0:16], [[0, 128], [1, 16]])), ["kvT", "posT"], ["kvA"])
            kb.v("dve", lambda: V.tensor_add(out=kvB[:].rearrange("p (a b) -> p a b", b=16), in0=kvT[:].rearrange("p (a b) -> p a b", b=16),
                                             in1=bcast_ap(posT[:, 16:32], [[0, 128], [1, 16]])), ["kvT", "posT"], ["kvB"])
            for g in range(2):
                pr = slice(64 * g, 64 * g + 64)
                gT = kb.sb("gT", [128, 2, 128], BF16)
                for ch in range(2):
                    pb, pk_ = pbank()
                    for l in range(32):
                        srcT = kvA if l < 16 else kvB
                        rhs = bcast_ap(srcT[pr, l:l + 1], [[16, 127]])
                        kb.mm(pb[:, 0:127], w1t[pr, l, 128 * ch:128 * ch + 128], rhs, l == 0, l == 31,
                              [w1k, "kvA", "kvB"], [pk_])
                    hs = kb.sb("hs", [128, 4, 127], F32)
                    kb.act(hs[:, 0, :], pb[:, 0:127], AF.Copy, [pk_], ["hs0"])
                    kb.v("dve", lambda: V.tensor_mul(out=hs[:, 1, :], in0=hs[:, 0, :], in1=hs[:, 0, :]), ["hs0"], ["hs1"])
                    kb.v("dve", lambda: V.tensor_scalar(out=hs[:, 2, :], in0=hs[:, 1, :], scalar1=0.044715, scalar2=1.0,
                                                        op0=ALU.mult, op1=ALU.add), ["hs1"], ["hs2"])
                    kb.v("dve", lambda: V.tensor_mul(out=hs[:, 3, :], in0=hs[:, 2, :], in1=hs[:, 0, :]), ["hs2", "hs0"], ["hs3"])
                    kb.act(hs[:, 1, :], hs[:, 3, :], AF.Sigmoid, ["hs3", "hs2"], ["hs1"], scale=2.0 * math.sqrt(2.0 / math.pi))
                    kb.v("dve", lambda: V.tensor_mul(out=gT[:, ch, 0:127], in0=hs[:, 1, :], in1=hs[:, 0, :]), ["hs1", "hs0"], ["gT%d" % ch])
                pb, pk_ = pbank()
                if kv == 0:
                    for ch in range(2):
                        kb.mm(pb[:, 0:127], w2t[:, ch, :], gT[:, ch, 0:127], ch == 0, ch == 1, ["w2t", "gT%d" % ch], [pk_])
                    for par in range(2):
                        kb.act(kcx[64 * par:64 * par + 64, par, g, 0:127], pb[64 * par:64 * par + 64, 0:127], AF.Copy, [pk_], ["kcx%d" % g])
                else:
                    for ch in range(2):
                        kb.mm(pb[0:127, 0:64], gT[:, ch, 0:127], w2t[:, ch, 0:64], ch == 0, ch == 1, ["w2t", "gT%d" % ch], [pk_])
                    kb.act(vca[0:127, g, 0:64], pb[0:127, 0:64], AF.Copy, [pk_], ["vca%d" % g])
        kb.release(mk2)
        if stop_after == "nsa1":
            raise _Stop()

        PTr = Rot(kb, "PT", [128, 512], BF16, 4)
        for g in range(2):
            mk3 = kb.mark()
            ksx = kb.sb("ksx", [128, 2, T], BF16); kwx = kb.sb("kwx", [128, 2, T], BF16)
            kb.v("pool", lambda: G.memset(kwx[:], 0.0), [], ["kwx"])
            for par in range(2):
                own = slice(64 * par, 64 * par + 64)
                oth = slice(64 * (1 - par), 64 * (1 - par) + 64)
                kb.dma(ksx[own, par, :], P_ksT[128 * g + 64 * par:128 * g + 64 * par + 64, :], [(("P_ksT", g), jb) for jb in range(4)], ["ksx"])
                kb.dma(ksx[oth, par, :], c_expand, [], ["ksx"])
                kb.dma(kwx[own, par, :], P_kwT[128 * g + 64 * par:128 * g + 64 * par + 64, :], [(("P_kwT", g), jb) for jb in range(4)], ["kwx"])
            vsa = kb.sb("vsa", [128, 16, 65], BF16); vwa = kb.sb("vwa", [128, 16, 65], BF16)
            kb.v("pool", lambda: G.memset(vsa[:], 1.0), [], ["vsa"])
            kb.v("pool", lambda: G.memset(vwa[:], 1.0), [], ["vwa"])
            kb.dma(vsa[:, :, 0:64], P_vs[:, 64 * g:64 * g + 64].rearrange("(t p) d -> p t d", p=128),
                   [("P_vs", 0, t) for t in range(NT)], ["vsa"])
            kb.dma(vwa[:, :, 0:64], P_vw[:, 64 * g:64 * g + 64].rearrange("(t p) d -> p t d", p=128),
                   [("P_vw", 0, t) for t in range(NT)], ["vwa"])
            qs = kb.sb("qs", [128, 8, T], BF16)
            kb.v("pool", lambda: G.memset(qs[:, 0:4, :], 0.0), [], [("qsz", 0)])
            kb.v("pool", lambda: G.memset(qs[:, 4:8, :], 0.0), [], [("qsz", 1)])
            for hh in range(8):
                par = hh % 2
                own = slice(64 * par, 64 * par + 64)
                kb.dma(qs[own, hh, :], P_qnT[128 * (4 * g + hh // 2) + 64 * par:128 * (4 * g + hh // 2) + 64 * par + 64, :],
                       [(("P_qnT", 4 * g + hh // 2), jb) for jb in range(4)] + [("qsz", hh // 4)], [("qsq", hh)])
            og = kb.sb("og", [128, 16, 512], F32)
            ocr = Rot(kb, "ocimp", [128, 8, 96], F32, 2)
            zcr = Rot(kb, "zc", [128, 127], F32, 3)
            pcr = Rot(kb, "pc", [128, 128], BF16, 4)
            pTr = Rot(kb, "pTc", [128, 128], BF16, 4)
            smr = Rot(kb, "sm", [128, 12], F32, 4)
            impr = Rot(kb, "imp", [128, 4, 32], F32, 2)
            ngr = Rot(kb, "ngm", [128, 64], BF16, 2)
            for i in range(2):
                kb.v("pool", lambda: G.memset(ngr.tiles[i][:], 0.0), [], [ngr.keys[i]])
            citems = [(t, hh) for t in range(NT) for hh in range(8)]
            octile = {}
            crr = [0]

            def cA(t, hh):
                pr = slice(64 * (hh % 2), 64 * (hh % 2) + 64)
                h = 8 * g + hh
                j4 = crr[0]; crr[0] = (j4 + 1) % 4
                pb, pk = PB[j4], PBK[j4]
                kb.mm(pb[:, 0:127], qs[:, hh, 128 * t:128 * t + 128], kcx[:, hh % 2, g, 0:127], True, True,
                      [("qsq", hh), ("qsz", hh // 4), ("qsm", hh % 2, t), "kcx%d" % g], [pk])
                zc, zk = zcr.next()
                kb.v("dve", lambda: V.tensor_add(out=zc[:], in0=pb[:, 0:127], in1=Gt[:, h, 120 - 8 * t:120 - 8 * t + 127]), [pk, "Gt"], [zk])
                pc, pck = pcr.next()
                kb.act(pc[:, 0:127], zc[:], AF.Exp, [zk], [pck])
                return pc, pck

            def cB(t, hh, pc, pck):
                pt, ptk = ptbank()
                kb.tr(pt[0:127, 0:128], pc[:, 0:127], ident[:], [pck, "ident"], [ptk])
                pT, pTk = pTr.next()
                kb.act(pT[0:127, :], pt[0:127, 0:128], AF.Copy, [ptk], [pTk])
                return pT, pTk

            def cC(t, hh, pT, pTk):
                h = 8 * g + hh
                if hh == 0:
                    octile[t] = ocr.next()
                oc, ock = octile[t]
                po, pok = PB[4 + hh // 4], PBK[4 + hh // 4]
                P4 = po[:].rearrange("p (i c) -> p i c", c=128)
                kb.mm(P4[:, hh % 4, 0:97], pT[0:127, :], vca[0:127, g, :], True, True, [pTk, "vca%d" % g], [pok], sgc=True)
                if hh % 4 == 3:
                    j0 = hh - 3
                    sm, smk = smr.next()
                    kb.v("dve", lambda: V.tensor_scalar_max(out=sm[:, 0:4], in0=P4[:, :, 96], scalar1=1e-30), [pok], [smk + "a"])
                    kb.v("dve", lambda: V.reciprocal(out=sm[:, 4:8], in_=sm[:, 0:4]), [smk + "a"], [smk + "b"])
                    ock4 = [(ock, x_) for x_ in range(j0, j0 + 4)]
                    kb.v("dve", lambda: V.tensor_tensor(out=oc[:, j0:j0 + 4, :], in0=P4[:, :, 0:96], in1=bcast_ap(sm[:, 4:8], [[1, 4], [0, 96]]), op=ALU.mult),
                         [pok, smk + "b"], ock4)
                    gl = gates[:, t, 3 * (8 * g + j0):3 * (8 * g + j0) + 1]
                    kb.v("pool", lambda: G.tensor_tensor(out=og[:, t, 64 * j0:64 * j0 + 256].rearrange("p (i c) -> p i c", c=64), in0=oc[:, j0:j0 + 4, 0:64],
                                                         in1=bcast_ap(gl, [[3, 4], [0, 64]]), op=ALU.mult),
                         ock4 + ["gates"], [("og", t, x_) for x_ in range(j0, j0 + 4)])
                if hh == 7:
                    im, imk = impr.next()
                    kb.v("dve", lambda: V.tensor_reduce(out=im[:, 0, :], in_=oc[:, :, 64:96].rearrange("p h j -> p j h"), axis=AX.X, op=ALU.add),
                         [(ock, x_) for x_ in range(8)], [imk + "0"])
                    kb.v("dve", lambda: V.tensor_mul(out=im[:, 1, :], in0=im[:, 0, :], in1=keep[:, t, :]), [imk + "0", "keep"], [imk + "1"])
                    kb.v("dve", lambda: V.tensor_add(out=im[:, 2, :], in0=im[:, 1, :], in1=addm[:, t, :]), [imk + "1", "addm"], [imk + "2"])
                    sm2, smk2 = smr.next()
                    kb.v("dve", lambda: V.max(out=sm2[:, 0:8], in_=im[:, 2, :]), [imk + "2"], [smk2 + "a"])
                    kb.v("dve", lambda: V.tensor_scalar(out=im[:, 3, :], in0=im[:, 2, :], scalar1=sm2[:, 7:8], scalar2=None, op0=ALU.is_ge),
                         [imk + "2", smk2 + "a"], [imk + "3"])
                    ng, ngk = ngr.next()
                    kb.v("dve", lambda: V.tensor_scalar(out=ng[:, 0:32], in0=im[:, 3, :], scalar1=-1.0, scalar2=NEGM, op0=ALU.add, op1=ALU.mult),
                         [imk + "3"], [ngk])
                    pt, ptk = ptbank()
                    kb.tr(pt[0:64, 0:128], ng[:], ident[:], [ngk, "ident"], [ptk])
                    for par in range(2):
                        oth = slice(64 * (1 - par), 64 * (1 - par) + 64)
                        kb.act(bcast_ap(qs[oth, par, 128 * t:128 * t + 128], [[2 * T, 4], [1, 128]]),
                               bcast_ap(pt[0:64, 0:128], [[0, 4], [1, 128]]), AF.Copy,
                               [ptk, ("qsz", 0), ("qsz", 1)], [("qsm", par, t)])

            nci = len(citems)
            hA = {}
            hB = {}
            for n in range(nci + 2):
                if n < nci:
                    hA[n] = cA(*citems[n])
                if 1 <= n <= nci:
                    hB[n - 1] = cB(*citems[n - 1], *hA.pop(n - 1))
                if 2 <= n:
                    cC(*citems[n - 2], *hB.pop(n - 2))
            if stop_after == "nsa2":
                raise _Stop()
            LOOK = 2
            STB, STK = PB[0:4], PBK[0:4]
            ACB, ACK = PB[4:6], PBK[4:6]
            tmpr = Rot(kb, "fin", [128, 4, 64], F32, 2)
            units = [(hh, qb, br) for hh in range(8) for qb in range(4) for br in range(2)]
            items = []
            for ui, (hh, qb, br) in enumerate(units):
                kt_lo = 0 if br == 0 else max(0, 4 * qb - 4)
                for kt in range(kt_lo, 4 * qb + 4):
                    items.append((ui, hh, qb, br, kt, kt == kt_lo, kt == 4 * qb + 3))

            def sQK(n, item):
                ui, hh, qb, br, kt, isfirst, islast = item
                h = 8 * g + hh
                pr = slice(64 * (hh % 2), 64 * (hh % 2) + 64)
                c = hh // 2
                kx_, kxk = (ksx, "ksx") if br == 0 else (kwx, "kwx")
                st_, stk_ = STB[n % 4], STK[n % 4]
                par = hh % 2
                vi = [i for i in range(4) if 0 <= 4 * qb + i - kt and not (br == 1 and 4 * qb + i - kt > 4)]
                c0_, c1_ = 128 * vi[0], 128 * vi[-1] + 128
                kb.mm(st_[:, c0_:c1_], kx_[:, par, 128 * kt:128 * kt + 128], qs[:, hh, 512 * qb + c0_:512 * qb + c1_], True, True,
                      [kxk, ("qsq", hh), ("qsz", hh // 4)] + [("qsm", par, 4 * qb + i) for i in vi], [stk_])
                PTt, PTk = PTr.next()
                kb.act(PTt[:, c0_:c1_], st_[:, c0_:c1_], AF.Exp, [stk_], [(PTk, i) for i in range(4)])
                for i in range(4):
                    off = 4 * qb + i - kt
                    if off < 0 or (br == 1 and off > 4):
                        continue
                    sl = slice(128 * i, 128 * i + 128)
                    if off == 0:
                        kb.v("dve", lambda: V.tensor_mul(out=PTt[:, sl], in0=PTt[:, sl], in1=E0[:, h, :]), [(PTk, i), "E0"], [(PTk, i)])
                    elif off == 1:
                        kb.v("pool", lambda: G.tensor_mul(out=PTt[:, sl], in0=PTt[:, sl], in1=E1[:, h, :]), [(PTk, i), "E1"], [(PTk, i)])
                    elif off == 4 and br == 1:
                        kb.v("pool", lambda: G.tensor_mul(out=PTt[:, sl], in0=PTt[:, sl], in1=wedge[:]), [(PTk, i), "wedge"], [(PTk, i)])
                return PTt, PTk

            def sPV(item, PTt, PTk):
                ui, hh, qb, br, kt, isfirst, islast = item
                h = 8 * g + hh
                va, vak = (vsa, "vsa") if br == 0 else (vwa, "vwa")
                accb, acck = ACB[ui % 2], ACK[ui % 2]
                A = accb[:].rearrange("p (i c) -> p i c", c=128)
                firstmm = isfirst
                for i in range(4):
                    off = 4 * qb + i - kt
                    if off < 0 or (br == 1 and off > 4):
                        continue
                    sl = slice(128 * i, 128 * i + 128)
                    kb.mm(A[:, i, 0:65], PTt[:, sl], va[:, kt, :], firstmm, kt == 4 * qb + i, [(PTk, i), vak], [acck], sgc=True)
                    firstmm = False
                if islast:
                    sm, smk = smr.next()
                    kb.v("dve", lambda: V.tensor_scalar_max(out=sm[:, 0:4], in0=A[:, :, 64], scalar1=1e-30), [acck], [smk + "a"])
                    kb.v("dve", lambda: V.reciprocal(out=sm[:, 4:8], in_=sm[:, 0:4]), [smk + "a"], [smk + "b"])
                    kb.v("dve", lambda: V.tensor_mul(out=sm[:, 8:12], in0=sm[:, 4:8], in1=gates[:, 4 * qb:4 * qb + 4, 3 * h + 1 + br]),
                         [smk + "b", "gates"], [smk + "c"])
                    tm, tmk = tmpr.next()
                    kb.v("dve", lambda: V.tensor_tensor(out=tm[:], in0=A[:, :, 0:64], in1=bcast_ap(sm[:, 8:12], [[1, 4], [0, 64]]), op=ALU.mult),
                         [acck, smk + "c"], [tmk])
                    ogk = [("og", 4 * qb + i, hh) for i in range(4)]
                    kb.v("pool", lambda: G.tensor_add(out=og[:, 4 * qb:4 * qb + 4, 64 * hh:64 * hh + 64],
                                                      in0=og[:, 4 * qb:4 * qb + 4, 64 * hh:64 * hh + 64], in1=tm[:]), ogk + [tmk], ogk)

            nit = len(items)
            hq = {}
            for n in range(nit + LOOK):
                if n < nit:
                    hq[n] = sQK(n, items[n])
                if n >= LOOK:
                    sPV(items[n - LOOK], *hq.pop(n - LOOK))
            obr = Rot(kb, "ob", [128, 512], BF16, 2)
            oTr = Rot(kb, "oT", [128, 4, 128], BF16, 2)
            for t in range(NT):
                ob, obk = obr.next()
                kb.act(ob[:], og[:, t, :], AF.Copy, [("og", t, hh) for hh in range(8)], [obk])
                pt, ptk = ptbank()
                for e in range(4):
                    kb.tr(pt[:, 128 * e:128 * e + 128], ob[:, 128 * e:128 * e + 128], ident[:], [obk, "ident"], [ptk])
                oT, oTk = oTr.next()
                kb.act(oT[:].rearrange("p e n -> p (e n)"), pt[:, 0:512], AF.Copy, [ptk], [oTk])
                kb.dma(ON_T[512 * g:512 * g + 512, 128 * t:128 * t + 128].rearrange("(e p) n -> p e n", p=128), oT[:], [oTk],
                       [("ON_T", g, t)], q="pool")
            kb.release(mk3)
        kb.release(mk)

    prr2 = [0]

    def stage_merge(li):
        mk = kb.mark()
        mT = kb.sb("mT", [128, 8, T], BF16)
        mk2 = kb.mark()
        wor = kb.sb("wor", [128, 16, D], BF16)
        won = kb.sb("won", [128, 8, D], BF16)
        for k4 in range(4):
            kb.dma(wor[:, 4 * k4:4 * k4 + 4, :], wb["w_o_ret"][2048 * li + 512 * k4:2048 * li + 512 * k4 + 512, :].rearrange("(k p) c -> p k c", p=128),
                   [("wb", "w_o_ret")], ["wor"])
        for k4 in range(2):
            kb.dma(won[:, 4 * k4:4 * k4 + 4, :], wb["w_o_nsa"][D * li + 512 * k4:D * li + 512 * k4 + 512, :].rearrange("(k p) c -> p k c", p=128),
                   [("wb", "w_o_nsa")], ["won"])
        zTr = Rot(kb, "zTl", [128, 16, 128], BF16, 2)
        oTr = Rot(kb, "oTl", [128, 8, 128], BF16, 2)
        sar = Rot(kb, "sa", [128, D], BF16, 2)
        sbr = Rot(kb, "sb", [128, D], BF16, 2)
        t1r = Rot(kb, "t1", [128, 512], F32, 2)
        t2r = Rot(kb, "t2", [128, 512], F32, 2)
        mbr = Rot(kb, "mb", [128, D], BF16, 2)
        for t in range(NT):
            cols = slice(128 * t, 128 * t + 128)
            zT, zk = zTr.next(); oT, ok = oTr.next(); sa, sak = sar.next(); sb_, sbk = sbr.next()
            kb.dma(zT[:], Z_T[:, cols].rearrange("(k p) n -> p k n", p=128), [("Z_T", t, h_) for h_ in range(4)], [zk])
            kb.dma(oT[:], ON_T[:, cols].rearrange("(k p) n -> p k n", p=128), [("ON_T", 0, t), ("ON_T", 1, t)], [ok])
            kb.dma(sa[:], P_ma[cols, :], [("P_ma", 0, t), ("P_ma", 1, t)], [sak])
            kb.dma(sb_[:], P_mb[cols, :], [("P_mb", 0, t), ("P_mb", 1, t)], [sbk])
            mb, mbk = mbr.next()
            for half in range(2):
                hs = slice(512 * half, 512 * half + 512)
                pr_, prk = pbank()
                for k in range(16):
                    kb.mm(pr_[:], zT[:, k, :], wor[:, k, hs], k == 0, k == 15, [zk, "wor"], [prk])
                pn, pnk = pbank()
                for k in range(8):
                    kb.mm(pn[:], oT[:, k, :], won[:, k, hs], k == 0, k == 7, [ok, "won"], [pnk])
                t1, t1k = t1r.next(); t2, t2k = t2r.next()
                kb.v("dve", lambda: V.tensor_mul(out=t1[:], in0=pr_[:], in1=sa[:, hs]), [prk, sak], [t1k])
                kb.v("dve", lambda: V.tensor_mul(out=t2[:], in0=pn[:], in1=sb_[:, hs]), [pnk, sbk], [t2k])
                kb.v("pool", lambda: G.tensor_add(out=mb[:, hs], in0=t1[:], in1=t2[:]), [t1k, t2k], [(mbk, half)])
            pt, ptk = ptbank()
            for k in range(8):
                kb.tr(pt[:, 128 * k:128 * k + 128], mb[:, 128 * k:128 * k + 128], ident[:], [(mbk, 0), (mbk, 1), "ident"], [ptk])
            kb.act(mT[:, :, cols], pt[:].rearrange("p (k n) -> p k n", k=8), AF.Copy, [ptk], [("mT", t)])
        kb.release(mk2)
        wo = kb.sb("wo", [128, 8, D], BF16)
        for k4 in range(2):
            kb.dma(wo[:, 4 * k4:4 * k4 + 4, :], wb["w_out"][D * li + 512 * k4:D * li + 512 * k4 + 512, :].rearrange("(k p) c -> p k c", p=128),
                   [("wb", "w_out")], ["wo"])
        for t in range(NT):
            for half in range(2):
                hs = slice(512 * half, 512 * half + 512)
                pb, pk = pbank()
                for k in range(8):
                    kb.mm(pb[:], mT[:, k, 128 * t:128 * t + 128], wo[:, k, hs], k == 0, k == 7, [("mT", t), "wo"], [pk])
                kb.v("dve", lambda: V.tensor_add(out=x_sb[:, t, hs], in0=x_sb[:, t, hs], in1=pb[:]), [("x", t), pk], [("x", t)])
        kb.release(mk)

    def stage_ffn(li, hT):
        mk = kb.mark()
        wpool = Rot(kb, "wf", [128, 8, 256], BF16, 3)
        sar = Rot(kb, "fsa", [128, 512], F32, 2)
        ur = Rot(kb, "fu", [128, 512], BF16, 3)
        for j in range(22):
            wt, wk = wpool.next()
            kb.dma(wt[:, :, 0:128], wb["w_ffn_in"][D * li:D * li + D, 128 * j:128 * j + 128].rearrange("(k p) c -> p k c", p=128),
                   [("wb", "w_ffn_in")], [wk])
            kb.dma(wt[:, :, 128:256], wb["w_ffn_in"][D * li:D * li + D, FFN + 128 * j:FFN + 128 * j + 128].rearrange("(k p) c -> p k c", p=128),
                   [("wb", "w_ffn_in")], [wk])
            for jb in range(4):
                pa, pak = gemm_fm(hT, wt, wk, jb, 128, 0)
                pb, pbk = gemm_fm(hT, wt, wk, jb, 128, 128)
                sa, sak = sar.next()
                kb.act(sa[:], pa[:], AF.Silu, [pak], [sak])
                u, uk = ur.next()
                kb.v("dve", lambda: V.tensor_mul(out=u[:], in0=pb[:], in1=sa[:]), [pbk, sak], [uk])
                kb.dma(U_T[128 * j:128 * j + 128, 512 * jb:512 * jb + 512], u[:], [uk], [("U_T", j, jb)], q="pool")
        kb.release(mk)
        mk = kb.mark()
        wfo = kb.sb("wfo", [128, 22, D], BF16)
        for k0 in range(0, 22, 4):
            k1 = min(22, k0 + 4)
            kb.dma(wfo[:, k0:k1, :], wb["w_ffn_out"][FFN * li + 128 * k0:FFN * li + 128 * k1, :].rearrange("(k p) c -> p k c", p=128),
                   [("wb", "w_ffn_out")], ["wfo"])
        uTr = Rot(kb, "uTl", [128, 22, 128], BF16, 2)
        for t in range(NT):
            uT, uk = uTr.next()
            kb.dma(uT[:], U_T[:, 128 * t:128 * t + 128].rearrange("(k p) n -> p k n", p=128), [("U_T", j, t // 4) for j in range(22)], [uk])
            for half in range(2):
                hs = slice(512 * half, 512 * half + 512)
                pb, pk = pbank()
                for k in range(22):
                    kb.mm(pb[:], uT[:, k, :], wfo[:, k, hs], k == 0, k == 21, [uk, "wfo"], [pk])
                kb.v("dve", lambda: V.tensor_add(out=x_sb[:, t, hs], in0=x_sb[:, t, hs], in1=pb[:]), [("x", t), pk], [("x", t)])
        kb.release(mk)

    def final_norm(s):
        mk = kb.mark()
        gbc = kb.sb("gbcf", [128, D], F32)
        kb.dma(gbc[:], bass.AP(tensor=norm_final_g.tensor, offset=0, ap=[[0, 128], [1, D]]), [], ["gbcf"])
        junk = Rot(kb, "junkf", [128, D], BF16, 2)
        ssr = Rot(kb, "ssf", [128, 4], F32, 3)
        yr = Rot(kb, "yo", [128, D], F32, 3)
        for t in range(NT):
            jt, jk = junk.next(); ss, sk = ssr.next(); yo, yk = yr.next()
            kb.act(jt[:], x_sb[:, t, :], AF.Square, [("x", t)], [jk, sk + "a"], accum_out=ss[:, 0:1])
            kb.act(ss[:, 1:2], ss[:, 0:1], AF.Sqrt, [sk + "a"], [sk + "b"], scale=1.0 / D, bias=1e-6)
            kb.v("dve", lambda: V.reciprocal(out=ss[:, 2:3], in_=ss[:, 1:2]), [sk + "b"], [sk + "c"])
            kb.v("dve", lambda: V.scalar_tensor_tensor(out=yo[:], in0=x_sb[:, t, :], scalar=ss[:, 2:3], in1=gbc[:],
                                                       op0=ALU.mult, op1=ALU.mult), [("x", t), sk + "c", "gbcf"], [yk])
            ev = kb.dma(y_d[s * T + 128 * t:s * T + 128 * t + 128, :], yo[:], [yk], [("y", s, t)], q="sp")
            out_events.append(ev)
        kb.release(mk)

    out_events = []

    for s in range(nseq):
        for t in range(NT):
            kb.dma(x_sb[:, t, :], x_d[s * T + 128 * t:s * T + 128 * t + 128, :], [], [("x", t)])
        for li in range(depth):
            mk = kb.mark()
            hT = kb.sb("hT", [128, 8, T], BF16)
            rmsnorm_to_hT(hT, bass.AP(tensor=norm_mix_g.tensor, offset=li * D, ap=[[0, 128], [1, D]]))
            if stop_after == "norm":
                break
            stage_proj(li, hT)
            kb.release(mk)
            if stop_after == "proj":
                break
            stage_retention(li)
            if stop_after == "ret":
                break
            try:
                stage_nsa(li)
            except _Stop:
                break
            if stop_after == "nsa":
                break
            stage_merge(li)
            if stop_after == "merge":
                break
            mk = kb.mark()
            hT = kb.sb("hT2", [128, 8, T], BF16)
            rmsnorm_to_hT(hT, bass.AP(tensor=norm_ffn_g.tensor, offset=li * D, ap=[[0, 128], [1, D]]))
            stage_ffn(li, hT)
            kb.release(mk)
            kb.prune()
        final_norm(s)

    sp = nc.sync
    for sname, sd in kb.streams.items():
        if sd["count"] > 0:
            sp.wait_ge(sd["sem"], sd["count"] * sd["inc"])
    kb.release(0)
    return nc, kb


_CACHE = {}


def _host_inputs(inputs):
    C = _consts()
    f = lambda a: np.ascontiguousarray(np.asarray(a, dtype=np.float32))
    m = {
        "w_in": f(inputs["w_in"]).reshape(DEPTH * D, CIN),
        "cmp_w1_k": f(inputs["cmp_w1_k"]).reshape(DEPTH * 2048, 256),
        "cmp_w2_k": f(inputs["cmp_w2_k"]).reshape(DEPTH * 256, 64),
        "cmp_w1_v": f(inputs["cmp_w1_v"]).reshape(DEPTH * 2048, 256),
        "cmp_w2_v": f(inputs["cmp_w2_v"]).reshape(DEPTH * 256, 64),
        "w_o_ret": f(inputs["w_o_ret"]).reshape(DEPTH * 2048, D),
        "w_o_nsa": f(inputs["w_o_nsa"]).reshape(DEPTH * D, D),
        "w_out": f(inputs["w_out"]).reshape(DEPTH * D, D),
        "w_ffn_in": f(inputs["w_ffn_in"]).reshape(DEPTH * D, 2 * FFN),
        "w_ffn_out": f(inputs["w_ffn_out"]).reshape(DEPTH * FFN, D),
        "norm_mix_g": f(inputs["norm_mix_g"]),
        "norm_ffn_g": f(inputs["norm_ffn_g"]),
        "norm_final_g": f(inputs["norm_final_g"]).reshape(1, D),
        "cmp_pos_k": f(inputs["cmp_pos_k"]).reshape(DEPTH * 32, 64),
        "cmp_pos_v": f(inputs["cmp_pos_v"]).reshape(DEPTH * 32, 64),
        "rel_bias": f(inputs["rel_bias"]).reshape(1, 512),
        "c_cos": C["cosT"], "c_sin": C["sinT"], "c_ident": C["ident"],
        "c_dmask": C["dmaskT"].reshape(128, 512), "c_retsc": C["retsc"],
        "c_dist0": C["dist0"], "c_dist1": C["dist1"], "c_distg": C["distg"],
        "c_wedge": C["wedge"], "c_expand": C["expand"], "c_overlap": C["overlap"],
        "c_keep": C["keep"].reshape(128, 512), "c_addm": C["addm"].reshape(128, 512),
    }
    return m


def kernel(**inputs):
    x = np.ascontiguousarray(np.asarray(inputs["x"], dtype=np.float32))
    B = x.shape[0]
    if "nc" not in _CACHE:
        _CACHE["nc"] = build()[0]
    nc = _CACHE["nc"]
    shared = _host_inputs(inputs)
    per = B // NCORES
    in_maps = []
    for c in range(NCORES):
        m = dict(shared)
        m["x"] = x[c * per:(c + 1) * per].reshape(per * T, D)
        in_maps.append(m)
    res = run_bass_kernel_spmd(nc, in_maps, core_ids=list(range(NCORES)))
    out = np.concatenate([r["y"].reshape(per, T, D) for r in res.results], axis=0)
    return out.astype(np.float32)
```

```python
import math
import numpy as np
import ml_dtypes
import concourse.bass as bass
import concourse.mybir as mybir
from concourse.bass_utils import run_bass_kernel_spmd

F32 = mybir.dt.float32
BF16 = mybir.dt.bfloat16
AF = mybir.ActivationFunctionType
ALU = mybir.AluOpType
AX = mybir.AxisListType

T = 2048
D = 1024
NT = 16
DEPTH = 2
CIN = 10032
NSEQ = 4
NCORES = 8
FFN = 2816
NEGM = 30000.0
O_QR, O_KR, O_VR, O_GR, O_QN, O_KV, O_GATE, O_MA, O_MB = 0, 1024, 2048, 4096, 6144, 7168, 7936, 7984, 9008


def _t5_bucket_np(dist):
    dist = np.maximum(dist, 0)
    d32 = np.maximum(dist, 1).astype(np.float32)
    large = 16 + (np.log(d32 / np.float32(16)) / np.float32(math.log(128 / 16)) * np.float32(16)).astype(np.int32)
    large = np.minimum(large, 31)
    return np.where(dist < 16, dist, large)


def _consts():
    c = {}
    half = 128
    freqs = (10000.0 ** (-np.arange(half, dtype=np.float32) / half)).astype(np.float32)
    ang = (np.arange(T, dtype=np.float32)[None, :] * freqs[:, None]).astype(np.float32)
    c["cosT"] = np.cos(ang).astype(np.float32)
    c["sinT"] = np.sin(ang).astype(np.float32)
    c["ident"] = np.eye(128, dtype=np.float32).astype(ml_dtypes.bfloat16)
    lg = np.log(1.0 - 2.0 ** (-5.0 - np.arange(4, dtype=np.float64)))
    m = np.arange(128, dtype=np.float64)
    dm = np.zeros((128, 4, 128), np.float32)
    for h in range(4):
        dm[:, h, :] = (np.exp(-(m + 1.0) * lg[h])[:, None] * (m[None, :] >= m[:, None])).astype(np.float32)
    c["dmaskT"] = dm
    rs = np.zeros((128, 12), np.float32)
    for h in range(4):
        rs[:, h] = np.exp((m + 1.0) * lg[h])
        rs[:, 4 + h] = np.exp((127.0 - m) * lg[h])
    c["retsc"] = rs
    c["gchunk"] = [float(np.exp(128.0 * lg[h])) for h in range(4)]
    bk = _t5_bucket_np(np.arange(0, 4096))
    c["thr"] = [float(np.argmax(bk >= b)) for b in range(32)]
    r = np.arange(128, dtype=np.float32)[:, None]
    cc = np.arange(128, dtype=np.float32)[None, :]
    c["dist0"] = (cc - r).astype(np.float32)
    c["dist1"] = (cc - r + 128).astype(np.float32)
    cg = np.arange(247, dtype=np.float32)[None, :]
    c["distg"] = (r - 16.0 * (cg - 120.0) - 31.0).astype(np.float32)
    c["wedge"] = (cc < r).astype(np.float32).astype(ml_dtypes.bfloat16)
    ex = np.zeros((64, T), np.float32)
    for j in range(32):
        ex[j, 64 * j:64 * j + 64] = 1.0
    c["expand"] = ex.astype(ml_dtypes.bfloat16)
    cmp_end = np.arange(127) * 16 + 31
    sel_start = np.arange(32) * 64
    ov = ((cmp_end[:, None] - 31 < sel_start[None] + 64) & (cmp_end[:, None] >= sel_start[None]))
    c["overlap"] = ov.astype(np.float32).astype(ml_dtypes.bfloat16)
    keep = np.zeros((128, 16, 32), np.float32)
    add = np.zeros((128, 16, 32), np.float32)
    jj = np.arange(32)
    for t in range(16):
        pos = 128 * t + np.arange(128)
        cur = pos // 64
        forced = (jj[None] == 0) | (jj[None] == cur[:, None]) | (jj[None] == cur[:, None] - 1)
        valid = sel_start[None] <= pos[:, None]
        keep[:, t, :] = (valid & ~forced)
        add[:, t, :] = np.where(valid, np.where(forced, 1e4, 0.0), -1e30)
    c["keep"] = keep
    c["addm"] = add
    return c


NO_SAME_ENGINE_SYNC = False


class KB:
    def __init__(self):
        self.nc = bass.Bass("TRN2", target_bir_lowering=False)
        nc = self.nc
        self.ctx = []
        self.eng = {"pe": nc.tensor, "act": nc.scalar, "dve": nc.vector, "pool": nc.gpsimd, "sp": nc.sync}
        self.streams = {}
        for e in self.eng:
            self.new_stream(e, e, 1)
        self.NDQ = 16
        self.dq = {}
        self.dqi = {}
        for q in ("sp", "pool", "act"):
            self.dq[q] = []
            self.dqi[q] = 0
            for j in range(self.NDQ if q != "pool" else 6):
                nm = "d%s%d" % (q, j)
                self.new_stream(nm, q, 16)
                self.dq[q].append(nm)
        self.clock = {e: {} for e in self.eng}
        self.evclock = {}
        self.state = {}
        self.nwaits = 0
        self.ninst = 0
        self._uid = 0

    def enter(self, cm):
        v = cm.__enter__()
        self.ctx.append(cm)
        return v

    def mark(self):
        return len(self.ctx)

    def release(self, mark):
        if len(self.ctx) > mark and hasattr(self, "clock"):
            self.barrier()
        while len(self.ctx) > mark:
            self.ctx.pop().__exit__(None, None, None)

    def barrier(self):
        for en, e in self.eng.items():
            clk = self.clock[en]
            for sname, sd in self.streams.items():
                if sd["count"] > clk.get(sname, 0):
                    e.wait_ge(sd["sem"], sd["count"] * sd["inc"])
                    clk[sname] = sd["count"]
                    self.nwaits += 1

    def new_stream(self, name, eng, inc):
        sem = self.enter(self.nc.semaphore("sem_" + name))
        self.streams[name] = dict(sem=sem, count=0, inc=inc, eng=eng)

    def sb(self, name, shape, dtype):
        self._uid += 1
        return self.enter(self.nc.sbuf_tensor("%s_%d" % (name, self._uid), list(shape), dtype))

    def ps(self, name, shape, dtype):
        self._uid += 1
        return self.enter(self.nc.psum_tensor("%s_%d" % (name, self._uid), list(shape), dtype))

    def dram(self, name, shape, dtype, kind="Internal"):
        return self.nc.dram_tensor(name, list(shape), dtype, kind=kind).ap()

    def op(self, eng, fn, reads=(), writes=(), stream=None, nosame=False, extra=()):
        st = self.state
        need = {}
        for w in extra:
            if w[1] > 0 and need.get(w[0], 0) < w[1]:
                need[w[0]] = w[1]
        for k in reads:
            s = st.get(k)
            if s and s[0]:
                w = s[0]
                if need.get(w[0], 0) < w[1]:
                    need[w[0]] = w[1]
        for k in writes:
            s = st.get(k)
            if s:
                if s[0]:
                    w = s[0]
                    if need.get(w[0], 0) < w[1]:
                        need[w[0]] = w[1]
                for w in s[1]:
                    if need.get(w[0], 0) < w[1]:
                        need[w[0]] = w[1]
        clk = self.clock[eng]
        e = self.eng[eng]
        for sname, idx in need.items():
            if (nosame or NO_SAME_ENGINE_SYNC) and sname == eng:
                continue
            if clk.get(sname, 0) >= idx:
                continue
            sd = self.streams[sname]
            e.wait_ge(sd["sem"], idx * sd["inc"])
            self.nwaits += 1
            ev = self.evclock.get((sname, idx))
            clk[sname] = idx
            if ev:
                for k2, v2 in ev.items():
                    if clk.get(k2, 0) < v2:
                        clk[k2] = v2
        inst = fn()
        sname = stream or eng
        sd = self.streams[sname]
        sd["count"] += 1
        idx = sd["count"]
        inst.then_inc(sd["sem"], sd["inc"])
        self.ninst += 1
        snap = dict(clk)
        self.evclock[(sname, idx)] = snap
        evt = (sname, idx)
        for k in reads:
            s = st.get(k)
            if s is None:
                st[k] = [None, [evt]]
            else:
                s[1].append(evt)
        for k in writes:
            st[k] = [evt, []]
        return evt

    def prune(self):
        if len(self.evclock) > 400000:
            keep = {}
            for k, s in self.state.items():
                if s[0]:
                    keep[s[0]] = self.evclock.get(s[0])
                for w in s[1]:
                    keep[w] = self.evclock.get(w)
            self.evclock = {k: v for k, v in keep.items() if v is not None}

    def dma(self, out, in_, reads, writes, q="sp"):
        eng = q
        j = self.dqi[q]
        self.dqi[q] = (j + 1) % len(self.dq[q])
        stream = self.dq[q][j]
        e = self.eng[eng]
        prev = (stream, self.streams[stream]["count"])
        return self.op(eng, lambda: e.dma_start(out=out, in_=in_), reads, writes, stream=stream, extra=[prev])

    def mm(self, out, lhsT, rhs, start, stop, reads, writes, sgc=False):
        pe = self.nc.tensor
        if sgc:
            return self.op("pe", lambda: pe.matmul(out, lhsT, rhs, start=start, stop=stop, skip_group_check=True),
                           reads, writes, nosame=True)
        return self.op("pe", lambda: pe.matmul(out, lhsT, rhs, start=start, stop=stop), reads, writes, nosame=True)

    def tr(self, out, in_, ident, reads, writes):
        pe = self.nc.tensor
        return self.op("pe", lambda: pe.transpose(out, in_, ident), reads, writes, nosame=True)

    def act(self, out, in_, func, reads, writes, scale=1.0, bias=0.0, accum_out=None):
        a = self.nc.scalar
        if accum_out is not None:
            return self.op("act", lambda: a.activation(out=out, in_=in_, func=func, bias=bias, scale=scale,
                                                       accum_out=accum_out), reads, writes)
        return self.op("act", lambda: a.activation(out=out, in_=in_, func=func, bias=bias, scale=scale), reads, writes)

    def v(self, eng, fn, reads, writes):
        return self.op(eng, fn, reads, writes)


class _Stop(Exception):
    pass


class Rot:
    def __init__(self, kb, name, shape, dtype, n, psum=False):
        self.tiles = [(kb.ps if psum else kb.sb)(name, shape, dtype) for _ in range(n)]
        self.keys = ["%s#%d#%d" % (name, id(self) % 100000, i) for i in range(n)]
        self.i = 0

    def next(self):
        t, k = self.tiles[self.i], self.keys[self.i]
        self.i = (self.i + 1) % len(self.tiles)
        return t, k


def bcast_ap(ap, dims):
    pa = ap.ap
    return bass.AP(tensor=ap.tensor, offset=ap.offset, ap=[[pa[0][0], pa[0][1]]] + [list(d) for d in dims])


def build(nseq=NSEQ, depth=DEPTH, debug=False, stop_after=None):
    C = _consts()
    kb = KB()
    nc = kb.nc
    V = nc.vector
    G = nc.gpsimd
    dk = "ExternalOutput" if debug else "Internal"

    def dbg(name, ap, shape, dtype, keys):
        if not debug:
            return
        d = nc.dram_tensor("dbg_" + name, list(shape), dtype, kind="ExternalOutput").ap()
        kb.dma(d, ap, keys, [("dbg", name)])

    def din(name, shape, dtype=F32):
        return nc.dram_tensor(name, list(shape), dtype, kind="ExternalInput").ap()

    x_d = din("x", [nseq * T, D])
    y_d = nc.dram_tensor("y", [nseq * T, D], F32, kind="ExternalOutput").ap()
    w_f32 = {
        "w_in": din("w_in", [DEPTH * D, CIN]),
        "cmp_w1_k": din("cmp_w1_k", [DEPTH * 2048, 256]),
        "cmp_w2_k": din("cmp_w2_k", [DEPTH * 256, 64]),
        "cmp_w1_v": din("cmp_w1_v", [DEPTH * 2048, 256]),
        "cmp_w2_v": din("cmp_w2_v", [DEPTH * 256, 64]),
        "w_o_ret": din("w_o_ret", [DEPTH * 2048, D]),
        "w_o_nsa": din("w_o_nsa", [DEPTH * D, D]),
        "w_out": din("w_out", [DEPTH * D, D]),
        "w_ffn_in": din("w_ffn_in", [DEPTH * D, 2 * FFN]),
        "w_ffn_out": din("w_ffn_out", [DEPTH * FFN, D]),
    }
    norm_mix_g = din("norm_mix_g", [DEPTH, D])
    norm_ffn_g = din("norm_ffn_g", [DEPTH, D])
    norm_final_g = din("norm_final_g", [1, D])
    cmp_pos_k = din("cmp_pos_k", [DEPTH * 32, 64])
    cmp_pos_v = din("cmp_pos_v", [DEPTH * 32, 64])
    rel_bias = din("rel_bias", [1, 512])
    c_cos = din("c_cos", [128, T]); c_sin = din("c_sin", [128, T])
    c_ident = din("c_ident", [128, 128], BF16)
    c_dmask = din("c_dmask", [128, 512]); c_retsc = din("c_retsc", [128, 12])
    c_dist0 = din("c_dist0", [128, 128]); c_dist1 = din("c_dist1", [128, 128]); c_distg = din("c_distg", [128, 247])
    c_wedge = din("c_wedge", [128, 128], BF16)
    c_expand = din("c_expand", [64, T], BF16)
    c_overlap = din("c_overlap", [127, 32], BF16)
    c_keep = din("c_keep", [128, 512]); c_addm = din("c_addm", [128, 512])

    wb = {k: kb.dram("wb_" + k, v.shape, BF16) for k, v in w_f32.items()}
    P_qrT = kb.dram("P_qrT", [1024, T], BF16, dk)
    P_krT = kb.dram("P_krT", [1024, T], BF16, dk)
    P_kz = kb.dram("P_kz", [T, 1024], BF16, dk)
    P_v = kb.dram("P_v", [T, 2048], BF16, dk)
    P_sg = kb.dram("P_sg", [T, 2048], BF16, dk)
    P_qnT = kb.dram("P_qnT", [1024, T], BF16, dk)
    P_kcT = kb.dram("P_kcT", [128, T], BF16, dk)
    P_vcT = kb.dram("P_vcT", [128, T], BF16, dk)
    P_ksT = kb.dram("P_ksT", [256, T], BF16, dk)
    P_kwT = kb.dram("P_kwT", [256, T], BF16, dk)
    P_vs = kb.dram("P_vs", [T, 128], BF16, dk)
    P_vw = kb.dram("P_vw", [T, 128], BF16, dk)
    P_gate = kb.dram("P_gate", [T, 48], F32, dk)
    P_ma = kb.dram("P_ma", [T, 1024], BF16, dk)
    P_mb = kb.dram("P_mb", [T, 1024], BF16, dk)
    Z_T = kb.dram("Z_T", [2048, T], BF16, dk)
    ON_T = kb.dram("ON_T", [1024, T], BF16, dk)
    U_T = kb.dram("U_T", [FFN, T], BF16, dk)

    for k, src in w_f32.items():
        rows = src.shape[0]
        step = 256
        for r0 in range(0, rows, step):
            r1 = min(rows, r0 + step)
            kb.dma(wb[k][r0:r1, :], src[r0:r1, :], reads=[], writes=[("wb", k)], q="pool")

    x_sb = kb.sb("x", [128, NT, D], F32)
    ident = kb.sb("ident", [128, 128], BF16)
    kb.dma(ident[:], c_ident, [], ["ident"])
    PB = [kb.ps("pb", [128, 512], F32) for _ in range(6)]
    PBK = ["pb%d" % i for i in range(6)]
    PT2 = [kb.ps("pt", [128, 1024], BF16) for _ in range(2)]
    PTK = ["pt0", "pt1"]
    prr = [0]
    ptr = [0]

    def pbank():
        i = prr[0]; prr[0] = (i + 1) % 6
        return PB[i], PBK[i]

    def ptbank():
        i = ptr[0]; ptr[0] = (i + 1) % 2
        return PT2[i], PTK[i]

    Gt = kb.sb("Gt", [128, 16, 247], BF16)
    E0 = kb.sb("E0", [128, 16, 128], BF16)
    E1 = kb.sb("E1", [128, 16, 128], BF16)
    wedge = kb.sb("wedge", [128, 128], BF16)
    kb.dma(wedge[:], c_wedge, [], ["wedge"])

    def build_tables():
        mk = kb.mark()
        relb = kb.sb("relb", [128, 32, 16], F32)
        kb.dma(relb[:].rearrange("p b h -> p (b h)"),
               bass.AP(tensor=rel_bias.tensor, offset=0, ap=[[0, 128], [1, 512]]), [], ["relb"])
        dl = kb.sb("dl", [128, 32, 16], F32)
        kb.v("dve", lambda: V.tensor_sub(out=dl[:, 1:32, :], in0=relb[:, 1:32, :], in1=relb[:, 0:31, :]), ["relb"], ["dl"])
        kb.v("dve", lambda: V.tensor_sub(out=dl[:, 0:1, :], in0=relb[:, 0:1, :], in1=relb[:, 31:32, :]), ["relb"], ["dl"])
        W = 503
        dist = kb.sb("dist", [128, W], F32)
        kb.dma(dist[:, 0:128], c_dist0, [], ["dist"])
        kb.dma(dist[:, 128:256], c_dist1, [], ["dist"])
        kb.dma(dist[:, 256:503], c_distg, [], ["dist"])
        acc = kb.sb("acc", [128, 16, W], F32)
        tmps = Rot(kb, "tmp01", [128, W], F32, 3)
        ACCK = [("acc", h) for h in range(16)]
        kb.v("dve", lambda: V.tensor_copy(out=acc[:], in_=bcast_ap(dl[:, 0, :], [[1, 16], [0, W]])), ["dl"], ACCK)
        for b in range(1, 33):
            tmp, tk = tmps.next()
            if b < 32:
                thr = C["thr"][b]
                kb.v("dve", lambda: V.tensor_single_scalar(out=tmp[:], in_=dist[:], scalar=thr, op=ALU.is_ge), ["dist"], [tk])
                for h in range(16):
                    kb.v("dve", lambda: V.scalar_tensor_tensor(out=acc[:, h, :], in0=tmp[:], scalar=dl[:, b, h:h + 1], in1=acc[:, h, :],
                                                               op0=ALU.mult, op1=ALU.add), [tk, "dl", ("acc", h)], [("acc", h)])
            else:
                kb.v("dve", lambda: V.tensor_scalar(out=tmp[:], in0=dist[:], scalar1=0.0, scalar2=-NEGM, op0=ALU.is_lt, op1=ALU.mult),
                     ["dist"], [tk])
                kb.v("dve", lambda: V.tensor_add(out=acc[:], in0=acc[:], in1=bcast_ap(tmp[:], [[0, 16], [1, W]])), ACCK + [tk], ACCK)
        kb.act(E0[:], acc[:, :, 0:128], AF.Exp, ACCK, ["E0"])
        kb.act(E1[:], acc[:, :, 128:256], AF.Exp, ACCK, ["E1"])
        kb.v("dve", lambda: V.tensor_copy(out=Gt[:], in_=acc[:, :, 256:503]), ACCK, ["Gt"])
        kb.release(mk)

    build_tables()

    HT_ALL = [("hT", t) for t in range(NT)]
    dbg_once = [True]

    def rmsnorm_to_hT(hT, g_row_ap):
        mk = kb.mark()
        gbc = kb.sb("gbc", [128, D], F32)
        kb.dma(gbc[:], g_row_ap, [], ["gbc"])
        junk = Rot(kb, "junk", [128, D], BF16, 2)
        ssr = Rot(kb, "ss", [128, 4], F32, 4)
        hbr = Rot(kb, "hb", [128, D], BF16, 3)

        def n_p1(t):
            jt, jk = junk.next(); ss, sk = ssr.next(); hb, hk = hbr.next()
            kb.act(jt[:], x_sb[:, t, :], AF.Square, [("x", t)], [jk, sk + "a"], accum_out=ss[:, 0:1])
            kb.act(ss[:, 1:2], ss[:, 0:1], AF.Sqrt, [sk + "a"], [sk + "b"], scale=1.0 / D, bias=1e-6)
            kb.v("dve", lambda: V.reciprocal(out=ss[:, 2:3], in_=ss[:, 1:2]), [sk + "b"], [sk + "c"])
            kb.v("dve", lambda: V.scalar_tensor_tensor(out=hb[:], in0=x_sb[:, t, :], scalar=ss[:, 2:3], in1=gbc[:],
                                                       op0=ALU.mult, op1=ALU.mult), [("x", t), sk + "c", "gbc"], [hk])
            return hb, hk

        def n_p2(t, hb, hk):
            pt, pk = ptbank()
            for k in range(8):
                kb.tr(pt[:, 128 * k:128 * k + 128], hb[:, 128 * k:128 * k + 128], ident[:], [hk, "ident"], [pk])
            kb.act(hT[:, :, 128 * t:128 * t + 128], pt[:].rearrange("p (k n) -> p k n", k=8), AF.Copy, [pk], [("hT", t)])

        hs_ = {}
        for t in range(NT + 1):
            if t < NT:
                hs_[t] = n_p1(t)
            if t >= 1:
                n_p2(t - 1, *hs_.pop(t - 1))
        kb.release(mk)


    def load_w(pool, wname, row0, c0, ncols, dup64=None):
        wt, wk = pool.next()
        src = wb[wname]
        if dup64 is None:
            kb.dma(wt[:, :, 0:ncols], src[row0:row0 + 1024, c0:c0 + ncols].rearrange("(k p) c -> p k c", p=128),
                   [("wb", wname)], [wk])
        else:
            for hh in range(2):
                kb.dma(wt[:, :, 64 * hh:64 * hh + 64], src[row0:row0 + 1024, c0:c0 + 64].rearrange("(k p) c -> p k c", p=128),
                       [("wb", wname)], [wk])
        return wt, wk

    def gemm_fm(hT, wt, wk, jb, ncols=128, c0=0):
        pb, pk = pbank()
        for k in range(8):
            kb.mm(pb[0:ncols, :], wt[:, k, c0:c0 + ncols], hT[:, k, 512 * jb:512 * jb + 512], k == 0, k == 7,
                  [wk] + [("hT", 4 * jb + i) for i in range(4)], [pk])
        return pb, pk

    def gemm_tm(hT, wt, wk, t, ncols):
        pb, pk = pbank()
        for k in range(8):
            kb.mm(pb[:, 0:ncols], hT[:, k, 128 * t:128 * t + 128], wt[:, k, 0:ncols], k == 0, k == 7,
                  [wk, ("hT", t)], [pk])
        return pb, pk

    def stage_proj(li, hT):
        mk = kb.mark()
        row0 = li * D
        wpool = Rot(kb, "wt", [128, 8, 512], BF16, 3)
        cosT = kb.sb("cosT", [128, T], F32); sinT = kb.sb("sinT", [128, T], F32)
        kb.dma(cosT[:], c_cos, [], ["cosT"]); kb.dma(sinT[:], c_sin, [], ["sinT"])
        retsc = kb.sb("retsc", [128, 12], F32)
        kb.dma(retsc[:], c_retsc, [], ["retsc"])
        xab = Rot(kb, "xab", [128, 2, 512], F32, 2)
        tmpr = Rot(kb, "rtmp", [128, 4, 512], F32, 2)
        rotr = Rot(kb, "rot", [128, 2, 512], BF16, 2)
        kzr = Rot(kb, "kz", [128, 4, 256], BF16, 2)
        stg = Rot(kb, "stg", [128, 512], BF16, 4)
        stgf = Rot(kb, "stgf", [128, 48], F32, 2)
        flip = [0]

        for (isk, obase, dst) in ((0, O_QR, P_qrT), (1, O_KR, P_krT)):
            sc = 1.0 / 16.0 if isk else 1.0
            for h in range(4):
                wt, wk = load_w(wpool, "w_in", row0, obase + 256 * h, 256)
                for jb in range(4):
                    pa, pak = gemm_fm(hT, wt, wk, jb, 128, 0)
                    pbb, pbk = gemm_fm(hT, wt, wk, jb, 128, 128)
                    xa, xk = xab.next()
                    kb.act(xa[:, 0, :], pa[:], AF.Copy, [pak], [xk + "a"], scale=sc)
                    kb.act(xa[:, 1, :], pbb[:], AF.Copy, [pbk], [xk + "b"], scale=sc)
                    tm, tk = tmpr.next()
                    cs = cosT[:, 512 * jb:512 * jb + 512]; sn = sinT[:, 512 * jb:512 * jb + 512]
                    kb.v("dve", lambda: V.tensor_mul(out=tm[:, 0, :], in0=xa[:, 0, :], in1=cs), [xk + "a", "cosT"], [tk + "0"])
                    kb.v("pool", lambda: G.tensor_mul(out=tm[:, 1, :], in0=xa[:, 1, :], in1=sn), [xk + "b", "sinT"], [tk + "1"])
                    kb.v("dve", lambda: V.tensor_mul(out=tm[:, 2, :], in0=xa[:, 0, :], in1=sn), [xk + "a", "sinT"], [tk + "2"])
                    kb.v("pool", lambda: G.tensor_mul(out=tm[:, 3, :], in0=xa[:, 1, :], in1=cs), [xk + "b", "cosT"], [tk + "3"])
                    ro, rk = rotr.next()
                    kb.v("dve", lambda: V.tensor_sub(out=ro[:, 0, :], in0=tm[:, 0, :], in1=tm[:, 1, :]), [tk + "0", tk + "1"], [rk + "a"])
                    kb.v("pool", lambda: G.tensor_add(out=ro[:, 1, :], in0=tm[:, 2, :], in1=tm[:, 3, :]), [tk + "2", tk + "3"], [rk + "b"])
                    kb.dma(dst[256 * h:256 * h + 256, 512 * jb:512 * jb + 512].rearrange("(c p) n -> p c n", p=128), ro[:],
                           [rk + "a", rk + "b"], [("P_r", isk, h, jb)], q="pool")
                    if isk:
                        kz, kzk = kzr.next()
                        pt, pk = ptbank()
                        for i in range(4):
                            for c in range(2):
                                kb.tr(pt[:, 256 * i + 128 * c:256 * i + 128 * c + 128], ro[:, c, 128 * i:128 * i + 128], ident[:],
                                      [rk + "a", rk + "b", "ident"], [pk])
                        kb.act(kz[:].rearrange("p i d -> p (i d)"), pt[:], AF.Copy, [pk, "retsc"], [kzk], scale=retsc[:, 4 + h:5 + h])
                        kb.dma(P_kz[512 * jb:512 * jb + 512, 256 * h:256 * h + 256].rearrange("(i p) d -> p i d", p=128), kz[:],
                               [kzk], [("P_kz", h, jb)], q="pool")

        def tm_group(obase, ncols_total, dst, func, dkey, dstf32=False):
            for c0 in range(0, ncols_total, 512):
                ncol = min(512, ncols_total - c0)
                wt, wk = load_w(wpool, "w_in", row0, obase + c0, ncol)
                for t in range(NT):
                    pb, pk = gemm_tm(hT, wt, wk, t, ncol)
                    if dstf32:
                        sg, sk = stgf.next()
                    else:
                        sg, sk = stg.next()
                    if func == AF.Copy and (flip[0] % 2 == 0):
                        kb.v("dve", lambda: V.tensor_copy(out=sg[:, 0:ncol], in_=pb[:, 0:ncol]), [pk], [sk])
                    else:
                        kb.act(sg[:, 0:ncol], pb[:, 0:ncol], func, [pk], [sk])
                    flip[0] += 1
                    kb.dma(dst[128 * t:128 * t + 128, c0:c0 + ncol], sg[:, 0:ncol], [sk], [(dkey, c0 // 512, t)], q="pool")

        tm_group(O_VR, 2048, P_v, AF.Copy, "P_v")
        tm_group(O_GR, 2048, P_sg, AF.Silu, "P_sg")
        tm_group(O_KV + 3 * 128, 128, P_vs, AF.Copy, "P_vs")
        tm_group(O_KV + 5 * 128, 128, P_vw, AF.Copy, "P_vw")
        tm_group(O_GATE, 48, P_gate, AF.Sigmoid, "P_gate", dstf32=True)
        tm_group(O_MA, 1024, P_ma, AF.Sigmoid, "P_ma")
        tm_group(O_MB, 1024, P_mb, AF.Sigmoid, "P_mb")

        def fm_chunk(c0, dst_rows, scale, dkey, dup=False):
            wt, wk = load_w(wpool, "w_in", row0, c0, 128, dup64=(True if dup else None))
            for jb in range(4):
                pb, pk = gemm_fm(hT, wt, wk, jb, 128, 0)
                sg, sk = stg.next()
                kb.act(sg[:], pb[:], AF.Copy, [pk], [sk], scale=scale)
                kb.dma(dst_rows[:, 512 * jb:512 * jb + 512], sg[:], [sk], [(dkey, jb)], q="pool")

        for c in range(8):
            fm_chunk(O_QN + 128 * c, P_qnT[128 * c:128 * c + 128, :], 0.125, ("P_qnT", c))
        fm_chunk(O_KV + 0, P_kcT, 1.0, "P_kcT")
        fm_chunk(O_KV + 128, P_vcT, 1.0, "P_vcT")
        for g in range(2):
            fm_chunk(O_KV + 256 + 64 * g, P_ksT[128 * g:128 * g + 128, :], 1.0, ("P_ksT", g), dup=True)
            fm_chunk(O_KV + 512 + 64 * g, P_kwT[128 * g:128 * g + 128, :], 1.0, ("P_kwT", g), dup=True)
        kb.release(mk)

    PROJ_R_KEYS = [("P_r", isk, h, jb) for isk in range(2) for h in range(4) for jb in range(4)]

    def stage_retention(li):
        mk = kb.mark()
        dmask = kb.sb("dmask", [128, 4, 128], F32)
        kb.dma(dmask[:].rearrange("p h n -> p (h n)"), c_dmask, [], ["dmask"])
        retsc = kb.sb("retsc", [128, 12], F32)
        kb.dma(retsc[:], c_retsc, [], ["retsc"])
        R32s = [kb.sb("R32", [128, 2, 512], F32) for _ in range(4)]
        Rbs = [kb.sb("Rb", [128, 2, 512], BF16) for _ in range(4)]
        qTr = Rot(kb, "qT", [128, 2, 128], BF16, 8)
        kTr = Rot(kb, "kT", [128, 2, 128], BF16, 8)
        kzr = Rot(kb, "kzl", [128, 256], BF16, 8)
        vr = Rot(kb, "vl", [128, 512], BF16, 8)
        sgr = Rot(kb, "sgl", [128, 512], BF16, 8)
        sTr = Rot(kb, "sTb", [128, 128], BF16, 4)
        osr = Rot(kb, "osb", [128, 512], F32, 4)
        str_ = Rot(kb, "stat", [128, 16], F32, 6)
        zr = Rot(kb, "z", [128, 512], BF16, 4)
        z2r = Rot(kb, "z2", [128, 512], F32, 4)
        zTr = Rot(kb, "zT", [128, 4, 128], BF16, 4)
        for h in range(4):
            kb.v("pool", lambda: G.memset(R32s[h][:], 0.0), [], ["R32a%d" % h, "R32b%d" % h])
            kb.v("pool", lambda: G.memset(Rbs[h][:], 0.0), [], ["Rba%d" % h, "Rbb%d" % h])
        units = [(c, h) for c in range(NT) for h in range(4)]

        def r_load(c, h):
            jb = c // 4
            qT, qk = qTr.next(); kT, kk = kTr.next(); kz, kzk = kzr.next(); vv, vk = vr.next(); sg, sgk = sgr.next()
            cols = slice(128 * c, 128 * c + 128)
            kb.dma(qT[:], P_qrT[256 * h:256 * h + 256, cols].rearrange("(c p) n -> p c n", p=128), [("P_r", 0, h, jb)], [qk])
            kb.dma(kT[:], P_krT[256 * h:256 * h + 256, cols].rearrange("(c p) n -> p c n", p=128), [("P_r", 1, h, jb)], [kk])
            kb.dma(kz[:], P_kz[cols, 256 * h:256 * h + 256], [("P_kz", h, jb)], [kzk])
            kb.dma(vv[:], P_v[cols, 512 * h:512 * h + 512], [("P_v", h, c)], [vk])
            kb.dma(sg[:], P_sg[cols, 512 * h:512 * h + 512], [("P_sg", h, c)], [sgk])
            return dict(qT=qT, qk=qk, kT=kT, kk=kk, kz=kz, kzk=kzk, vv=vv, vk=vk, sg=sg, sgk=sgk)

        def r_p1(c, h, L):
            Rb = Rbs[h]
            ps_, psk = pbank()
            for cc in range(2):
                kb.mm(ps_[:, 0:128], L["kT"][:, cc, :], L["qT"][:, cc, :], cc == 0, cc == 1, [L["kk"], L["qk"]], [psk])
            sT, sTk = sTr.next()
            kb.v("dve", lambda: V.tensor_mul(out=sT[:], in0=ps_[:, 0:128], in1=dmask[:, h, :]), [psk, "dmask"], [sTk])
            po, pok = pbank()
            kb.mm(po[:], sT[:], L["vv"][:], True, False, [sTk, L["vk"]], [pok])
            for cc in range(2):
                kb.mm(po[:], L["qT"][:, cc, :], Rb[:, cc, :], False, cc == 1, [L["qk"], "Rb" + "ab"[cc] + str(h)], [pok])
            osb, osk = osr.next()
            kb.act(osb[:], po[:], AF.Copy, [pok, "retsc"], [osk], scale=retsc[:, h:h + 1])
            st, stk = str_.next()
            kb.v("dve", lambda: V.bn_stats(out=st[:, 0:6], in_=osb[:]), [osk], [stk + "a"])
            kb.v("dve", lambda: V.bn_aggr(out=st[:, 6:8], in_=st[:, 0:6]), [stk + "a"], [stk + "b"])
            kb.act(st[:, 8:9], st[:, 7:8], AF.Sqrt, [stk + "b"], [stk + "c"], bias=1e-5)
            L.update(osb=osb, osk=osk, st=st, stk=stk)

        def r_p2(c, h, L):
            gch = C["gchunk"][h]
            R32 = R32s[h]; Rb = Rbs[h]
            st, stk, osb, osk = L["st"], L["stk"], L["osb"], L["osk"]
            cols = slice(128 * c, 128 * c + 128)
            if c < NT - 1:
                for cc in range(2):
                    pr, prk = pbank()
                    kb.mm(pr[:], L["kz"][:, 128 * cc:128 * cc + 128], L["vv"][:], True, True, [L["kzk"], L["vk"]], [prk])
                    kb.v("dve", lambda: V.scalar_tensor_tensor(out=R32[:, cc, :], in0=R32[:, cc, :], scalar=gch, in1=pr[:],
                                                               op0=ALU.mult, op1=ALU.add), ["R32" + "ab"[cc] + str(h), prk], ["R32" + "ab"[cc] + str(h)])
                    kb.act(Rb[:, cc, :], R32[:, cc, :], AF.Copy, ["R32" + "ab"[cc] + str(h)], ["Rb" + "ab"[cc] + str(h)])
            kb.v("dve", lambda: V.reciprocal(out=st[:, 9:10], in_=st[:, 8:9]), [stk + "c"], [stk + "d"])
            z2, z2k = z2r.next()
            kb.v("dve", lambda: V.tensor_scalar(out=z2[:], in0=osb[:], scalar1=st[:, 6:7], scalar2=st[:, 9:10],
                                                op0=ALU.subtract, op1=ALU.mult), [osk, stk + "b", stk + "d"], [z2k])
            z, zk = zr.next()
            kb.v("dve", lambda: V.tensor_mul(out=z[:], in0=z2[:], in1=L["sg"][:]), [z2k, L["sgk"]], [zk])
            L.update(z=z, zk=zk)

        def r_p3(c, h, L):
            z, zk = L["z"], L["zk"]
            cols = slice(128 * c, 128 * c + 128)
            pt, pk = ptbank()
            for e in range(4):
                kb.tr(pt[:, 128 * e:128 * e + 128], z[:, 128 * e:128 * e + 128], ident[:], [zk, "ident"], [pk])
            zT, zTk = zTr.next()
            kb.act(zT[:].rearrange("p e n -> p (e n)"), pt[:, 0:512], AF.Copy, [pk], [zTk])
            kb.dma(Z_T[512 * h:512 * h + 512, cols].rearrange("(e p) n -> p e n", p=128), zT[:], [zTk], [("Z_T", c, h)], q="pool")

        NU = len(units)
        PRE = 4
        Ls = {}
        for n in range(min(PRE, NU)):
            Ls[n] = r_load(*units[n])
        for n in range(NU + 2):
            if n + PRE < NU:
                Ls[n + PRE] = r_load(*units[n + PRE])
            if n < NU:
                r_p1(*units[n], Ls[n])
            if 1 <= n <= NU:
                r_p2(*units[n - 1], Ls[n - 1])
            if n >= 2:
                r_p3(*units[n - 2], Ls.pop(n - 2))
        kb.release(mk)

    def stage_nsa(li):
        mk = kb.mark()
        keep = kb.sb("keep", [128, 16, 32], F32); addm = kb.sb("addm", [128, 16, 32], F32)
        kb.dma(keep[:].rearrange("p t j -> p (t j)"), c_keep, [], ["keep"])
        kb.dma(addm[:].rearrange("p t j -> p (t j)"), c_addm, [], ["addm"])
        gates = kb.sb("gates", [128, 16, 48], F32)
        kb.dma(gates[:], P_gate.rearrange("(t p) c -> p t c", p=128), [("P_gate", 0, t) for t in range(NT)], ["gates"])
        if stop_after == "nsa0":
            raise _Stop()
        kcx = kb.sb("kcx", [128, 2, 2, 128], BF16)
        kb.v("pool", lambda: G.memset(kcx[:], 0.0), [], ["kcx0", "kcx1"])
        vca = kb.sb("vcaug", [128, 2, 97], BF16)
        kb.v("pool", lambda: G.memset(vca[:], 1.0), [], ["vca0", "vca1"])
        for g in range(2):
            kb.dma(vca[0:127, g, 64:96], c_overlap, [], ["vca%d" % g])
        mk2 = kb.mark()
        w1 = Rot(kb, "w1", [128, 32, 256], BF16, 2)
        for kv in range(2):
            nm1 = "cmp_w1_v" if kv else "cmp_w1_k"
            nm2 = "cmp_w2_v" if kv else "cmp_w2_k"
            pos_d = cmp_pos_v if kv else cmp_pos_k
            w1t, w1k = w1.next()
            for hh in range(2):
                kb.dma(w1t[64 * hh:64 * hh + 64, :, :], wb[nm1][2048 * li:2048 * li + 2048, :].rearrange("(l d) h -> d l h", d=64),
                       [("wb", nm1)], [w1k])
            w2t = kb.sb("w2t", [128, 2, 128], BF16)
            for hh in range(2):
                kb.dma(w2t[:, :, 64 * hh:64 * hh + 64], wb[nm2][256 * li:256 * li + 256, :].rearrange("(c p) d -> p c d", p=128),
                       [("wb", nm2)], ["w2t"])
            posl = kb.sb("posl", [32, 64], F32)
            kb.dma(posl[:], pos_d[32 * li:32 * li + 32, :], [], ["posl"])
            posb = kb.sb("posb", [32, 64], BF16)
            kb.v("dve", lambda: V.tensor_copy(out=posb[:], in_=posl[:]), ["posl"], ["posb"])
            pt, pk = ptbank()
            kb.tr(pt[0:64, 0:32], posb[:], ident[0:32, 0:32], ["posb", "ident"], [pk])
            posT = kb.sb("posT", [128, 32], F32)
            kb.act(posT[0:64, :], pt[0:64, 0:32], AF.Copy, [pk], ["posT"])
            kb.act(posT[64:128, :], pt[0:64, 0:32], AF.Copy, [pk], ["posT"])
            kvT = kb.sb("kvT", [128, T], BF16)
            src = P_vcT if kv else P_kcT
            kb.dma(kvT[:], src, [("P_vcT" if kv else "P_kcT", jb) for jb in range(4)], ["kvT"])
            kvA = kb.sb("kvA", [128, T], BF16); kvB = kb.sb("kvB", [128, T], BF16)
            kb.v("dve", lambda: V.tensor_add(out=kvA[:].rearrange("p (a b) -> p a b", b=16), in0=kvT[:].rearrange("p (a b) -> p a b", b=16),
                                             in1=bcast_ap(posT[:, 0:16], [[0, 128], [1, 16]])), ["kvT", "posT"], ["kvA"])
            kb.v("dve", lambda: V.tensor_add(out=kvB[:].rearrange("p (a b) -> p a b", b=16), in0=kvT[:].rearrange("p (a b) -> p a b", b=16),
                                             in1=bcast_ap(posT[:, 16:32], [[0, 128], [1, 16]])), ["kvT", "posT"], ["kvB"])
            for g in range(2):
                pr = slice(64 * g, 64 * g + 64)
                gT = kb.sb("gT", [128, 2, 128], BF16)
                for ch in range(2):
                    pb, pk_ = pbank()
                    for l in range(32):
                        srcT = kvA if l < 16 else kvB
                        rhs = bcast_ap(srcT[pr, l:l + 1], [[16, 127]])
                        kb.mm(pb[:, 0:127], w1t[pr, l, 128 * ch:128 * ch + 128], rhs, l == 0, l == 31,
                              [w1k, "kvA", "kvB"], [pk_])
                    hs = kb.sb("hs", [128, 4, 127], F32)
                    kb.act(hs[:, 0, :], pb[:, 0:127], AF.Copy, [pk_], ["hs0"])
                    kb.v("dve", lambda: V.tensor_mul(out=hs[:, 1, :], in0=hs[:, 0, :], in1=hs[:, 0, :]), ["hs0"], ["hs1"])
                    kb.v("dve", lambda: V.tensor_scalar(out=hs[:, 2, :], in0=hs[:, 1, :], scalar1=0.044715, scalar2=1.0,
                                                        op0=ALU.mult, op1=ALU.add), ["hs1"], ["hs2"])
                    kb.v("dve", lambda: V.tensor_mul(out=hs[:, 3, :], in0=hs[:, 2, :], in1=hs[:, 0, :]), ["hs2", "hs0"], ["hs3"])
                    kb.act(hs[:, 1, :], hs[:, 3, :], AF.Sigmoid, ["hs3", "hs2"], ["hs1"], scale=2.0 * math.sqrt(2.0 / math.pi))
                    kb.v("dve", lambda: V.tensor_mul(out=gT[:, ch, 0:127], in0=hs[:, 1, :], in1=hs[:, 0, :]), ["hs1", "hs0"], ["gT%d" % ch])
                pb, pk_ = pbank()
                if kv == 0:
                    for ch in range(2):
                        kb.mm(pb[:, 0:127], w2t[:, ch, :], gT[:, ch, 0:127], ch == 0, ch == 1, ["w2t", "gT%d" % ch], [pk_])
                    for par in range(2):
                        kb.act(kcx[64 * par:64 * par + 64, par, g, 0:127], pb[64 * par:64 * par + 64, 0:127], AF.Copy, [pk_], ["kcx%d" % g])
                else:
                    for ch in range(2):
                        kb.mm(pb[0:127, 0:64], gT[:, ch, 0:127], w2t[:, ch, 0:64], ch == 0, ch == 1, ["w2t", "gT%d" % ch], [pk_])
                    kb.act(vca[0:127, g, 0:64], pb[0:127, 0:64], AF.Copy, [pk_], ["vca%d" % g])
        kb.release(mk2)
        if stop_after == "nsa1":
            raise _Stop()

        PTr = Rot(kb, "PT", [128, 512], BF16, 4)
        for g in range(2):
            mk3 = kb.mark()
            ksx = kb.sb("ksx", [128, 2, T], BF16); kwx = kb.sb("kwx", [128, 2, T], BF16)
            kb.v("pool", lambda: G.memset(kwx[:], 0.0), [], ["kwx"])
            for par in range(2):
                own = slice(64 * par, 64 * par + 64)
                oth = slice(64 * (1 - par), 64 * (1 - par) + 64)
                kb.dma(ksx[own, par, :], P_ksT[128 * g + 64 * par:128 * g + 64 * par + 64, :], [(("P_ksT", g), jb) for jb in range(4)], ["ksx"])
                kb.dma(ksx[oth, par, :], c_expand, [], ["ksx"])
                kb.dma(kwx[own, par, :], P_kwT[128 * g + 64 * par:128 * g + 64 * par + 64, :], [(("P_kwT", g), jb) for jb in range(4)], ["kwx"])
            vsa = kb.sb("vsa", [128, 16, 65], BF16); vwa = kb.sb("vwa", [128, 16, 65], BF16)
            kb.v("pool", lambda: G.memset(vsa[:], 1.0), [], ["vsa"])
            kb.v("pool", lambda: G.memset(vwa[:], 1.0), [], ["vwa"])
            kb.dma(vsa[:, :, 0:64], P_vs[:, 64 * g:64 * g + 64].rearrange("(t p) d -> p t d", p=128),
                   [("P_vs", 0, t) for t in range(NT)], ["vsa"])
            kb.dma(vwa[:, :, 0:64], P_vw[:, 64 * g:64 * g + 64].rearrange("(t p) d -> p t d", p=128),
                   [("P_vw", 0, t) for t in range(NT)], ["vwa"])
            qs = kb.sb("qs", [128, 8, T], BF16)
            kb.v("pool", lambda: G.memset(qs[:, 0:4, :], 0.0), [], [("qsz", 0)])
            kb.v("pool", lambda: G.memset(qs[:, 4:8, :], 0.0), [], [("qsz", 1)])
            for hh in range(8):
                par = hh % 2
                own = slice(64 * par, 64 * par + 64)
                kb.dma(qs[own, hh, :], P_qnT[128 * (4 * g + hh // 2) + 64 * par:128 * (4 * g + hh // 2) + 64 * par + 64, :],
                       [(("P_qnT", 4 * g + hh // 2), jb) for jb in range(4)] + [("qsz", hh // 4)], [("qsq", hh)])
            og = kb.sb("og", [128, 16, 512], F32)
            ocr = Rot(kb, "ocimp", [128, 8, 96], F32, 2)
            zcr = Rot(kb, "zc", [128, 127], F32, 3)
            pcr = Rot(kb, "pc", [128, 128], BF16, 4)
            pTr = Rot(kb, "pTc", [128, 128], BF16, 4)
            smr = Rot(kb, "sm", [128, 12], F32, 4)
            impr = Rot(kb, "imp", [128, 4, 32], F32, 2)
            ngr = Rot(kb, "ngm", [128, 64], BF16, 2)
            for i in range(2):
                kb.v("pool", lambda: G.memset(ngr.tiles[i][:], 0.0), [], [ngr.keys[i]])
            citems = [(t, hh) for t in range(NT) for hh in range(8)]
            octile = {}
            crr = [0]

            def cA(t, hh):
                pr = slice(64 * (hh % 2), 64 * (hh % 2) + 64)
                h = 8 * g + hh
                j4 = crr[0]; crr[0] = (j4 + 1) % 4
                pb, pk = PB[j4], PBK[j4]
                kb.mm(pb[:, 0:127], qs[:, hh, 128 * t:128 * t + 128], kcx[:, hh % 2, g, 0:127], True, True,
                      [("qsq", hh), ("qsz", hh // 4), ("qsm", hh % 2, t), "kcx%d" % g], [pk])
                zc, zk = zcr.next()
                kb.v("dve", lambda: V.tensor_add(out=zc[:], in0=pb[:, 0:127], in1=Gt[:, h, 120 - 8 * t:120 - 8 * t + 127]), [pk, "Gt"], [zk])
                pc, pck = pcr.next()
                kb.act(pc[:, 0:127], zc[:], AF.Exp, [zk], [pck])
                return pc, pck

            def cB(t, hh, pc, pck):
                pt, ptk = ptbank()
                kb.tr(pt[0:127, 0:128], pc[:, 0:127], ident[:], [pck, "ident"], [ptk])
                pT, pTk = pTr.next()
                kb.act(pT[0:127, :], pt[0:127, 0:128], AF.Copy, [ptk], [pTk])
                return pT, pTk

            def cC(t, hh, pT, pTk):
                h = 8 * g + hh
                if hh == 0:
                    octile[t] = ocr.next()
                oc, ock = octile[t]
                po, pok = PB[4 + hh // 4], PBK[4 + hh // 4]
                P4 = po[:].rearrange("p (i c) -> p i c", c=128)
                kb.mm(P4[:, hh % 4, 0:97], pT[0:127, :], vca[0:127, g, :], True, True, [pTk, "vca%d" % g], [pok], sgc=True)
                if hh % 4 == 3:
                    j0 = hh - 3
                    sm, smk = smr.next()
                    kb.v("dve", lambda: V.tensor_scalar_max(out=sm[:, 0:4], in0=P4[:, :, 96], scalar1=1e-30), [pok], [smk + "a"])
                    kb.v("dve", lambda: V.reciprocal(out=sm[:, 4:8], in_=sm[:, 0:4]), [smk + "a"], [smk + "b"])
                    ock4 = [(ock, x_) for x_ in range(j0, j0 + 4)]
                    kb.v("dve", lambda: V.tensor_tensor(out=oc[:, j0:j0 + 4, :], in0=P4[:, :, 0:96], in1=bcast_ap(sm[:, 4:8], [[1, 4], [0, 96]]), op=ALU.mult),
                         [pok, smk + "b"], ock4)
                    gl = gates[:, t, 3 * (8 * g + j0):3 * (8 * g + j0) + 1]
                    kb.v("pool", lambda: G.tensor_tensor(out=og[:, t, 64 * j0:64 * j0 + 256].rearrange("p (i c) -> p i c", c=64), in0=oc[:, j0:j0 + 4, 0:64],
                                                         in1=bcast_ap(gl, [[3, 4], [0, 64]]), op=ALU.mult),
                         ock4 + ["gates"], [("og", t, x_) for x_ in range(j0, j0 + 4)])
                if hh == 7:
                    im, imk = impr.next()
                    kb.v("dve", lambda: V.tensor_reduce(out=im[:, 0, :], in_=oc[:, :, 64:96].rearrange("p h j -> p j h"), axis=AX.X, op=ALU.add),
                         [(ock, x_) for x_ in range(8)], [imk + "0"])
                    kb.v("dve", lambda: V.tensor_mul(out=im[:, 1, :], in0=im[:, 0, :], in1=keep[:, t, :]), [imk + "0", "keep"], [imk + "1"])
                    kb.v("dve", lambda: V.tensor_add(out=im[:, 2, :], in0=im[:, 1, :], in1=addm[:, t, :]), [imk + "1", "addm"], [imk + "2"])
                    sm2, smk2 = smr.next()
                    kb.v("dve", lambda: V.max(out=sm2[:, 0:8], in_=im[:, 2, :]), [imk + "2"], [smk2 + "a"])
                    kb.v("dve", lambda: V.tensor_scalar(out=im[:, 3, :], in0=im[:, 2, :], scalar1=sm2[:, 7:8], scalar2=None, op0=ALU.is_ge),
                         [imk + "2", smk2 + "a"], [imk + "3"])
                    ng, ngk = ngr.next()
                    kb.v("dve", lambda: V.tensor_scalar(out=ng[:, 0:32], in0=im[:, 3, :], scalar1=-1.0, scalar2=NEGM, op0=ALU.add, op1=ALU.mult),
                         [imk + "3"], [ngk])
                    pt, ptk = ptbank()
                    kb.tr(pt[0:64, 0:128], ng[:], ident[:], [ngk, "ident"], [ptk])
                    for par in range(2):
                        oth = slice(64 * (1 - par), 64 * (1 - par) + 64)
                        kb.act(bcast_ap(qs[oth, par, 128 * t:128 * t + 128], [[2 * T, 4], [1, 128]]),
                               bcast_ap(pt[0:64, 0:128], [[0, 4], [1, 128]]), AF.Copy,
                               [ptk, ("qsz", 0), ("qsz", 1)], [("qsm", par, t)])

            nci = len(citems)
            hA = {}
            hB = {}
            for n in range(nci + 2):
                if n < nci:
                    hA[n] = cA(*citems[n])
                if 1 <= n <= nci:
                    hB[n - 1] = cB(*citems[n - 1], *hA.pop(n - 1))
                if 2 <= n:
                    cC(*citems[n - 2], *hB.pop(n - 2))
            if stop_after == "nsa2":
                raise _Stop()
            LOOK = 2
            STB, STK = PB[0:4], PBK[0:4]
            ACB, ACK = PB[4:6], PBK[4:6]
            tmpr = Rot(kb, "fin", [128, 4, 64], F32, 2)
            units = [(hh, qb, br) for hh in range(8) for qb in range(4) for br in range(2)]
            items = []
            for ui, (hh, qb, br) in enumerate(units):
                kt_lo = 0 if br == 0 else max(0, 4 * qb - 4)
                for kt in range(kt_lo, 4 * qb + 4):
                    items.append((ui, hh, qb, br, kt, kt == kt_lo, kt == 4 * qb + 3))

            def sQK(n, item):
                ui, hh, qb, br, kt, isfirst, islast = item
                h = 8 * g + hh
                pr = slice(64 * (hh % 2), 64 * (hh % 2) + 64)
                c = hh // 2
                kx_, kxk = (ksx, "ksx") if br == 0 else (kwx, "kwx")
                st_, stk_ = STB[n % 4], STK[n % 4]
                par = hh % 2
                vi = [i for i in range(4) if 0 <= 4 * qb + i - kt and not (br == 1 and 4 * qb + i - kt > 4)]
                c0_, c1_ = 128 * vi[0], 128 * vi[-1] + 128
                kb.mm(st_[:, c0_:c1_], kx_[:, par, 128 * kt:128 * kt + 128], qs[:, hh, 512 * qb + c0_:512 * qb + c1_], True, True,
                      [kxk, ("qsq", hh), ("qsz", hh // 4)] + [("qsm", par, 4 * qb + i) for i in vi], [stk_])
                PTt, PTk = PTr.next()
                kb.act(PTt[:, c0_:c1_], st_[:, c0_:c1_], AF.Exp, [stk_], [(PTk, i) for i in range(4)])
                for i in range(4):
                    off = 4 * qb + i - kt
                    if off < 0 or (br == 1 and off > 4):
                        continue
                    sl = slice(128 * i, 128 * i + 128)
                    if off == 0:
                        kb.v("dve", lambda: V.tensor_mul(out=PTt[:, sl], in0=PTt[:, sl], in1=E0[:, h, :]), [(PTk, i), "E0"], [(PTk, i)])
                    elif off == 1:
                        kb.v("pool", lambda: G.tensor_mul(out=PTt[:, sl], in0=PTt[:, sl], in1=E1[:, h, :]), [(PTk, i), "E1"], [(PTk, i)])
                    elif off == 4 and br == 1:
                        kb.v("pool", lambda: G.tensor_mul(out=PTt[:, sl], in0=PTt[:, sl], in1=wedge[:]), [(PTk, i), "wedge"], [(PTk, i)])
                return PTt, PTk

            def sPV(item, PTt, PTk):
                ui, hh, qb, br, kt, isfirst, islast = item
                h = 8 * g + hh
                va, vak = (vsa, "vsa") if br == 0 else (vwa, "vwa")
                accb, acck = ACB[ui % 2], ACK[ui % 2]
                A = accb[:].rearrange("p (i c) -> p i c", c=128)
                firstmm = isfirst
                for i in range(4):
                    off = 4 * qb + i - kt
                    if off < 0 or (br == 1 and off > 4):
                        continue
                    sl = slice(128 * i, 128 * i + 128)
                    kb.mm(A[:, i, 0:65], PTt[:, sl], va[:, kt, :], firstmm, kt == 4 * qb + i, [(PTk, i), vak], [acck], sgc=True)
                    firstmm = False
                if islast:
                    sm, smk = smr.next()
                    kb.v("dve", lambda: V.tensor_scalar_max(out=sm[:, 0:4], in0=A[:, :, 64], scalar1=1e-30), [acck], [smk + "a"])
                    kb.v("dve", lambda: V.reciprocal(out=sm[:, 4:8], in_=sm[:, 0:4]), [smk + "a"], [smk + "b"])
                    kb.v("dve", lambda: V.tensor_mul(out=sm[:, 8:12], in0=sm[:, 4:8], in1=gates[:, 4 * qb:4 * qb + 4, 3 * h + 1 + br]),
                         [smk + "b", "gates"], [smk + "c"])
                    tm, tmk = tmpr.next()
                    kb.v("dve", lambda: V.tensor_tensor(out=tm[:], in0=A[:, :, 0:64], in1=bcast_ap(sm[:, 8:12], [[1, 4], [0, 64]]), op=ALU.mult),
                         [acck, smk + "c"], [tmk])
                    ogk = [("og", 4 * qb + i, hh) for i in range(4)]
                    kb.v("pool", lambda: G.tensor_add(out=og[:, 4 * qb:4 * qb + 4, 64 * hh:64 * hh + 64],
                                                      in0=og[:, 4 * qb:4 * qb + 4, 64 * hh:64 * hh + 64], in1=tm[:]), ogk + [tmk], ogk)

            nit = len(items)
            hq = {}
            for n in range(nit + LOOK):
                if n < nit:
                    hq[n] = sQK(n, items[n])
                if n >= LOOK:
                    sPV(items[n - LOOK], *hq.pop(n - LOOK))
            obr = Rot(kb, "ob", [128, 512], BF16, 2)
            oTr = Rot(kb, "oT", [128, 4, 128], BF16, 2)
            for t in range(NT):
                ob, obk = obr.next()
                kb.act(ob[:], og[:, t, :], AF.Copy, [("og", t, hh) for hh in range(8)], [obk])
                pt, ptk = ptbank()
                for e in range(4):
                    kb.tr(pt[:, 128 * e:128 * e + 128], ob[:, 128 * e:128 * e + 128], ident[:], [obk, "ident"], [ptk])
                oT, oTk = oTr.next()
                kb.act(oT[:].rearrange("p e n -> p (e n)"), pt[:, 0:512], AF.Copy, [ptk], [oTk])
                kb.dma(ON_T[512 * g:512 * g + 512, 128 * t:128 * t + 128].rearrange("(e p) n -> p e n", p=128), oT[:], [oTk],
                       [("ON_T", g, t)], q="pool")
            kb.release(mk3)
        kb.release(mk)

    prr2 = [0]

    def stage_merge(li):
        mk = kb.mark()
        mT = kb.sb("mT", [128, 8, T], BF16)
        mk2 = kb.mark()
        wor = kb.sb("wor", [128, 16, D], BF16)
        won = kb.sb("won", [128, 8, D], BF16)
        for k4 in range(4):
            kb.dma(wor[:, 4 * k4:4 * k4 + 4, :], wb["w_o_ret"][2048 * li + 512 * k4:2048 * li + 512 * k4 + 512, :].rearrange("(k p) c -> p k c", p=128),
                   [("wb", "w_o_ret")], ["wor"])
        for k4 in range(2):
            kb.dma(won[:, 4 * k4:4 * k4 + 4, :], wb["w_o_nsa"][D * li + 512 * k4:D * li + 512 * k4 + 512, :].rearrange("(k p) c -> p k c", p=128),
                   [("wb", "w_o_nsa")], ["won"])
        zTr = Rot(kb, "zTl", [128, 16, 128], BF16, 2)
        oTr = Rot(kb, "oTl", [128, 8, 128], BF16, 2)
        sar = Rot(kb, "sa", [128, D], BF16, 2)
        sbr = Rot(kb, "sb", [128, D], BF16, 2)
        t1r = Rot(kb, "t1", [128, 512], F32, 2)
        t2r = Rot(kb, "t2", [128, 512], F32, 2)
        mbr = Rot(kb, "mb", [128, D], BF16, 2)
        for t in range(NT):
            cols = slice(128 * t, 128 * t + 128)
            zT, zk = zTr.next(); oT, ok = oTr.next(); sa, sak = sar.next(); sb_, sbk = sbr.next()
            kb.dma(zT[:], Z_T[:, cols].rearrange("(k p) n -> p k n", p=128), [("Z_T", t, h_) for h_ in range(4)], [zk])
            kb.dma(oT[:], ON_T[:, cols].rearrange("(k p) n -> p k n", p=128), [("ON_T", 0, t), ("ON_T", 1, t)], [ok])
            kb.dma(sa[:], P_ma[cols, :], [("P_ma", 0, t), ("P_ma", 1, t)], [sak])
            kb.dma(sb_[:], P_mb[cols, :], [("P_mb", 0, t), ("P_mb", 1, t)], [sbk])
            mb, mbk = mbr.next()
            for half in range(2):
                hs = slice(512 * half, 512 * half + 512)
                pr_, prk = pbank()
                for k in range(16):
                    kb.mm(pr_[:], zT[:, k, :], wor[:, k, hs], k == 0, k == 15, [zk, "wor"], [prk])
                pn, pnk = pbank()
                for k in range(8):
                    kb.mm(pn[:], oT[:, k, :], won[:, k, hs], k == 0, k == 7, [ok, "won"], [pnk])
                t1, t1k = t1r.next(); t2, t2k = t2r.next()
                kb.v("dve", lambda: V.tensor_mul(out=t1[:], in0=pr_[:], in1=sa[:, hs]), [prk, sak], [t1k])
                kb.v("dve", lambda: V.tensor_mul(out=t2[:], in0=pn[:], in1=sb_[:, hs]), [pnk, sbk], [t2k])
                kb.v("pool", lambda: G.tensor_add(out=mb[:, hs], in0=t1[:], in1=t2[:]), [t1k, t2k], [(mbk, half)])
            pt, ptk = ptbank()
            for k in range(8):
                kb.tr(pt[:, 128 * k:128 * k + 128], mb[:, 128 * k:128 * k + 128], ident[:], [(mbk, 0), (mbk, 1), "ident"], [ptk])
            kb.act(mT[:, :, cols], pt[:].rearrange("p (k n) -> p k n", k=8), AF.Copy, [ptk], [("mT", t)])
        kb.release(mk2)
        wo = kb.sb("wo", [128, 8, D], BF16)
        for k4 in range(2):
            kb.dma(wo[:, 4 * k4:4 * k4 + 4, :], wb["w_out"][D * li + 512 * k4:D * li + 512 * k4 + 512, :].rearrange("(k p) c -> p k c", p=128),
                   [("wb", "w_out")], ["wo"])
        for t in range(NT):
            for half in range(2):
                hs = slice(512 * half, 512 * half + 512)
                pb, pk = pbank()
                for k in range(8):
                    kb.mm(pb[:], mT[:, k, 128 * t:128 * t + 128], wo[:, k, hs], k == 0, k == 7, [("mT", t), "wo"], [pk])
                kb.v("dve", lambda: V.tensor_add(out=x_sb[:, t, hs], in0=x_sb[:, t, hs], in1=pb[:]), [("x", t), pk], [("x", t)])
        kb.release(mk)

    def stage_ffn(li, hT):
        mk = kb.mark()
        wpool = Rot(kb, "wf", [128, 8, 256], BF16, 3)
        sar = Rot(kb, "fsa", [128, 512], F32, 2)
        ur = Rot(kb, "fu", [128, 512], BF16, 3)
        for j in range(22):
            wt, wk = wpool.next()
            kb.dma(wt[:, :, 0:128], wb["w_ffn_in"][D * li:D * li + D, 128 * j:128 * j + 128].rearrange("(k p) c -> p k c", p=128),
                   [("wb", "w_ffn_in")], [wk])
            kb.dma(wt[:, :, 128:256], wb["w_ffn_in"][D * li:D * li + D, FFN + 128 * j:FFN + 128 * j + 128].rearrange("(k p) c -> p k c", p=128),
                   [("wb", "w_ffn_in")], [wk])
            for jb in range(4):
                pa, pak = gemm_fm(hT, wt, wk, jb, 128, 0)
                pb, pbk = gemm_fm(hT, wt, wk, jb, 128, 128)
                sa, sak = sar.next()
                kb.act(sa[:], pa[:], AF.Silu, [pak], [sak])
                u, uk = ur.next()
                kb.v("dve", lambda: V.tensor_mul(out=u[:], in0=pb[:], in1=sa[:]), [pbk, sak], [uk])
                kb.dma(U_T[128 * j:128 * j + 128, 512 * jb:512 * jb + 512], u[:], [uk], [("U_T", j, jb)], q="pool")
        kb.release(mk)
        mk = kb.mark()
        wfo = kb.sb("wfo", [128, 22, D], BF16)
        for k0 in range(0, 22, 4):
            k1 = min(22, k0 + 4)
            kb.dma(wfo[:, k0:k1, :], wb["w_ffn_out"][FFN * li + 128 * k0:FFN * li + 128 * k1, :].rearrange("(k p) c -> p k c", p=128),
                   [("wb", "w_ffn_out")], ["wfo"])
        uTr = Rot(kb, "uTl", [128, 22, 128], BF16, 2)
        for t in range(NT):
            uT, uk = uTr.next()
            kb.dma(uT[:], U_T[:, 128 * t:128 * t + 128].rearrange("(k p) n -> p k n", p=128), [("U_T", j, t // 4) for j in range(22)], [uk])
            for half in range(2):
                hs = slice(512 * half, 512 * half + 512)
                pb, pk = pbank()
                for k in range(22):
                    kb.mm(pb[:], uT[:, k, :], wfo[:, k, hs], k == 0, k == 21, [uk, "wfo"], [pk])
                kb.v("dve", lambda: V.tensor_add(out=x_sb[:, t, hs], in0=x_sb[:, t, hs], in1=pb[:]), [("x", t), pk], [("x", t)])
        kb.release(mk)

    def final_norm(s):
        mk = kb.mark()
        gbc = kb.sb("gbcf", [128, D], F32)
        kb.dma(gbc[:], bass.AP(tensor=norm_final_g.tensor, offset=0, ap=[[0, 128], [1, D]]), [], ["gbcf"])
        junk = Rot(kb, "junkf", [128, D], BF16, 2)
        ssr = Rot(kb, "ssf", [128, 4], F32, 3)
        yr = Rot(kb, "yo", [128, D], F32, 3)
        for t in range(NT):
            jt, jk = junk.next(); ss, sk = ssr.next(); yo, yk = yr.next()
            kb.act(jt[:], x_sb[:, t, :], AF.Square, [("x", t)], [jk, sk + "a"], accum_out=ss[:, 0:1])
            kb.act(ss[:, 1:2], ss[:, 0:1], AF.Sqrt, [sk + "a"], [sk + "b"], scale=1.0 / D, bias=1e-6)
            kb.v("dve", lambda: V.reciprocal(out=ss[:, 2:3], in_=ss[:, 1:2]), [sk + "b"], [sk + "c"])
            kb.v("dve", lambda: V.scalar_tensor_tensor(out=yo[:], in0=x_sb[:, t, :], scalar=ss[:, 2:3], in1=gbc[:],
                                                       op0=ALU.mult, op1=ALU.mult), [("x", t), sk + "c", "gbcf"], [yk])
            ev = kb.dma(y_d[s * T + 128 * t:s * T + 128 * t + 128, :], yo[:], [yk], [("y", s, t)], q="sp")
            out_events.append(ev)
        kb.release(mk)

    out_events = []

    for s in range(nseq):
        for t in range(NT):
            kb.dma(x_sb[:, t, :], x_d[s * T + 128 * t:s * T + 128 * t + 128, :], [], [("x", t)])
        for li in range(depth):
            mk = kb.mark()
            hT = kb.sb("hT", [128, 8, T], BF16)
            rmsnorm_to_hT(hT, bass.AP(tensor=norm_mix_g.tensor, offset=li * D, ap=[[0, 128], [1, D]]))
            if stop_after == "norm":
                break
            stage_proj(li, hT)
            kb.release(mk)
            if stop_after == "proj":
                break
            stage_retention(li)
            if stop_after == "ret":
                break
            try:
                stage_nsa(li)
            except _Stop:
                break
            if stop_after == "nsa":
                break
            stage_merge(li)
            if stop_after == "merge":
                break
            mk = kb.mark()
            hT = kb.sb("hT2", [128, 8, T], BF16)
            rmsnorm_to_hT(hT, bass.AP(tensor=norm_ffn_g.tensor, offset=li * D, ap=[[0, 128], [1, D]]))
            stage_ffn(li, hT)
            kb.release(mk)
            kb.prune()
        final_norm(s)

    sp = nc.sync
    for sname, sd in kb.streams.items():
        if sd["count"] > 0:
            sp.wait_ge(sd["sem"], sd["count"] * sd["inc"])
    kb.release(0)
    return nc, kb


_CACHE = {}


def _host_inputs(inputs):
    C = _consts()
    f = lambda a: np.ascontiguousarray(np.asarray(a, dtype=np.float32))
    m = {
        "w_in": f(inputs["w_in"]).reshape(DEPTH * D, CIN),
        "cmp_w1_k": f(inputs["cmp_w1_k"]).reshape(DEPTH * 2048, 256),
        "cmp_w2_k": f(inputs["cmp_w2_k"]).reshape(DEPTH * 256, 64),
        "cmp_w1_v": f(inputs["cmp_w1_v"]).reshape(DEPTH * 2048, 256),
        "cmp_w2_v": f(inputs["cmp_w2_v"]).reshape(DEPTH * 256, 64),
        "w_o_ret": f(inputs["w_o_ret"]).reshape(DEPTH * 2048, D),
        "w_o_nsa": f(inputs["w_o_nsa"]).reshape(DEPTH * D, D),
        "w_out": f(inputs["w_out"]).reshape(DEPTH * D, D),
        "w_ffn_in": f(inputs["w_ffn_in"]).reshape(DEPTH * D, 2 * FFN),
        "w_ffn_out": f(inputs["w_ffn_out"]).reshape(DEPTH * FFN, D),
        "norm_mix_g": f(inputs["norm_mix_g"]),
        "norm_ffn_g": f(inputs["norm_ffn_g"]),
        "norm_final_g": f(inputs["norm_final_g"]).reshape(1, D),
        "cmp_pos_k": f(inputs["cmp_pos_k"]).reshape(DEPTH * 32, 64),
        "cmp_pos_v": f(inputs["cmp_pos_v"]).reshape(DEPTH * 32, 64),
        "rel_bias": f(inputs["rel_bias"]).reshape(1, 512),
        "c_cos": C["cosT"], "c_sin": C["sinT"], "c_ident": C["ident"],
        "c_dmask": C["dmaskT"].reshape(128, 512), "c_retsc": C["retsc"],
        "c_dist0": C["dist0"], "c_dist1": C["dist1"], "c_distg": C["distg"],
        "c_wedge": C["wedge"], "c_expand": C["expand"], "c_overlap": C["overlap"],
        "c_keep": C["keep"].reshape(128, 512), "c_addm": C["addm"].reshape(128, 512),
    }
    return m


def kernel(**inputs):
    x = np.ascontiguousarray(np.asarray(inputs["x"], dtype=np.float32))
    B = x.shape[0]
    if "nc" not in _CACHE:
        _CACHE["nc"] = build()[0]
    nc = _CACHE["nc"]
    shared = _host_inputs(inputs)
    per = B // NCORES
    in_maps = []
    for c in range(NCORES):
        m = dict(shared)
        m["x"] = x[c * per:(c + 1) * per].reshape(per * T, D)
        in_maps.append(m)
    res = run_bass_kernel_spmd(nc, in_maps, core_ids=list(range(NCORES)))
    out = np.concatenate([r["y"].reshape(per, T, D) for r in res.results], axis=0)
    return out.astype(np.float32)
```

```python
import math
import numpy as np
import ml_dtypes
import concourse.bass as bass
import concourse.mybir as mybir
from concourse.bass_utils import run_bass_kernel_spmd

F32 = mybir.dt.float32
BF16 = mybir.dt.bfloat16
AF = mybir.ActivationFunctionType
ALU = mybir.AluOpType
AX = mybir.AxisListType

T = 2048
D = 1024
NT = 16
DEPTH = 2
CIN = 10032
NSEQ = 4
NCORES = 8
FFN = 2816
NEGM = 30000.0
O_QR, O_KR, O_VR, O_GR, O_QN, O_KV, O_GATE, O_MA, O_MB = 0, 1024, 2048, 4096, 6144, 7168, 7936, 7984, 9008


def _t5_bucket_np(dist):
    dist = np.maximum(dist, 0)
    d32 = np.maximum(dist, 1).astype(np.float32)
    large = 16 + (np.log(d32 / np.float32(16)) / np.float32(math.log(128 / 16)) * np.float32(16)).astype(np.int32)
    large = np.minimum(large, 31)
    return np.where(dist < 16, dist, large)


def _consts():
    c = {}
    half = 128
    freqs = (10000.0 ** (-np.arange(half, dtype=np.float32) / half)).astype(np.float32)
    ang = (np.arange(T, dtype=np.float32)[None, :] * freqs[:, None]).astype(np.float32)
    c["cosT"] = np.cos(ang).astype(np.float32)
    c["sinT"] = np.sin(ang).astype(np.float32)
    c["ident"] = np.eye(128, dtype=np.float32).astype(ml_dtypes.bfloat16)
    lg = np.log(1.0 - 2.0 ** (-5.0 - np.arange(4, dtype=np.float64)))
    m = np.arange(128, dtype=np.float64)
    dm = np.zeros((128, 4, 128), np.float32)
    for h in range(4):
        dm[:, h, :] = (np.exp(-(m + 1.0) * lg[h])[:, None] * (m[None, :] >= m[:, None])).astype(np.float32)
    c["dmaskT"] = dm
    rs = np.zeros((128, 12), np.float32)
    for h in range(4):
        rs[:, h] = np.exp((m + 1.0) * lg[h])
        rs[:, 4 + h] = np.exp((127.0 - m) * lg[h])
    c["retsc"] = rs
    c["gchunk"] = [float(np.exp(128.0 * lg[h])) for h in range(4)]
    bk = _t5_bucket_np(np.arange(0, 4096))
    c["thr"] = [float(np.argmax(bk >= b)) for b in range(32)]
    r = np.arange(128, dtype=np.float32)[:, None]
    cc = np.arange(128, dtype=np.float32)[None, :]
    c["dist0"] = (cc - r).astype(np.float32)
    c["dist1"] = (cc - r + 128).astype(np.float32)
    cg = np.arange(247, dtype=np.float32)[None, :]
    c["distg"] = (r - 16.0 * (cg - 120.0) - 31.0).astype(np.float32)
    c["wedge"] = (cc < r).astype(np.float32).astype(ml_dtypes.bfloat16)
    ex = np.zeros((64, T), np.float32)
    for j in range(32):
        ex[j, 64 * j:64 * j + 64] = 1.0
    c["expand"] = ex.astype(ml_dtypes.bfloat16)
    cmp_end = np.arange(127) * 16 + 31
    sel_start = np.arange(32) * 64
    ov = ((cmp_end[:, None] - 31 < sel_start[None] + 64) & (cmp_end[:, None] >= sel_start[None]))
    c["overlap"] = ov.astype(np.float32).astype(ml_dtypes.bfloat16)
    keep = np.zeros((128, 16, 32), np.float32)
    add = np.zeros((128, 16, 32), np.float32)
    jj = np.arange(32)
    for t in range(16):
        pos = 128 * t + np.arange(128)
        cur = pos // 64
        forced = (jj[None] == 0) | (jj[None] == cur[:, None]) | (jj[None] == cur[:, None] - 1)
        valid = sel_start[None] <= pos[:, None]
        keep[:, t, :] = (valid & ~forced)
        add[:, t, :] = np.where(valid, np.where(forced, 1e4, 0.0), -1e30)
    c["keep"] = keep
    c["addm"] = add
    return c


NO_SAME_ENGINE_SYNC = False


class KB:
    def __init__(self):
        self.nc = bass.Bass("TRN2", target_bir_lowering=False)
        nc = self.nc
        self.ctx = []
        self.eng = {"pe": nc.tensor, "act": nc.scalar, "dve": nc.vector, "pool": nc.gpsimd, "sp": nc.sync}
        self.streams = {}
        for e in self.eng:
            self.new_stream(e, e, 1)
        self.NDQ = 16
        self.dq = {}
        self.dqi = {}
        for q in ("sp", "pool", "act"):
            self.dq[q] = []
            self.dqi[q] = 0
            for j in range(self.NDQ if q != "pool" else 6):
                nm = "d%s%d" % (q, j)
                self.new_stream(nm, q, 16)
                self.dq[q].append(nm)
        self.clock = {e: {} for e in self.eng}
        self.evclock = {}
        self.state = {}
        self.nwaits = 0
        self.ninst = 0
        self._uid = 0

    def enter(self, cm):
        v = cm.__enter__()
        self.ctx.append(cm)
        return v

    def mark(self):
        return len(self.ctx)

    def release(self, mark):
        if len(self.ctx) > mark and hasattr(self, "clock"):
            self.barrier()
        while len(self.ctx) > mark:
            self.ctx.pop().__exit__(None, None, None)

    def barrier(self):
        for en, e in self.eng.items():
            clk = self.clock[en]
            for sname, sd in self.streams.items():
                if sd["count"] > clk.get(sname, 0):
                    e.wait_ge(sd["sem"], sd["count"] * sd["inc"])
                    clk[sname] = sd["count"]
                    self.nwaits += 1

    def new_stream(self, name, eng, inc):
        sem = self.enter(self.nc.semaphore("sem_" + name))
        self.streams[name] = dict(sem=sem, count=0, inc=inc, eng=eng)

    def sb(self, name, shape, dtype):
        self._uid += 1
        return self.enter(self.nc.sbuf_tensor("%s_%d" % (name, self._uid), list(shape), dtype))

    def ps(self, name, shape, dtype):
        self._uid += 1
        return self.enter(self.nc.psum_tensor("%s_%d" % (name, self._uid), list(shape), dtype))

    def dram(self, name, shape, dtype, kind="Internal"):
        return self.nc.dram_tensor(name, list(shape), dtype, kind=kind).ap()

    def op(self, eng, fn, reads=(), writes=(), stream=None, nosame=False, extra=()):
        st = self.state
        need = {}
        for w in extra:
            if w[1] > 0 and need.get(w[0], 0) < w[1]:
                need[w[0]] = w[1]
        for k in reads:
            s = st.get(k)
            if s and s[0]:
                w = s[0]
                if need.get(w[0], 0) < w[1]:
                    need[w[0]] = w[1]
        for k in writes:
            s = st.get(k)
            if s:
                if s[0]:
                    w = s[0]
                    if need.get(w[0], 0) < w[1]:
                        need[w[0]] = w[1]
                for w in s[1]:
                    if need.get(w[0], 0) < w[1]:
                        need[w[0]] = w[1]
        clk = self.clock[eng]
        e = self.eng[eng]
        for sname, idx in need.items():
            if (nosame or NO_SAME_ENGINE_SYNC) and sname == eng:
                continue
            if clk.get(sname, 0) >= idx:
                continue
            sd = self.streams[sname]
            e.wait_ge(sd["sem"], idx * sd["inc"])
            self.nwaits += 1
            ev = self.evclock.get((sname, idx))
            clk[sname] = idx
            if ev:
                for k2, v2 in ev.items():
                    if clk.get(k2, 0) < v2:
                        clk[k2] = v2
        inst = fn()
        sname = stream or eng
        sd = self.streams[sname]
        sd["count"] += 1
        idx = sd["count"]
        inst.then_inc(sd["sem"], sd["inc"])
        self.ninst += 1
        snap = dict(clk)
        self.evclock[(sname, idx)] = snap
        evt = (sname, idx)
        for k in reads:
            s = st.get(k)
            if s is None:
                st[k] = [None, [evt]]
            else:
                s[1].append(evt)
        for k in writes:
            st[k] = [evt, []]
        return evt

    def prune(self):
        if len(self.evclock) > 400000:
            keep = {}
            for k, s in self.state.items():
                if s[0]:
                    keep[s[0]] = self.evclock.get(s[0])
                for w in s[1]:
                    keep[w] = self.evclock.get(w)
            self.evclock = {k: v for k, v in keep.items() if v is not None}

    def dma(self, out, in_, reads, writes, q="sp"):
        eng = q
        j = self.dqi[q]
        self.dqi[q] = (j + 1) % len(self.dq[q])
        stream = self.dq[q][j]
        e = self.eng[eng]
        prev = (stream, self.streams[stream]["count"])
        return self.op(eng, lambda: e.dma_start(out=out, in_=in_), reads, writes, stream=stream, extra=[prev])

    def mm(self, out, lhsT, rhs, start, stop, reads, writes, sgc=False):
        pe = self.nc.tensor
        if sgc:
            return self.op("pe", lambda: pe.matmul(out, lhsT, rhs, start=start, stop=stop, skip_group_check=True),
                           reads, writes, nosame=True)
        return self.op("pe", lambda: pe.matmul(out, lhsT, rhs, start=start, stop=stop), reads, writes, nosame=True)

    def tr(self, out, in_, ident, reads, writes):
        pe = self.nc.tensor
        return self.op("pe", lambda: pe.transpose(out, in_, ident), reads, writes, nosame=True)

    def act(self, out, in_, func, reads, writes, scale=1.0, bias=0.0, accum_out=None):
        a = self.nc.scalar
        if accum_out is not None:
            return self.op("act", lambda: a.activation(out=out, in_=in_, func=func, bias=bias, scale=scale,
                                                       accum_out=accum_out), reads, writes)
        return self.op("act", lambda: a.activation(out=out, in_=in_, func=func, bias=bias, scale=scale), reads, writes)

    def v(self, eng, fn, reads, writes):
        return self.op(eng, fn, reads, writes)


class _Stop(Exception):
    pass


class Rot:
    def __init__(self, kb, name, shape, dtype, n, psum=False):
        self.tiles = [(kb.ps if psum else kb.sb)(name, shape, dtype) for _ in range(n)]
        self.keys = ["%s#%d#%d" % (name, id(self) % 100000, i) for i in range(n)]
        self.i = 0

    def next(self):
        t, k = self.tiles[self.i], self.keys[self.i]
        self.i = (self.i + 1) % len(self.tiles)
        return t, k


def bcast_ap(ap, dims):
    pa = ap.ap
    return bass.AP(tensor=ap.tensor, offset=ap.offset, ap=[[pa[0][0], pa[0][1]]] + [list(d) for d in dims])


def build(nseq=NSEQ, depth=DEPTH, debug=False, stop_after=None):
    C = _consts()
    kb = KB()
    nc = kb.nc
    V = nc.vector
    G = nc.gpsimd
    dk = "ExternalOutput" if debug else "Internal"

    def dbg(name, ap, shape, dtype, keys):
        if not debug:
            return
        d = nc.dram_tensor("dbg_" + name, list(shape), dtype, kind="ExternalOutput").ap()
        kb.dma(d, ap, keys, [("dbg", name)])

    def din(name, shape, dtype=F32):
        return nc.dram_tensor(name, list(shape), dtype, kind="ExternalInput").ap()

    x_d = din("x", [nseq * T, D])
    y_d = nc.dram_tensor("y", [nseq * T, D], F32, kind="ExternalOutput").ap()
    w_f32 = {
        "w_in": din("w_in", [DEPTH * D, CIN]),
        "cmp_w1_k": din("cmp_w1_k", [DEPTH * 2048, 256]),
        "cmp_w2_k": din("cmp_w2_k", [DEPTH * 256, 64]),
        "cmp_w1_v": din("cmp_w1_v", [DEPTH * 2048, 256]),
        "cmp_w2_v": din("cmp_w2_v", [DEPTH * 256, 64]),
        "w_o_ret": din("w_o_ret", [DEPTH * 2048, D]),
        "w_o_nsa": din("w_o_nsa", [DEPTH * D, D]),
        "w_out": din("w_out", [DEPTH * D, D]),
        "w_ffn_in": din("w_ffn_in", [DEPTH * D, 2 * FFN]),
        "w_ffn_out": din("w_ffn_out", [DEPTH * FFN, D]),
    }
    norm_mix_g = din("norm_mix_g", [DEPTH, D])
    norm_ffn_g = din("norm_ffn_g", [DEPTH, D])
    norm_final_g = din("norm_final_g", [1, D])
    cmp_pos_k = din("cmp_pos_k", [DEPTH * 32, 64])
    cmp_pos_v = din("cmp_pos_v", [DEPTH * 32, 64])
    rel_bias = din("rel_bias", [1, 512])
    c_cos = din("c_cos", [128, T]); c_sin = din("c_sin", [128, T])
    c_ident = din("c_ident", [128, 128], BF16)
    c_dmask = din("c_dmask", [128, 512]); c_retsc = din("c_retsc", [128, 12])
    c_dist0 = din("c_dist0", [128, 128]); c_dist1 = din("c_dist1", [128, 128]); c_distg = din("c_distg", [128, 247])
    c_wedge = din("c_wedge", [128, 128], BF16)
    c_expand = din("c_expand", [64, T], BF16)
    c_overlap = din("c_overlap", [127, 32], BF16)
    c_keep = din("c_keep", [128, 512]); c_addm = din("c_addm", [128, 512])

    wb = {k: kb.dram("wb_" + k, v.shape, BF16) for k, v in w_f32.items()}
    P_qrT = kb.dram("P_qrT", [1024, T], BF16, dk)
    P_krT = kb.dram("P_krT", [1024, T], BF16, dk)
    P_kz = kb.dram("P_kz", [T, 1024], BF16, dk)
    P_v = kb.dram("P_v", [T, 2048], BF16, dk)
    P_sg = kb.dram("P_sg", [T, 2048], BF16, dk)
    P_qnT = kb.dram("P_qnT", [1024, T], BF16, dk)
    P_kcT = kb.dram("P_kcT", [128, T], BF16, dk)
    P_vcT = kb.dram("P_vcT", [128, T], BF16, dk)
    P_ksT = kb.dram("P_ksT", [256, T], BF16, dk)
    P_kwT = kb.dram("P_kwT", [256, T], BF16, dk)
    P_vs = kb.dram("P_vs", [T, 128], BF16, dk)
    P_vw = kb.dram("P_vw", [T, 128], BF16, dk)
    P_gate = kb.dram("P_gate", [T, 48], F32, dk)
    P_ma = kb.dram("P_ma", [T, 1024], BF16, dk)
    P_mb = kb.dram("P_mb", [T, 1024], BF16, dk)
    Z_T = kb.dram("Z_T", [2048, T], BF16, dk)
    ON_T = kb.dram("ON_T", [1024, T], BF16, dk)
    U_T = kb.dram("U_T", [FFN, T], BF16, dk)

    for k, src in w_f32.items():
        rows = src.shape[0]
        step = 256
        for r0 in range(0, rows, step):
            r1 = min(rows, r0 + step)
            kb.dma(wb[k][r0:r1, :], src[r0:r1, :], reads=[], writes=[("wb", k)], q="pool")

    x_sb = kb.sb("x", [128, NT, D], F32)
    ident = kb.sb("ident", [128, 128], BF16)
    kb.dma(ident[:], c_ident, [], ["ident"])
    PB = [kb.ps("pb", [128, 512], F32) for _ in range(6)]
    PBK = ["pb%d" % i for i in range(6)]
    PT2 = [kb.ps("pt", [128, 1024], BF16) for _ in range(2)]
    PTK = ["pt0", "pt1"]
    prr = [0]
    ptr = [0]

    def pbank():
        i = prr[0]; prr[0] = (i + 1) % 6
        return PB[i], PBK[i]

    def ptbank():
        i = ptr[0]; ptr[0] = (i + 1) % 2
        return PT2[i], PTK[i]

    Gt = kb.sb("Gt", [128, 16, 247], BF16)
    E0 = kb.sb("E0", [128, 16, 128], BF16)
    E1 = kb.sb("E1", [128, 16, 128], BF16)
    wedge = kb.sb("wedge", [128, 128], BF16)
    kb.dma(wedge[:], c_wedge, [], ["wedge"])

    def build_tables():
        mk = kb.mark()
        relb = kb.sb("relb", [128, 32, 16], F32)
        kb.dma(relb[:].rearrange("p b h -> p (b h)"),
               bass.AP(tensor=rel_bias.tensor, offset=0, ap=[[0, 128], [1, 512]]), [], ["relb"])
        dl = kb.sb("dl", [128, 32, 16], F32)
        kb.v("dve", lambda: V.tensor_sub(out=dl[:, 1:32, :], in0=relb[:, 1:32, :], in1=relb[:, 0:31, :]), ["relb"], ["dl"])
        kb.v("dve", lambda: V.tensor_sub(out=dl[:, 0:1, :], in0=relb[:, 0:1, :], in1=relb[:, 31:32, :]), ["relb"], ["dl"])
        W = 503
        dist = kb.sb("dist", [128, W], F32)
        kb.dma(dist[:, 0:128], c_dist0, [], ["dist"])
        kb.dma(dist[:, 128:256], c_dist1, [], ["dist"])
        kb.dma(dist[:, 256:503], c_distg, [], ["dist"])
        acc = kb.sb("acc", [128, 16, W], F32)
        tmps = Rot(kb, "tmp01", [128, W], F32, 3)
        ACCK = [("acc", h) for h in range(16)]
        kb.v("dve", lambda: V.tensor_copy(out=acc[:], in_=bcast_ap(dl[:, 0, :], [[1, 16], [0, W]])), ["dl"], ACCK)
        for b in range(1, 33):
            tmp, tk = tmps.next()
            if b < 32:
                thr = C["thr"][b]
                kb.v("dve", lambda: V.tensor_single_scalar(out=tmp[:], in_=dist[:], scalar=thr, op=ALU.is_ge), ["dist"], [tk])
                for h in range(16):
                    kb.v("dve", lambda: V.scalar_tensor_tensor(out=acc[:, h, :], in0=tmp[:], scalar=dl[:, b, h:h + 1], in1=acc[:, h, :],
                                                               op0=ALU.mult, op1=ALU.add), [tk, "dl", ("acc", h)], [("acc", h)])
            else:
                kb.v("dve", lambda: V.tensor_scalar(out=tmp[:], in0=dist[:], scalar1=0.0, scalar2=-NEGM, op0=ALU.is_lt, op1=ALU.mult),
                     ["dist"], [tk])
                kb.v("dve", lambda: V.tensor_add(out=acc[:], in0=acc[:], in1=bcast_ap(tmp[:], [[0, 16], [1, W]])), ACCK + [tk], ACCK)
        kb.act(E0[:], acc[:, :, 0:128], AF.Exp, ACCK, ["E0"])
        kb.act(E1[:], acc[:, :, 128:256], AF.Exp, ACCK, ["E1"])
        kb.v("dve", lambda: V.tensor_copy(out=Gt[:], in_=acc[:, :, 256:503]), ACCK, ["Gt"])
        kb.release(mk)

    build_tables()

    HT_ALL = [("hT", t) for t in range(NT)]
    dbg_once = [True]

    def rmsnorm_to_hT(hT, g_row_ap):
        mk = kb.mark()
        gbc = kb.sb("gbc", [128, D], F32)
        kb.dma(gbc[:], g_row_ap, [], ["gbc"])
        junk = Rot(kb, "junk", [128, D], BF16, 2)
        ssr = Rot(kb, "ss", [128, 4], F32, 4)
        hbr = Rot(kb, "hb", [128, D], BF16, 3)

        def n_p1(t):
            jt, jk = junk.next(); ss, sk = ssr.next(); hb, hk = hbr.next()
            kb.act(jt[:], x_sb[:, t, :], AF.Square, [("x", t)], [jk, sk + "a"], accum_out=ss[:, 0:1])
            kb.act(ss[:, 1:2], ss[:, 0:1], AF.Sqrt, [sk + "a"], [sk + "b"], scale=1.0 / D, bias=1e-6)
            kb.v("dve", lambda: V.reciprocal(out=ss[:, 2:3], in_=ss[:, 1:2]), [sk + "b"], [sk + "c"])
            kb.v("dve", lambda: V.scalar_tensor_tensor(out=hb[:], in0=x_sb[:, t, :], scalar=ss[:, 2:3], in1=gbc[:],
                                                       op0=ALU.mult, op1=ALU.mult), [("x", t), sk + "c", "gbc"], [hk])
            return hb, hk

        def n_p2(t, hb, hk):
            pt, pk = ptbank()
            for k in range(8):
                kb.tr(pt[:, 128 * k:128 * k + 128], hb[:, 128 * k:128 * k + 128], ident[:], [hk, "ident"], [pk])
            kb.act(hT[:, :, 128 * t:128 * t + 128], pt[:].rearrange("p (k n) -> p k n", k=8), AF.Copy, [pk], [("hT", t)])

        hs_ = {}
        for t in range(NT + 1):
            if t < NT:
                hs_[t] = n_p1(t)
            if t >= 1:
                n_p2(t - 1, *hs_.pop(t - 1))


    def load_w(pool, wname, row0, c0, ncols, dup64=None):
        wt, wk = pool.next()
        src = wb[wname]
        if dup64 is None:
            kb.dma(wt[:, :, 0:ncols], src[row0:row0 + 1024, c0:c0 + ncols].rearrange("(k p) c -> p k c", p=128),
                   [("wb", wname)], [wk])
        else:
            for hh in range(2):
                kb.dma(wt[:, :, 64 * hh:64 * hh + 64], src[row0:row0 + 1024, c0:c0 + 64].rearrange("(k p) c -> p k c", p=128),
                       [("wb", wname)], [wk])
        return wt, wk

    def gemm_fm(hT, wt, wk, jb, ncols=128, c0=0):
        pb, pk = pbank()
        for k in range(8):
            kb.mm(pb[0:ncols, :], wt[:, k, c0:c0 + ncols], hT[:, k, 512 * jb:512 * jb + 512], k == 0, k == 7,
                  [wk] + [("hT", 4 * jb + i) for i in range(4)], [pk])
        return pb, pk

    def gemm_tm(hT, wt, wk, t, ncols):
        pb, pk = pbank()
        for k in range(8):
            kb.mm(pb[:, 0:ncols], hT[:, k, 128 * t:128 * t + 128], wt[:, k, 0:ncols], k == 0, k == 7,
                  [wk, ("hT", t)], [pk])
        return pb, pk

    def stage_proj(li, hT):
        mk = kb.mark()
        row0 = li * D
        wpool = Rot(kb, "wt", [128, 8, 512], BF16, 3)
        cosT = kb.sb("cosT", [128, T], F32); sinT = kb.sb("sinT", [128, T], F32)
        kb.dma(cosT[:], c_cos, [], ["cosT"]); kb.dma(sinT[:], c_sin, [], ["sinT"])
        retsc = kb.sb("retsc", [128, 12], F32)
        kb.dma(retsc[:], c_retsc, [], ["retsc"])
        xab = Rot(kb, "xab", [128, 2, 512], F32, 2)
        tmpr = Rot(kb, "rtmp", [128, 4, 512], F32, 2)
        rotr = Rot(kb, "rot", [128, 2, 512], BF16, 2)
        kzr = Rot(kb, "kz", [128, 4, 256], BF16, 2)
        stg = Rot(kb, "stg", [128, 512], BF16, 4)
        stgf = Rot(kb, "stgf", [128, 48], F32, 2)
        flip = [0]

        for (isk, obase, dst) in ((0, O_QR, P_qrT), (1, O_KR, P_krT)):
            sc = 1.0 / 16.0 if isk else 1.0
            for h in range(4):
                wt, wk = load_w(wpool, "w_in", row0, obase + 256 * h, 256)
                for jb in range(4):
                    pa, pak = gemm_fm(hT, wt, wk, jb, 128, 0)
                    pbb, pbk = gemm_fm(hT, wt, wk, jb, 128, 128)
                    xa, xk = xab.next()
                    kb.act(xa[:, 0, :], pa[:], AF.Copy, [pak], [xk + "a"], scale=sc)
                    kb.act(xa[:, 1, :], pbb[:], AF.Copy, [pbk], [xk + "b"], scale=sc)
                    tm, tk = tmpr.next()
                    cs = cosT[:, 512 * jb:512 * jb + 512]; sn = sinT[:, 512 * jb:512 * jb + 512]
                    kb.v("dve", lambda: V.tensor_mul(out=tm[:, 0, :], in0=xa[:, 0, :], in1=cs), [xk + "a", "cosT"], [tk + "0"])
                    kb.v("pool", lambda: G.tensor_mul(out=tm[:, 1, :], in0=xa[:, 1, :], in1=sn), [xk + "b", "sinT"], [tk + "1"])
                    kb.v("dve", lambda: V.tensor_mul(out=tm[:, 2, :], in0=xa[:, 0, :], in1=sn), [xk + "a", "sinT"], [tk + "2"])
                    kb.v("pool", lambda: G.tensor_mul(out=tm[:, 3, :], in0=xa[:, 1, :], in1=cs), [xk + "b", "cosT"], [tk + "3"])
                    ro, rk = rotr.next()
                    kb.v("dve", lambda: V.tensor_sub(out=ro[:, 0, :], in0=tm[:, 0, :], in1=tm[:, 1, :]), [tk + "0", tk + "1"], [rk + "a"])
                    kb.v("pool", lambda: G.tensor_add(out=ro[:, 1, :], in0=tm[:, 2, :], in1=tm[:, 3, :]), [tk + "2", tk + "3"], [rk + "b"])
                    kb.dma(dst[256 * h:256 * h + 256, 512 * jb:512 * jb + 512].rearrange("(c p) n -> p c n", p=128), ro[:],
                           [rk + "a", rk + "b"], [("P_r", isk, h, jb)], q="pool")
                    if isk:
                        kz, kzk = kzr.next()
                        pt, pk = ptbank()
                        for i in range(4):
                            for c in range(2):
                                kb.tr(pt[:, 256 * i + 128 * c:256 * i + 128 * c + 128], ro[:, c, 128 * i:128 * i + 128], ident[:],
                                      [rk + "a", rk + "b", "ident"], [pk])
                        kb.act(kz[:].rearrange("p i d -> p (i d)"), pt[:], AF.Copy, [pk, "retsc"], [kzk], scale=retsc[:, 4 + h:5 + h])
                        kb.dma(P_kz[512 * jb:512 * jb + 512, 256 * h:256 * h + 256].rearrange("(i p) d -> p i d", p=128), kz[:],
                               [kzk], [("P_kz", h, jb)], q="pool")

        def tm_group(obase, ncols_total, dst, func, dkey, dstf32=False):
            for c0 in range(0, ncols_total, 512):
                ncol = min(512, ncols_total - c0)
                wt, wk = load_w(wpool, "w_in", row0, obase + c0, ncol)
                for t in range(NT):
                    pb, pk = gemm_tm(hT, wt, wk, t, ncol)
                    if dstf32:
                        sg, sk = stgf.next()
                    else:
                        sg, sk = stg.next()
                    if func == AF.Copy and (flip[0] % 2 == 0):
                        kb.v("dve", lambda: V.tensor_copy(out=sg[:, 0:ncol], in_=pb[:, 0:ncol]), [pk], [sk])
                    else:
                        kb.act(sg[:, 0:ncol], pb[:, 0:ncol], func, [pk], [sk])
                    flip[0] += 1
                    kb.dma(dst[128 * t:128 * t + 128, c0:c0 + ncol], sg[:, 0:ncol], [sk], [(dkey, c0 // 512, t)], q="pool")

        tm_group(O_VR, 2048, P_v, AF.Copy, "P_v")
        tm_group(O_GR, 2048, P_sg, AF.Silu, "P_sg")
        tm_group(O_KV + 3 * 128, 128, P_vs, AF.Copy, "P_vs")
        tm_group(O_KV + 5 * 128, 128, P_vw, AF.Copy, "P_vw")
        tm_group(O_GATE, 48, P_gate, AF.Sigmoid, "P_gate", dstf32=True)
        tm_group(O_MA, 1024, P_ma, AF.Sigmoid, "P_ma")
        tm_group(O_MB, 1024, P_mb, AF.Sigmoid, "P_mb")

        def fm_chunk(c0, dst_rows, scale, dkey, dup=False):
            wt, wk = load_w(wpool, "w_in", row0, c0, 128, dup64=(True if dup else None))
            for jb in range(4):
                pb, pk = gemm_fm(hT, wt, wk, jb, 128, 0)
                sg, sk = stg.next()
                kb.act(sg[:], pb[:], AF.Copy, [pk], [sk], scale=scale)
                kb.dma(dst_rows[:, 512 * jb:512 * jb + 512], sg[:], [sk], [(dkey, jb)], q="pool")

        for c in range(8):
            fm_chunk(O_QN + 128 * c, P_qnT[128 * c:128 * c + 128, :], 0.125, ("P_qnT", c))
        fm_chunk(O_KV + 0, P_kcT, 1.0, "P_kcT")
        fm_chunk(O_KV + 128, P_vcT, 1.0, "P_vcT")
        for g in range(2):
            fm_chunk(O_KV + 256 + 64 * g, P_ksT[128 * g:128 * g + 128, :], 1.0, ("P_ksT", g), dup=True)
            fm_chunk(O_KV + 512 + 64 * g, P_kwT[128 * g:128 * g + 128, :], 1.0, ("P_kwT", g), dup=True)
        kb.release(mk)

    PROJ_R_KEYS = [("P_r", isk, h, jb) for isk in range(2) for h in range(4) for jb in range(4)]

    def stage_retention(li):
        mk = kb.mark()
        dmask = kb.sb("dmask", [128, 4, 128], F32)
        kb.dma(dmask[:].rearrange("p h n -> p (h n)"), c_dmask, [], ["dmask"])
        retsc = kb.sb("retsc", [128, 12], F32)
        kb.dma(retsc[:], c_retsc, [], ["retsc"])
        R32s = [kb.sb("R32", [128, 2, 512], F32) for _ in range(4)]
        Rbs = [kb.sb("Rb", [128, 2, 512], BF16) for _ in range(4)]
        qTr = Rot(kb, "qT", [128, 2, 128], BF16, 8)
        kTr = Rot(kb, "kT", [128, 2, 128], BF16, 8)
        kzr = Rot(kb, "kzl", [128, 256], BF16, 8)
        vr = Rot(kb, "vl", [128, 512], BF16, 8)
        sgr = Rot(kb, "sgl", [128, 512], BF16, 8)
        sTr = Rot(kb, "sTb", [128, 128], BF16, 4)
        osr = Rot(kb, "osb", [128, 512], F32, 4)
        str_ = Rot(kb, "stat", [128, 16], F32, 6)
        zr = Rot(kb, "z", [128, 512], BF16, 4)
        z2r = Rot(kb, "z2", [128, 512], F32, 4)
        zTr = Rot(kb, "zT", [128, 4, 128], BF16, 4)
        for h in range(4):
            kb.v("pool", lambda: G.memset(R32s[h][:], 0.0), [], ["R32a%d" % h, "R32b%d" % h])
            kb.v("pool", lambda: G.memset(Rbs[h][:], 0.0), [], ["Rba%d" % h, "Rbb%d" % h])
        units = [(c, h) for c in range(NT) for h in range(4)]

        def r_load(c, h):
            jb = c // 4
            qT, qk = qTr.next(); kT, kk = kTr.next(); kz, kzk = kzr.next(); vv, vk = vr.next(); sg, sgk = sgr.next()
            cols = slice(128 * c, 128 * c + 128)
            kb.dma(qT[:], P_qrT[256 * h:256 * h + 256, cols].rearrange("(c p) n -> p c n", p=128), [("P_r", 0, h, jb)], [qk])
            kb.dma(kT[:], P_krT[256 * h:256 * h + 256, cols].rearrange("(c p) n -> p c n", p=128), [("P_r", 1, h, jb)], [kk])
            kb.dma(kz[:], P_kz[cols, 256 * h:256 * h + 256], [("P_kz", h, jb)], [kzk])
            kb.dma(vv[:], P_v[cols, 512 * h:512 * h + 512], [("P_v", h, c)], [vk])
            kb.dma(sg[:], P_sg[cols, 512 * h:512 * h + 512], [("P_sg", h, c)], [sgk])
            return dict(qT=qT, qk=qk, kT=kT, kk=kk, kz=kz, kzk=kzk, vv=vv, vk=vk, sg=sg, sgk=sgk)

        def r_p1(c, h, L):
            Rb = Rbs[h]
            ps_, psk = pbank()
            for cc in range(2):
                kb.mm(ps_[:, 0:128], L["kT"][:, cc, :], L["qT"][:, cc, :], cc == 0, cc == 1, [L["kk"], L["qk"]], [psk])
            sT, sTk = sTr.next()
            kb.v("dve", lambda: V.tensor_mul(out=sT[:], in0=ps_[:, 0:128], in1=dmask[:, h, :]), [psk, "dmask"], [sTk])
            po, pok = pbank()
            kb.mm(po[:], sT[:], L["vv"][:], True, False, [sTk, L["vk"]], [pok])
            for cc in range(2):
                kb.mm(po[:], L["qT"][:, cc, :], Rb[:, cc, :], False, cc == 1, [L["qk"], "Rb" + "ab"[cc] + str(h)], [pok])
            osb, osk = osr.next()
            kb.act(osb[:], po[:], AF.Copy, [pok, "retsc"], [osk], scale=retsc[:, h:h + 1])
            st, stk = str_.next()
            kb.v("dve", lambda: V.bn_stats(out=st[:, 0:6], in_=osb[:]), [osk], [stk + "a"])
            kb.v("dve", lambda: V.bn_aggr(out=st[:, 6:8], in_=st[:, 0:6]), [stk + "a"], [stk + "b"])
            kb.act(st[:, 8:9], st[:, 7:8], AF.Sqrt, [stk + "b"], [stk + "c"], bias=1e-5)
            L.update(osb=osb, osk=osk, st=st, stk=stk)

        def r_p2(c, h, L):
            gch = C["gchunk"][h]
            R32 = R32s[h]; Rb = Rbs[h]
            st, stk, osb, osk = L["st"], L["stk"], L["osb"], L["osk"]
            cols = slice(128 * c, 128 * c + 128)
            if c < NT - 1:
                for cc in range(2):
                    pr, prk = pbank()
                    kb.mm(pr[:], L["kz"][:, 128 * cc:128 * cc + 128], L["vv"][:], True, True, [L["kzk"], L["vk"]], [prk])
                    kb.v("dve", lambda: V.scalar_tensor_tensor(out=R32[:, cc, :], in0=R32[:, cc, :], scalar=gch, in1=pr[:],
                                                               op0=ALU.mult, op1=ALU.add), ["R32" + "ab"[cc] + str(h), prk], ["R32" + "ab"[cc] + str(h)])
                    kb.act(Rb[:, cc, :], R32[:, cc, :], AF.Copy, ["R32" + "ab"[cc] + str(h)], ["Rb" + "ab"[cc] + str(h)])
            kb.v("dve", lambda: V.reciprocal(out=st[:, 9:10], in_=st[:, 8:9]), [stk + "c"], [stk + "d"])
            z2, z2k = z2r.next()
            kb.v("dve", lambda: V.tensor_scalar(out=z2[:], in0=osb[:], scalar1=st[:, 6:7], scalar2=st[:, 9:10],
                                                op0=ALU.subtract, op1=ALU.mult), [osk, stk + "b", stk + "d"], [z2k])
            z, zk = zr.next()
            kb.v("dve", lambda: V.tensor_mul(out=z[:], in0=z2[:], in1=L["sg"][:]), [z2k, L["sgk"]], [zk])
            L.update(z=z, zk=zk)

        def r_p3(c, h, L):
            z, zk = L["z"], L["zk"]
            cols = slice(128 * c, 128 * c + 128)
            pt, pk = ptbank()
            for e in range(4):
                kb.tr(pt[:, 128 * e:128 * e + 128], z[:, 128 * e:128 * e + 128], ident[:], [zk, "ident"], [pk])
            zT, zTk = zTr.next()
            kb.act(zT[:].rearrange("p e n -> p (e n)"), pt[:, 0:512], AF.Copy, [pk], [zTk])
            kb.dma(Z_T[512 * h:512 * h + 512, cols].rearrange("(e p) n -> p e n", p=128), zT[:], [zTk], [("Z_T", c, h)], q="pool")

        NU = len(units)
        PRE = 4
        Ls = {}
        for n in range(min(PRE, NU)):
            Ls[n] = r_load(*units[n])
        for n in range(NU + 2):
            if n + PRE < NU:
                Ls[n + PRE] = r_load(*units[n + PRE])
            if n < NU:
                r_p1(*units[n], Ls[n])
            if 1 <= n <= NU:
                r_p2(*units[n - 1], Ls[n - 1])
            if n >= 2:
                r_p3(*units[n - 2], Ls.pop(n - 2))
        kb.release(mk)

    def stage_nsa(li):
        mk = kb.mark()
        keep = kb.sb("keep", [128, 16, 32], F32); addm = kb.sb("addm", [128, 16, 32], F32)
        kb.dma(keep[:].rearrange("p t j -> p (t j)"), c_keep, [], ["keep"])
        kb.dma(addm[:].rearrange("p t j -> p (t j)"), c_addm, [], ["addm"])
        gates = kb.sb("gates", [128, 16, 48], F32)
        kb.dma(gates[:], P_gate.rearrange("(t p) c -> p t c", p=128), [("P_gate", 0, t) for t in range(NT)], ["gates"])
        if stop_after == "nsa0":
            raise _Stop()
        kcx = kb.sb("kcx", [128, 2, 2, 128], BF16)
        kb.v("pool", lambda: G.memset(kcx[:], 0.0), [], ["kcx0", "kcx1"])
        vca = kb.sb("vcaug", [128, 2, 97], BF16)
        kb.v("pool", lambda: G.memset(vca[:], 1.0), [], ["vca0", "vca1"])
        for g in range(2):
            kb.dma(vca[0:127, g, 64:96], c_overlap, [], ["vca%d" % g])
        mk2 = kb.mark()
        w1 = Rot(kb, "w1", [128, 32, 256], BF16, 2)
        for kv in range(2):
            nm1 = "cmp_w1_v" if kv else "cmp_w1_k"
            nm2 = "cmp_w2_v" if kv else "cmp_w2_k"
            pos_d = cmp_pos_v if kv else cmp_pos_k
            w1t, w1k = w1.next()
            for hh in range(2):
                kb.dma(w1t[64 * hh:64 * hh + 64, :, :], wb[nm1][2048 * li:2048 * li + 2048, :].rearrange("(l d) h -> d l h", d=64),
                       [("wb", nm1)], [w1k])
            w2t = kb.sb("w2t", [128, 2, 128], BF16)
            for hh in range(2):
                kb.dma(w2t[:, :, 64 * hh:64 * hh + 64], wb[nm2][256 * li:256 * li + 256, :].rearrange("(c p) d -> p c d", p=128),
                       [("wb", nm2)], ["w2t"])
            posl = kb.sb("posl", [32, 64], F32)
            kb.dma(posl[:], pos_d[32 * li:32 * li + 32, :], [], ["posl"])
            posb = kb.sb("posb", [32, 64], BF16)
            kb.v("dve", lambda: V.tensor_copy(out=posb[:], in_=posl[:]), ["posl"], ["posb"])
            pt, pk = ptbank()
            kb.tr(pt[0:64, 0:32], posb[:], ident[0:32, 0:32], ["posb", "ident"], [pk])
            posT = kb.sb("posT", [128, 32], F32)
            kb.act(posT[0:64, :], pt[0:64, 0:32], AF.Copy, [pk], ["posT"])
            kb.act(posT[64:128, :], pt[0:64, 0:32], AF.Copy, [pk], ["posT"])
            kvT = kb.sb("kvT", [128, T], BF16)
            src = P_vcT if kv else P_kcT
            kb.dma(kvT[:], src, [("P_vcT" if kv else "P_kcT", jb) for jb in range(4)], ["kvT"])
            kvA = kb.sb("kvA", [128, T], BF16); kvB = kb.sb("kvB", [128, T], BF16)
            kb.v("dve", lambda: V.tensor_add(out=kvA[:].rearrange("p (a b) -> p a b", b=16), in0=kvT[:].rearrange("p (a b) -> p a b", b=16),
                                             in1=bcast_ap(posT[:, 0:16], [[0, 128], [1, 16]])), ["kvT", "posT"], ["kvA"])
            kb.v("dve", lambda: V.tensor_add(out=kvB[:].rearrange("p (a b) -> p a b", b=16), in0=kvT[:].rearrange("p (a b) -> p a b", b=16),
                                             in1=bcast_ap(posT[:, 16:32], [[0, 128], [1, 16]])), ["kvT", "posT"], ["kvB"])
            for g in range(2):
                pr = slice(64 * g, 64 * g + 64)
                gT = kb.sb("gT", [128, 2, 128], BF16)
                for ch in range(2):
                    pb, pk_ = pbank()
                    for l in range(32):
                        srcT = kvA if l < 16 else kvB
                        rhs = bcast_ap(srcT[pr, l:l + 1], [[16, 127]])
                        kb.mm(pb[:, 0:127], w1t[pr, l, 128 * ch:128 * ch + 128], rhs, l == 0, l == 31,
                              [w1k, "kvA", "kvB"], [pk_])
                    hs = kb.sb("hs", [128, 4, 127], F32)
                    kb.act(hs[:, 0, :], pb[:, 0:127], AF.Copy, [pk_], ["hs0"])
                    kb.v("dve", lambda: V.tensor_mul(out=hs[:, 1, :], in0=hs[:, 0, :], in1=hs[:, 0, :]), ["hs0"], ["hs1"])
                    kb.v("dve", lambda: V.tensor_scalar(out=hs[:, 2, :], in0=hs[:, 1, :], scalar1=0.044715, scalar2=1.0,
                                                        op0=ALU.mult, op1=ALU.add), ["hs1"], ["hs2"])
                    kb.v("dve", lambda: V.tensor_mul(out=hs[:, 3, :], in0=hs[:, 2, :], in1=hs[:, 0, :]), ["hs2", "hs0"], ["hs3"])
                    kb.act(hs[:, 1, :], hs[:, 3, :], AF.Sigmoid, ["hs3", "hs2"], ["hs1"], scale=2.0 * math.sqrt(2.0 / math.pi))
                    kb.v("dve", lambda: V.tensor_mul(out=gT[:, ch, 0:127], in0=hs[:, 1, :], in1=hs[:, 0, :]), ["hs1", "hs0"], ["gT%d" % ch])
                pb, pk_ = pbank()
                if kv == 0:
                    for ch in range(2):
                        kb.mm(pb[:, 0:127], w2t[:, ch, :], gT[:, ch, 0:127], ch == 0, ch == 1, ["w2t", "gT%d" % ch], [pk_])
                    for par in range(2):
                        kb.act(kcx[64 * par:64 * par + 64, par, g, 0:127], pb[64 * par:64 * par + 64, 0:127], AF.Copy, [pk_], ["kcx%d" % g])
                else:
                    for ch in range(2):
                        kb.mm(pb[0:127, 0:64], gT[:, ch, 0:127], w2t[:, ch, 0:64], ch == 0, ch == 1, ["w2t", "gT%d" % ch], [pk_])
                    kb.act(vca[0:127, g, 0:64], pb[0:127, 0:64], AF.Copy, [pk_], ["vca%d" % g])
        kb.release(mk2)
        if stop_after == "nsa1":
            raise _Stop()

        PTr = Rot(kb, "PT", [128, 512], BF16, 4)
        for g in range(2):
            mk3 = kb.mark()
            ksx = kb.sb("ksx", [128, 2, T], BF16); kwx = kb.sb("kwx", [128, 2, T], BF16)
            kb.v("pool", lambda: G.memset(kwx[:], 0.0), [], ["kwx"])
            for par in range(2):
                own = slice(64 * par, 64 * par + 64)
                oth = slice(64 * (1 - par), 64 * (1 - par) + 64)
                kb.dma(ksx[own, par, :], P_ksT[128 * g + 64 * par:128 * g + 64 * par + 64, :], [(("P_ksT", g), jb) for jb in range(4)], ["ksx"])
                kb.dma(ksx[oth, par, :], c_expand, [], ["ksx"])
                kb.dma(kwx[own, par, :], P_kwT[128 * g + 64 * par:128 * g + 64 * par + 64, :], [(("P_kwT", g), jb) for jb in range(4)], ["kwx"])
            vsa = kb.sb("vsa", [128, 16, 65], BF16); vwa = kb.sb("vwa", [128, 16, 65], BF16)
            kb.v("pool", lambda: G.memset(vsa[:], 1.0), [], ["vsa"])
            kb.v("pool", lambda: G.memset(vwa[:], 1.0), [], ["vwa"])
            kb.dma(vsa[:, :, 0:64], P_vs[:, 64 * g:64 * g + 64].rearrange("(t p) d -> p t d", p=128),
                   [("P_vs", 0, t) for t in range(NT)], ["vsa"])
            kb.dma(vwa[:, :, 0:64], P_vw[:, 64 * g:64 * g + 64].rearrange("(t p) d -> p t d", p=128),
                   [("P_vw", 0, t) for t in range(NT)], ["vwa"])
            qs = kb.sb("qs", [128, 8, T], BF16)
            kb.v("pool", lambda: G.memset(qs[:, 0:4, :], 0.0), [], [("qsz", 0)])
            kb.v("pool", lambda: G.memset(qs[:, 4:8, :], 0.0), [], [("qsz", 1)])
            for hh in range(8):
                par = hh % 2
                own = slice(64 * par, 64 * par + 64)
                kb.dma(qs[own, hh, :], P_qnT[128 * (4 * g + hh // 2) + 64 * par:128 * (4 * g + hh // 2) + 64 * par + 64, :],
                       [(("P_qnT", 4 * g + hh // 2), jb) for jb in range(4)] + [("qsz", hh // 4)], [("qsq", hh)])
            og = kb.sb("og", [128, 16, 512], F32)
            ocr = Rot(kb, "ocimp", [128, 8, 96], F32, 2)
            zcr = Rot(kb, "zc", [128, 127], F32, 3)
            pcr = Rot(kb, "pc", [128, 128], BF16, 4)
            pTr = Rot(kb, "pTc", [128, 128], BF16, 4)
            smr = Rot(kb, "sm", [128, 12], F32, 4)
            impr = Rot(kb, "imp", [128, 4, 32], F32, 2)
            ngr = Rot(kb, "ngm", [128, 64], BF16, 2)
            for i in range(2):
                kb.v("pool", lambda: G.memset(ngr.tiles[i][:], 0.0), [], [ngr.keys[i]])
            citems = [(t, hh) for t in range(NT) for hh in range(8)]
            octile = {}
            crr = [0]

            def cA(t, hh):
                pr = slice(64 * (hh % 2), 64 * (hh % 2) + 64)
                h = 8 * g + hh
                j4 = crr[0]; crr[0] = (j4 + 1) % 4
                pb, pk = PB[j4], PBK[j4]
                kb.mm(pb[:, 0:127], qs[:, hh, 128 * t:128 * t + 128], kcx[:, hh % 2, g, 0:127], True, True,
                      [("qsq", hh), ("qsz", hh // 4), ("qsm", hh % 2, t), "kcx%d" % g], [pk])
                zc, zk = zcr.next()
                kb.v("dve", lambda: V.tensor_add(out=zc[:], in0=pb[:, 0:127], in1=Gt[:, h, 120 - 8 * t:120 - 8 * t + 127]), [pk, "Gt"], [zk])
                pc, pck = pcr.next()
                kb.act(pc[:, 0:127], zc[:], AF.Exp, [zk], [pck])
                return pc, pck

            def cB(t, hh, pc, pck):
                pt, ptk = ptbank()
                kb.tr(pt[0:127, 0:128], pc[:, 0:127], ident[:], [pck, "ident"], [ptk])
                pT, pTk = pTr.next()
                kb.act(pT[0:127, :], pt[0:127, 0:128], AF.Copy, [ptk], [pTk])
                return pT, pTk

            def cC(t, hh, pT, pTk):
                h = 8 * g + hh
                if hh == 0:
                    octile[t] = ocr.next()
                oc, ock = octile[t]
                po, pok = PB[4 + hh // 4], PBK[4 + hh // 4]
                P4 = po[:].rearrange("p (i c) -> p i c", c=128)
                kb.mm(P4[:, hh % 4, 0:97], pT[0:127, :], vca[0:127, g, :], True, True, [pTk, "vca%d" % g], [pok], sgc=True)
                if hh % 4 == 3:
                    j0 = hh - 3
                    sm, smk = smr.next()
                    kb.v("dve", lambda: V.tensor_scalar_max(out=sm[:, 0:4], in0=P4[:, :, 96], scalar1=1e-30), [pok], [smk + "a"])
                    kb.v("dve", lambda: V.reciprocal(out=sm[:, 4:8], in_=sm[:, 0:4]), [smk + "a"], [smk + "b"])
                    ock4 = [(ock, x_) for x_ in range(j0, j0 + 4)]
                    kb.v("dve", lambda: V.tensor_tensor(out=oc[:, j0:j0 + 4, :], in0=P4[:, :, 0:96], in1=bcast_ap(sm[:, 4:8], [[1, 4], [0, 96]]), op=ALU.mult),
                         [pok, smk + "b"], ock4)
                    gl = gates[:, t, 3 * (8 * g + j0):3 * (8 * g + j0) + 1]
                    kb.v("pool", lambda: G.tensor_tensor(out=og[:, t, 64 * j0:64 * j0 + 256].rearrange("p (i c) -> p i c", c=64), in0=oc[:, j0:j0 + 4, 0:64],
                                                         in1=bcast_ap(gl, [[3, 4], [0, 64]]), op=ALU.mult),
                         ock4 + ["gates"], [("og", t, x_) for x_ in range(j0, j0 + 4)])
                if hh == 7:
                    im, imk = impr.next()
                    kb.v("dve", lambda: V.tensor_reduce(out=im[:, 0, :], in_=oc[:, :, 64:96].rearrange("p h j -> p j h"), axis=AX.X, op=ALU.add),
                         [(ock, x_) for x_ in range(8)], [imk + "0"])
                    kb.v("dve", lambda: V.tensor_mul(out=im[:, 1, :], in0=im[:, 0, :], in1=keep[:, t, :]), [imk + "0", "keep"], [imk + "1"])
                    kb.v("dve", lambda: V.tensor_add(out=im[:, 2, :], in0=im[:, 1, :], in1=addm[:, t, :]), [imk + "1", "addm"], [imk + "2"])
                    sm2, smk2 = smr.next()
                    kb.v("dve", lambda: V.max(out=sm2[:, 0:8], in_=im[:, 2, :]), [imk + "2"], [smk2 + "a"])
                    kb.v("dve", lambda: V.tensor_scalar(out=im[:, 3, :], in0=im[:, 2, :], scalar1=sm2[:, 7:8], scalar2=None, op0=ALU.is_ge),
                         [imk + "2", smk2 + "a"], [imk + "3"])
                    ng, ngk = ngr.next()
                    kb.v("dve", lambda: V.tensor_scalar(out=ng[:, 0:32], in0=im[:, 3, :], scalar1=-1.0, scalar2=NEGM, op0=ALU.add, op1=ALU.mult),
                         [imk + "3"], [ngk])
                    pt, ptk = ptbank()
                    kb.tr(pt[0:64, 0:128], ng[:], ident[:], [ngk, "ident"], [ptk])
                    for par in range(2):
                        oth = slice(64 * (1 - par), 64 * (1 - par) + 64)
                        kb.act(bcast_ap(qs[oth, par, 128 * t:128 * t + 128], [[2 * T, 4], [1, 128]]),
                               bcast_ap(pt[0:64, 0:128], [[0, 4], [1, 128]]), AF.Copy,
                               [ptk, ("qsz", 0), ("qsz", 1)], [("qsm", par, t)])

            nci = len(citems)
            hA = {}
            hB = {}
            for n in range(nci + 2):
                if n < nci:
                    hA[n] = cA(*citems[n])
                if 1 <= n <= nci:
                    hB[n - 1] = cB(*citems[n - 1], *hA.pop(n - 1))
                if 2 <= n:
                    cC(*citems[n - 2], *hB.pop(n - 2))
            if stop_after == "nsa2":
                raise _Stop()
            LOOK = 2
            STB, STK = PB[0:4], PBK[0:4]
            ACB, ACK = PB[4:6], PBK[4:6]
            tmpr = Rot(kb, "fin", [128, 4, 64], F32, 2)
            units = [(hh, qb, br) for hh in range(8) for qb in range(4) for br in range(2)]
            items = []
            for ui, (hh, qb, br) in enumerate(units):
                kt_lo = 0 if br == 0 else max(0, 4 * qb - 4)
                for kt in range(kt_lo, 4 * qb + 4):
                    items.append((ui, hh, qb, br, kt, kt == kt_lo, kt == 4 * qb + 3))

            def sQK(n, item):
                ui, hh, qb, br, kt, isfirst, islast = item
                h = 8 * g + hh
                pr = slice(64 * (hh % 2), 64 * (hh % 2) + 64)
                c = hh // 2
                kx_, kxk = (ksx, "ksx") if br == 0 else (kwx, "kwx")
                st_, stk_ = STB[n % 4], STK[n % 4]
                par = hh % 2
                vi = [i for i in range(4) if 0 <= 4 * qb + i - kt and not (br == 1 and 4 * qb + i - kt > 4)]
                c0_, c1_ = 128 * vi[0], 128 * vi[-1] + 128
                kb.mm(st_[:, c0_:c1_], kx_[:, par, 128 * kt:128 * kt + 128], qs[:, hh, 512 * qb + c0_:512 * qb + c1_], True, True,
                      [kxk, ("qsq", hh), ("qsz", hh // 4)] + [("qsm", par, 4 * qb + i) for i in vi], [stk_])
                PTt, PTk = PTr.next()
                kb.act(PTt[:, c0_:c1_], st_[:, c0_:c1_], AF.Exp, [stk_], [(PTk, i) for i in range(4)])
                for i in range(4):
                    off = 4 * qb + i - kt
                    if off < 0 or (br == 1 and off > 4):
                        continue
                    sl = slice(128 * i, 128 * i + 128)
                    if off == 0:
                        kb.v("dve", lambda: V.tensor_mul(out=PTt[:, sl], in0=PTt[:, sl], in1=E0[:, h, :]), [(PTk, i), "E0"], [(PTk, i)])
                    elif off == 1:
                        kb.v("pool", lambda: G.tensor_mul(out=PTt[:, sl], in0=PTt[:, sl], in1=E1[:, h, :]), [(PTk, i), "E1"], [(PTk, i)])
                    elif off == 4 and br == 1:
                        kb.v("pool", lambda: G.tensor_mul(out=PTt[:, sl], in0=PTt[:, sl], in1=wedge[:]), [(PTk, i), "wedge"], [(PTk, i)])
                return PTt, PTk

            def sPV(item, PTt, PTk):
                ui, hh, qb, br, kt, isfirst, islast = item
                h = 8 * g + hh
                va, vak = (vsa, "vsa") if br == 0 else (vwa, "vwa")
                accb, acck = ACB[ui % 2], ACK[ui % 2]
                A = accb[:].rearrange("p (i c) -> p i c", c=128)
                firstmm = isfirst
                for i in range(4):
                    off = 4 * qb + i - kt
                    if off < 0 or (br == 1 and off > 4):
                        continue
                    sl = slice(128 * i, 128 * i + 128)
                    kb.mm(A[:, i, 0:65], PTt[:, sl], va[:, kt, :], firstmm, kt == 4 * qb + i, [(PTk, i), vak], [acck], sgc=True)
                    firstmm = False
                if islast:
                    sm, smk = smr.next()
                    kb.v("dve", lambda: V.tensor_scalar_max(out=sm[:, 0:4], in0=A[:, :, 64], scalar1=1e-30), [acck], [smk + "a"])
                    kb.v("dve", lambda: V.reciprocal(out=sm[:, 4:8], in_=sm[:, 0:4]), [smk + "a"], [smk + "b"])
                    kb.v("dve", lambda: V.tensor_mul(out=sm[:, 8:12], in0=sm[:, 4:8], in1=gates[:, 4 * qb:4 * qb + 4, 3 * h + 1 + br]),
                         [smk + "b", "gates"], [smk + "c"])
                    tm, tmk = tmpr.next()
                    kb.v("dve", lambda: V.tensor_tensor(out=tm[:], in0=A[:, :, 0:64], in1=bcast_ap(sm[:, 8:12], [[1, 4], [0, 64]]), op=ALU.mult),
                         [acck, smk + "c"], [tmk])
                    ogk = [("og", 4 * qb + i, hh) for i in range(4)]
                    kb.v("pool", lambda: G.tensor_add(out=og[:, 4 * qb:4 * qb + 4, 64 * hh:64 * hh + 64],
                                                      in0=og[:, 4 * qb:4 * qb + 4, 64 * hh:64 * hh + 64], in1=tm[:]), ogk + [tmk], ogk)

            nit = len(items)
            hq = {}
            for n in range(nit + LOOK):
                if n < nit:
                    hq[n] = sQK(n, items[n])
                if n >= LOOK:
                    sPV(items[n - LOOK], *hq.pop(n - LOOK))
            obr = Rot(kb, "ob", [128, 512], BF16, 2)
            oTr = Rot(kb, "oT", [128, 4, 128], BF16, 2)
            for t in range(NT):
                ob, obk = obr.next()
                kb.act(ob[:], og[:, t, :], AF.Copy, [("og", t, hh) for hh in range(8)], [obk])
                pt, ptk = ptbank()
                for e in range(4):
                    kb.tr(pt[:, 128 * e:128 * e + 128], ob[:, 128 * e:128 * e + 128], ident[:], [obk, "ident"], [ptk])
                oT, oTk = oTr.next()
                kb.act(oT[:].rearrange("p e n -> p (e n)"), pt[:, 0:512], AF.Copy, [ptk], [oTk])
                kb.dma(ON_T[512 * g:512 * g + 512, 128 * t:128 * t + 128].rearrange("(e p) n -> p e n", p=128), oT[:], [oTk],
                       [("ON_T", g, t)], q="pool")
            kb.release(mk3)
        kb.release(mk)

    prr2 = [0]

    def stage_merge(li):
        mk = kb.mark()
        mT = kb.sb("mT", [128, 8, T], BF16)
        mk2 = kb.mark()
        wor = kb.sb("wor", [128, 16, D], BF16)
        won = kb.sb("won", [128, 8, D], BF16)
        for k4 in range(4):
            kb.dma(wor[:, 4 * k4:4 * k4 + 4, :], wb["w_o_ret"][2048 * li + 512 * k4:2048 * li + 512 * k4 + 512, :].rearrange("(k p) c -> p k c", p=128),
                   [("wb", "w_o_ret")], ["wor"])
        for k4 in range(2):
            kb.dma(won[:, 4 * k4:4 * k4 + 4, :], wb["w_o_nsa"][D * li + 512 * k4:D * li + 512 * k4 + 512, :].rearrange("(k p) c -> p k c", p=128),
                   [("wb", "w_o_nsa")], ["won"])
        zTr = Rot(kb, "zTl", [128, 16, 128], BF16, 2)
        oTr = Rot(kb, "oTl", [128, 8, 128], BF16, 2)
        sar = Rot(kb, "sa", [128, D], BF16, 2)
        sbr = Rot(kb, "sb", [128, D], BF16, 2)
        t1r = Rot(kb, "t1", [128, 512], F32, 2)
        t2r = Rot(kb, "t2", [128, 512], F32, 2)
        mbr = Rot(kb, "mb", [128, D], BF16, 2)
        for t in range(NT):
            cols = slice(128 * t, 128 * t + 128)
            zT, zk = zTr.next(); oT, ok = oTr.next(); sa, sak = sar.next(); sb_, sbk = sbr.next()
            kb.dma(zT[:], Z_T[:, cols].rearrange("(k p) n -> p k n", p=128), [("Z_T", t, h_) for h_ in range(4)], [zk])
            kb.dma(oT[:], ON_T[:, cols].rearrange("(k p) n -> p k n", p=128), [("ON_T", 0, t), ("ON_T", 1, t)], [ok])
            kb.dma(sa[:], P_ma[cols, :], [("P_ma", 0, t), ("P_ma", 1, t)], [sak])
            kb.dma(sb_[:], P_mb[cols, :], [("P_mb", 0, t), ("P_mb", 1, t)], [sbk])
            mb, mbk = mbr.next()
            for half in range(2):
                hs = slice(512 * half, 512 * half + 512)
                pr_, prk = pbank()
                for k in range(16):
                    kb.mm(pr_[:], zT[:, k, :], wor[:, k, hs], k == 0, k == 15, [zk, "wor"], [prk])
                pn, pnk = pbank()
                for k in range(8):
                    kb.mm(pn[:], oT[:, k, :], won[:, k, hs], k == 0, k == 7, [ok, "won"], [pnk])
                t1, t1k = t1r.next(); t2, t2k = t2r.next()
                kb.v("dve", lambda: V.tensor_mul(out=t1[:], in0=pr_[:], in1=sa[:, hs]), [prk, sak], [t1k])
                kb.v("dve", lambda: V.tensor_mul(out=t2[:], in0=pn[:], in1=sb_[:, hs]), [pnk, sbk], [t2k])
                kb.v("pool", lambda: G.tensor_add(out=mb[:, hs], in0=t1[:], in1=t2[:]), [t1k, t2k], [(mbk, half)])
            pt, ptk = ptbank()
            for k in range(8):
                kb.tr(pt[:, 128 * k:128 * k + 128], mb[:, 128 * k:128 * k + 128], ident[:], [(mbk, 0), (mbk, 1), "ident"], [ptk])
            kb.act(mT[:, :, cols], pt[:].rearrange("p (k n) -> p k n", k=8), AF.Copy, [ptk], [("mT", t)])
        kb.release(mk2)
        wo = kb.sb("wo", [128, 8, D], BF16)
        for k4 in range(2):
            kb.dma(wo[:, 4 * k4:4 * k4 + 4, :], wb["w_out"][D * li + 512 * k4:D * li + 512 * k4 + 512, :].rearrange("(k p) c -> p k c", p=128),
                   [("wb", "w_out")], ["wo"])
        for t in range(NT):
            for half in range(2):
                hs = slice(512 * half, 512 * half + 512)
                pb, pk = pbank()
                for k in range(8):
                    kb.mm(pb[:], mT[:, k, 128 * t:128 * t + 128], wo[:, k, hs], k == 0, k == 7, [("mT", t), "wo"], [pk])
                kb.v("dve", lambda: V.tensor_add(out=x_sb[:, t, hs], in0=x_sb[:, t, hs], in1=pb[:]), [("x", t), pk], [("x", t)])
        kb.release(mk)

    def stage_ffn(li, hT):
        mk = kb.mark()
        wpool = Rot(kb, "wf", [128, 8, 256], BF16, 3)
        sar = Rot(kb, "fsa", [128, 512], F32, 2)
        ur = Rot(kb, "fu", [128, 512], BF16, 3)
        for j in range(22):
            wt, wk = wpool.next()
            kb.dma(wt[:, :, 0:128], wb["w_ffn_in"][D * li:D * li + D, 128 * j:128 * j + 128].rearrange("(k p) c -> p k c", p=128),
                   [("wb", "w_ffn_in")], [wk])
            kb.dma(wt[:, :, 128:256], wb["w_ffn_in"][D * li:D * li + D, FFN + 128 * j:FFN + 128 * j + 128].rearrange("(k p) c -> p k c", p=128),
                   [("wb", "w_ffn_in")], [wk])
            for jb in range(4):
                pa, pak = gemm_fm(hT, wt, wk, jb, 128, 0)
                pb, pbk = gemm_fm(hT, wt, wk, jb, 128, 128)
                sa, sak = sar.next()
                kb.act(sa[:], pa[:], AF.Silu, [pak], [sak])
                u, uk = ur.next()
                kb.v("dve", lambda: V.tensor_mul(out=u[:], in0=pb[:], in1=sa[:]), [pbk, sak], [uk])
                kb.dma(U_T[128 * j:128 * j + 128, 512 * jb:512 * jb + 512], u[:], [uk], [("U_T", j, jb)], q="pool")
        kb.release(mk)
        mk = kb.mark()
        wfo = kb.sb("wfo", [128, 22, D], BF16)
        for k0 in range(0, 22, 4):
            k1 = min(22, k0 + 4)
            kb.dma(wfo[:, k0:k1, :], wb["w_ffn_out"][FFN * li + 128 * k0:FFN * li + 128 * k1, :].rearrange("(k p) c -> p k c", p=128),
                   [("wb", "w_ffn_out")], ["wfo"])
        uTr = Rot(kb, "uTl", [128, 22, 128], BF16, 2)
        for t in range(NT):
            uT, uk = uTr.next()
            kb.dma(uT[:], U_T[:, 128 * t:128 * t + 128].rearrange("(k p) n -> p k n", p=128), [("U_T", j, t // 4) for j in range(22)], [uk])
            for half in range(2):
                hs = slice(512 * half, 512 * half + 512)
                pb, pk = pbank()
                for k in range(22):
                    kb.mm(pb[:], uT[:, k, :], wfo[:, k, hs], k == 0, k == 21, [uk, "wfo"], [pk])
                kb.v("dve", lambda: V.tensor_add(out=x_sb[:, t, hs], in0=x_sb[:, t, hs], in1=pb[:]), [("x", t), pk], [("x", t)])
        kb.release(mk)

    def final_norm(s):
        mk = kb.mark()
        gbc = kb.sb("gbcf", [128, D], F32)
        kb.dma(gbc[:], bass.AP(tensor=norm_final_g.tensor, offset=0, ap=[[0, 128], [1, D]]), [], ["gbcf"])
        junk = Rot(kb, "junkf", [128, D], BF16, 2)
        ssr = Rot(kb, "ssf", [128, 4], F32, 3)
        yr = Rot(kb, "yo", [128, D], F32, 3)
        for t in range(NT):
            jt, jk = junk.next(); ss, sk = ssr.next(); yo, yk = yr.next()
            kb.act(jt[:], x_sb[:, t, :], AF.Square, [("x", t)], [jk, sk + "a"], accum_out=ss[:, 0:1])
            kb.act(ss[:, 1:2], ss[:, 0:1], AF.Sqrt, [sk + "a"], [sk + "b"], scale=1.0 / D, bias=1e-6)
            kb.v("dve", lambda: V.reciprocal(out=ss[:, 2:3], in_=ss[:, 1:2]), [sk + "b"], [sk + "c"])
            kb.v("dve", lambda: V.scalar_tensor_tensor(out=yo[:], in0=x_sb[:, t, :], scalar=ss[:, 2:3], in1=gbc[:],
                                                       op0=ALU.mult, op1=ALU.mult), [("x", t), sk + "c", "gbcf"], [yk])
            ev = kb.dma(y_d[s * T + 128 * t:s * T + 128 * t + 128, :], yo[:], [yk], [("y", s, t)], q="sp")
            out_events.append(ev)
        kb.release(mk)

    out_events = []

    for s in range(nseq):
        for t in range(NT):
            kb.dma(x_sb[:, t, :], x_d[s * T + 128 * t:s * T + 128 * t + 128, :], [], [("x", t)])
        for li in range(depth):
            mk = kb.mark()
            hT = kb.sb("hT", [128, 8, T], BF16)
            rmsnorm_to_hT(hT, bass.AP(tensor=norm_mix_g.tensor, offset=li * D, ap=[[0, 128], [1, D]]))
            if stop_after == "norm":
                break
            stage_proj(li, hT)
            kb.release(mk)
            if stop_after == "proj":
                break
            stage_retention(li)
            if stop_after == "ret":
                break
            try:
                stage_nsa(li)
            except _Stop:
                break
            if stop_after == "nsa":
                break
            stage_merge(li)
            if stop_after == "merge":
                break
            mk = kb.mark()
            hT = kb.sb("hT2", [128, 8, T], BF16)
            rmsnorm_to_hT(hT, bass.AP(tensor=norm_ffn_g.tensor, offset=li * D, ap=[[0, 128], [1, D]]))
            stage_ffn(li, hT)
            kb.release(mk)
            kb.prune()
        final_norm(s)

    sp = nc.sync
    for sname, sd in kb.streams.items():
        if sd["count"] > 0:
            sp.wait_ge(sd["sem"], sd["count"] * sd["inc"])
    kb.release(0)
    return nc, kb


_CACHE = {}


def _host_inputs(inputs):
    C = _consts()
    f = lambda a: np.ascontiguousarray(np.asarray(a, dtype=np.float32))
    m = {
        "w_in": f(inputs["w_in"]).reshape(DEPTH * D, CIN),
        "cmp_w1_k": f(inputs["cmp_w1_k"]).reshape(DEPTH * 2048, 256),
        "cmp_w2_k": f(inputs["cmp_w2_k"]).reshape(DEPTH * 256, 64),
        "cmp_w1_v": f(inputs["cmp_w1_v"]).reshape(DEPTH * 2048, 256),
        "cmp_w2_v": f(inputs["cmp_w2_v"]).reshape(DEPTH * 256, 64),
        "w_o_ret": f(inputs["w_o_ret"]).reshape(DEPTH * 2048, D),
        "w_o_nsa": f(inputs["w_o_nsa"]).reshape(DEPTH * D, D),
        "w_out": f(inputs["w_out"]).reshape(DEPTH * D, D),
        "w_ffn_in": f(inputs["w_ffn_in"]).reshape(DEPTH * D, 2 * FFN),
        "w_ffn_out": f(inputs["w_ffn_out"]).reshape(DEPTH * FFN, D),
        "norm_mix_g": f(inputs["norm_mix_g"]),
        "norm_ffn_g": f(inputs["norm_ffn_g"]),
        "norm_final_g": f(inputs["norm_final_g"]).reshape(1, D),
        "cmp_pos_k": f(inputs["cmp_pos_k"]).reshape(DEPTH * 32, 64),
        "cmp_pos_v": f(inputs["cmp_pos_v"]).reshape(DEPTH * 32, 64),
        "rel_bias": f(inputs["rel_bias"]).reshape(1, 512),
        "c_cos": C["cosT"], "c_sin": C["sinT"], "c_ident": C["ident"],
        "c_dmask": C["dmaskT"].reshape(128, 512), "c_retsc": C["retsc"],
        "c_dist0": C["dist0"], "c_dist1": C["dist1"], "c_distg": C["distg"],
        "c_wedge": C["wedge"], "c_expand": C["expand"], "c_overlap": C["overlap"],
        "c_keep": C["keep"].reshape(128, 512), "c_addm": C["addm"].reshape(128, 512),
    }
    return m


def kernel(**inputs):
    x = np.ascontiguousarray(np.asarray(inputs["x"], dtype=np.float32))
    B = x.shape[0]
    if "nc" not in _CACHE:
        _CACHE["nc"] = build()[0]
    nc = _CACHE["nc"]
    shared = _host_inputs(inputs)
    per = B // NCORES
    in_maps = []
    for c in range(NCORES):
        m = dict(shared)
        m["x"] = x[c * per:(c + 1) * per].reshape(per * T, D)
        in_maps.append(m)
    res = run_bass_kernel_spmd(nc, in_maps, core_ids=list(range(NCORES)))
    out = np.concatenate([r["y"].reshape(per, T, D) for r in res.results], axis=0)
    return out.astype(np.float32)
```
